# Optimizing a Trainium2 kernel written in Bass

```python
import jax, jax.numpy as jnp
from jax import lax
import numpy as np

D_MODEL = 1024
BATCH = 32
SEQ = 2048
DEPTH = 1

CHUNK = 64
Q_BLOCK = 128
A_HEADS = 8
A_HEAD_DIM = 64
A_WIDTH = A_HEADS * A_HEAD_DIM
KV_LATENT = 128
IDX_HEADS = 8
IDX_DIM = 64
TOPK_MAX = 256
B_HEADS = 8
B_HEAD_DIM = 64
B_WIDTH = B_HEADS * B_HEAD_DIM
W_LORA = 64
A_LORA = 64
G_LORA = 128
D_FF = 4 * D_MODEL
RMS_EPS = 1e-6
GN_EPS = 64e-5
N_IN_A = A_WIDTH + KV_LATENT + IDX_HEADS * IDX_DIM + IDX_DIM + IDX_HEADS
N_IN_B = 3 * B_WIDTH + W_LORA + A_LORA + G_LORA
N_IN = N_IN_A + N_IN_B
MIX_WIDTH = A_WIDTH + B_WIDTH

kernel_name = 'hybrid_dsa_rwkv7_adaln_block'


def rms_norm(x, g):
    xf = x.astype(jnp.float32)
    y = xf * lax.rsqrt(jnp.mean(xf * xf, axis=-1, keepdims=True) + RMS_EPS)
    return (y * g).astype(x.dtype)


def split_cols(p, sizes):
    outs, start = [], 0
    for s in sizes:
        outs.append(p[..., start:start + s])
        start += s
    return outs


def token_shift(p, mu):
    prev = jnp.pad(p, ((0, 0), (1, 0), (0, 0)))[:, :-1]
    return p + mu * (prev - p)


def dsa_attention(q, c_kv, q_idx, k_idx, w_idx, g_q, g_k, w_uk, w_uv):
    B_, S, H, HD = q.shape
    f32 = jnp.float32
    topk = min(TOPK_MAX, S // 4)
    k_full = jnp.einsum('bsl,lhd->bshd', c_kv, w_uk).astype(f32)
    inv_rms_k = lax.rsqrt(jnp.mean(k_full * k_full, axis=-1) + RMS_EPS)
    q_abs = jnp.einsum('bshd,lhd->bshl', rms_norm(q, g_q) * g_k, w_uk) * (HD ** -0.5)
    w_idx = w_idx * (IDX_HEADS ** -0.5 * IDX_DIM ** -0.5)
    slopes = jnp.exp2(-8.0 * jnp.arange(1, H + 1, dtype=f32) / H)
    key_pos = jnp.arange(S)
    gather = jax.vmap(lambda table, idx: table[idx])

    def block(i):
        t0 = i * Q_BLOCK
        tpos = t0 + jnp.arange(Q_BLOCK)
        limit = (tpos // CHUNK + 1) * CHUNK
        qi = lax.dynamic_slice_in_dim(q_idx, t0, Q_BLOCK, axis=1)
        wi = lax.dynamic_slice_in_dim(w_idx, t0, Q_BLOCK, axis=1)
        qa = lax.dynamic_slice_in_dim(q_abs, t0, Q_BLOCK, axis=1)
        rel = jax.nn.relu(jnp.einsum('bthd,bsd->bths', qi, k_idx).astype(f32))
        score_idx = jnp.einsum('bth,bths->bts', wi.astype(f32), rel)
        score_idx = jnp.where(key_pos[None, :] < limit[:, None], score_idx, -jnp.inf)
        _, sel = lax.top_k(score_idx, topk)
        valid = sel < limit[None, :, None]
        c_sel = gather(c_kv, sel)
        r_sel = gather(inv_rms_k, sel)
        logits = jnp.einsum('bthl,btkl->bthk', qa, c_sel).astype(f32) * jnp.swapaxes(r_sel, 2, 3)
        dist = jnp.abs(tpos[None, :, None] - sel).astype(f32)
        logits = logits - slopes[None, None, :, None] * dist[:, :, None, :]
        logits = jnp.where(valid[:, :, None, :], logits, -jnp.inf)
        probs = jax.nn.softmax(logits, axis=-1).astype(c_kv.dtype)
        return jnp.einsum('bthk,btkl->bthl', probs, c_sel)

    o_lat = lax.map(block, jnp.arange(S // Q_BLOCK))
    o_lat = jnp.moveaxis(o_lat, 0, 1).reshape(B_, S, H, KV_LATENT)
    return jnp.einsum('bshl,lhd->bshd', o_lat, w_uv)


def rwkv7_time_mix(p, w0, w2, a0, a2, g2, k_k, k_a, r_k, ln_w, ln_b):
    B_, S, _ = p.shape
    H, N = B_HEADS, B_HEAD_DIM
    f32 = jnp.float32
    r, k, v, xw, xa, xg = split_cols(p, (B_WIDTH, B_WIDTH, B_WIDTH, W_LORA, A_LORA, G_LORA))
    w_log = -jax.nn.softplus(-(w0 + jnp.tanh(xw) @ w2)) - 0.5
    decay = jnp.exp(-jnp.exp(w_log.astype(f32)))
    a = jax.nn.sigmoid(a0 + xa @ a2)
    g = jax.nn.sigmoid(xg) @ g2
    heads = lambda t: t.astype(f32).reshape(B_, S, H, N)
    kk = heads(k * k_k)
    kk = kk / jnp.maximum(jnp.sqrt(jnp.sum(kk * kk, axis=-1, keepdims=True)), 1e-12)
    k = k * (1 + (a - 1) * k_a)
    r_h, k_h, v_h, a_h, w_h = heads(r), heads(k), heads(v), heads(a), heads(decay)

    def step(state, inp):
        r_t, w_t, k_t, v_t, kk_t, a_t = inp
        sa = jnp.einsum('bhij,bhj->bhi', state, -kk_t)
        state = (state * w_t[:, :, None, :] + sa[..., None] * (kk_t * a_t)[:, :, None, :]
                 + v_t[..., None] * k_t[:, :, None, :])
        return state, jnp.einsum('bhij,bhj->bhi', state, r_t)

    xs = tuple(jnp.moveaxis(t, 1, 0) for t in (r_h, w_h, k_h, v_h, kk, a_h))
    state0 = jnp.zeros((B_, H, N, N), f32)
    _, out = lax.scan(step, state0, xs)
    out = jnp.moveaxis(out, 0, 1)
    mean = jnp.mean(out, axis=-1, keepdims=True)
    var = jnp.mean((out - mean) ** 2, axis=-1, keepdims=True)
    out = (out - mean) * lax.rsqrt(var + GN_EPS) * ln_w.reshape(H, N) + ln_b.reshape(H, N)
    out = out + jnp.sum(r_h * k_h * r_k, axis=-1, keepdims=True) * v_h
    return (out.reshape(B_, S, B_WIDTH) * g.astype(f32)).astype(p.dtype)


def setup_inputs(seed: int = 0) -> dict:
    key = jax.random.key(seed)
    ks = jax.random.split(key, 32)
    f32 = jnp.float32
    nrm = lambda k, shape, scale: jax.random.normal(k, shape, f32) * scale
    L = DEPTH
    return {
        'x': nrm(ks[0], (BATCH, SEQ, D_MODEL), 1.0),
        'c': nrm(ks[1], (BATCH, D_MODEL), 1.0),
        'w_ada': nrm(ks[2], (L, D_MODEL, 6 * D_MODEL), 0.2 * D_MODEL ** -0.5),
        'b_ada': nrm(ks[3], (L, 6 * D_MODEL), 0.01),
        'g_mix': 1.0 + nrm(ks[4], (L, D_MODEL), 0.02),
        'g_ffn': 1.0 + nrm(ks[5], (L, D_MODEL), 0.02),
        'w_in': nrm(ks[6], (L, D_MODEL, N_IN), D_MODEL ** -0.5),
        'g_q': 1.0 + nrm(ks[7], (L, A_HEAD_DIM), 0.02),
        'g_k': 1.0 + nrm(ks[8], (L, A_HEAD_DIM), 0.02),
        'g_kv': 1.0 + nrm(ks[9], (L, KV_LATENT), 0.02),
        'w_uk': nrm(ks[10], (L, KV_LATENT, A_HEADS, A_HEAD_DIM), KV_LATENT ** -0.5),
        'w_uv': nrm(ks[11], (L, KV_LATENT, A_HEADS, A_HEAD_DIM), KV_LATENT ** -0.5),
        'mu_shift': jax.random.uniform(ks[12], (L, N_IN_B), f32),
        'w0': jax.random.uniform(ks[13], (L, B_WIDTH), f32, -5.0, -1.0),
        'w2': nrm(ks[14], (L, W_LORA, B_WIDTH), 0.1 * W_LORA ** -0.5),
        'a0': nrm(ks[15], (L, B_WIDTH), 0.1),
        'a2': nrm(ks[16], (L, A_LORA, B_WIDTH), 0.1 * A_LORA ** -0.5),
        'g2': nrm(ks[17], (L, G_LORA, B_WIDTH), G_LORA ** -0.5),
        'k_k': 0.85 + nrm(ks[18], (L, B_WIDTH), 0.02),
        'k_a': 1.0 + nrm(ks[19], (L, B_WIDTH), 0.02),
        'r_k': nrm(ks[20], (L, B_HEADS, B_HEAD_DIM), 0.1),
        'ln_w': 1.0 + nrm(ks[21], (L, B_WIDTH), 0.02),
        'ln_b': nrm(ks[22], (L, B_WIDTH), 0.01),
        'w_out': nrm(ks[23], (L, MIX_WIDTH, D_MODEL), MIX_WIDTH ** -0.5),
        'w_ff1': nrm(ks[24], (L, D_MODEL, D_FF), D_MODEL ** -0.5),
        'w_ff2': nrm(ks[25], (L, D_FF, D_MODEL), D_FF ** -0.5),
    }


def reference(x, c, w_ada, b_ada, g_mix, g_ffn, w_in, g_q, g_k, g_kv, w_uk, w_uv,
              mu_shift, w0, w2, a0, a2, g2, k_k, k_a, r_k, ln_w, ln_b,
              w_out, w_ff1, w_ff2):
    B_, S, D = x.shape
    for l in range(DEPTH):
        mod = jnp.einsum('bd,de->be', jax.nn.silu(c), w_ada[l]) + b_ada[l]
        sh1, sc1, gt1, sh2, sc2, gt2 = [m[:, None, :] for m in jnp.split(mod, 6, axis=-1)]
        h = rms_norm(x, g_mix[l]) * (1 + sc1) + sh1
        proj = jnp.einsum('bsd,de->bse', h, w_in[l])
        p_a, p_b = proj[..., :N_IN_A], proj[..., N_IN_A:]
        q, c_lat, q_idx, k_idx, w_idx = split_cols(
            p_a, (A_WIDTH, KV_LATENT, IDX_HEADS * IDX_DIM, IDX_DIM, IDX_HEADS))
        o_a = dsa_attention(q.reshape(B_, S, A_HEADS, A_HEAD_DIM), rms_norm(c_lat, g_kv[l]),
                            q_idx.reshape(B_, S, IDX_HEADS, IDX_DIM), k_idx, w_idx,
                            g_q[l], g_k[l], w_uk[l], w_uv[l])
        o_b = rwkv7_time_mix(token_shift(p_b, mu_shift[l]), w0[l], w2[l], a0[l], a2[l], g2[l],
                             k_k[l], k_a[l], r_k[l], ln_w[l], ln_b[l])
        mixed = jnp.concatenate([o_a.reshape(B_, S, A_WIDTH).astype(x.dtype), o_b], axis=-1)
        x = x + gt1 * jnp.einsum('bse,ed->bsd', mixed, w_out[l])
        h = rms_norm(x, g_ffn[l]) * (1 + sc2) + sh2
        u = jax.nn.relu(jnp.einsum('bsd,df->bsf', h, w_ff1[l]))
        x = x + gt2 * jnp.einsum('bsf,fd->bsd', u * u, w_ff2[l])
    return x
```

```python
import contextlib
import os
import numpy as np
import concourse.bass as bass
import concourse.mybir as mybir
from concourse.bass_utils import run_bass_kernel_spmd


F32 = mybir.dt.float32
BF16 = mybir.dt.bfloat16
AF = mybir.ActivationFunctionType
ALU = mybir.AluOpType
AX = mybir.AxisListType

EPOCH = 16000
ENGS = ['pe', 'dve', 'act', 'pool', 'sp']


class Op:
    __slots__ = ('eng', 'fn', 'deps', 'pos', 'signal', 'seq', 'is_dma', 'ctr', 'val', 'waits', 'tag')


class Counter:
    def __init__(self, name):
        self.name = name
        self.sems = []
        self.count = 0

    def need(self, prog, v):
        ep = (v - 1) // EPOCH
        while len(self.sems) <= ep:
            self.sems.append(prog.new_sem())

    def sem_for(self, v):
        ep = (v - 1) // EPOCH
        return self.sems[ep], v - ep * EPOCH


class Prog:
    def __init__(self, nc, stack):
        self.nc = nc
        self.stack = stack
        self.eng_ops = {e: [] for e in ENGS}
        self.res = {}
        self.engctr = {e: Counter(e) for e in ENGS}
        self.dmactr = {}
        self.nsem = 0
        self.ntile = 0
        self.all_dma = []
        self.keep = []
        self._bar_t = {e: self.sb([1, 8], F32, name=f"bar{e}") for e in ENGS}
        self._bar_ps = self.ps([1, 8], F32, name="barps")
        self._bar_tok = {e: f"__bar_{e}" for e in ENGS}
        self._bar_init = False
        self._bar_last = {}

    def new_sem(self):
        self.nsem += 1
        return self.stack.enter_context(self.nc.semaphore(f"sm{self.nsem}"))

    def sb(self, shape, dtype, name=None, stack=None):
        self.ntile += 1
        nm = f"{name or 't'}_{self.ntile}"
        t = (stack or self.stack).enter_context(self.nc.sbuf_tensor(nm, list(shape), dtype))
        self.keep.append(t)
        return t

    def ps(self, shape, dtype=F32, name=None, stack=None):
        self.ntile += 1
        nm = f"{name or 'p'}_{self.ntile}"
        t = (stack or self.stack).enter_context(self.nc.psum_tensor(nm, list(shape), dtype))
        self.keep.append(t)
        return t

    @staticmethod
    def _key(r):
        if isinstance(r, tuple):
            return id(r[0]), r[1]
        return id(r), None

    def _deps(self, op, reads, writes):
        deps = []
        for r in reads:
            tk, sk = self._key(r)
            ent = self.res.setdefault(tk, {})
            for k2, e2 in ent.items():
                if sk is None or k2 is None or k2 == sk:
                    if e2[0] is not None:
                        deps.append((e2[0], 'raw'))
            e = ent.setdefault(sk, [None, []])
            if not op.is_dma:
                e[1] = [o for o in e[1] if o.is_dma or o.eng != op.eng]
            e[1].append(op)
        for w in writes:
            tk, sk = self._key(w)
            ent = self.res.setdefault(tk, {})
            for k2, e2 in ent.items():
                if sk is None or k2 is None or k2 == sk:
                    if e2[0] is not None:
                        deps.append((e2[0], 'waw'))
                    for o in e2[1]:
                        if o is not op:
                            deps.append((o, 'war'))
            if sk is None:
                ent.clear()
            ent[sk] = [op, []]
        return deps

    def op(self, eng, fn, reads=(), writes=(), tag=None):
        o = Op()
        o.eng = eng
        o.fn = fn
        o.is_dma = False
        o.signal = False
        o.seq = 0
        o.ctr = None
        o.val = 0
        o.tag = tag
        o.deps = self._deps(o, reads, writes)
        self.eng_ops[eng].append(o)
        o.pos = len(self.eng_ops[eng])
        return o

    def dma(self, q, out, in_, reads=(), writes=(), ctr=None, tag=None, **kw):
        o = Op()
        o.eng = q
        o.is_dma = True
        o.signal = True
        o.seq = 0
        o.tag = tag
        o.fn = lambda e: e.dma_start(out=out, in_=in_, **kw)
        ck = self._key(ctr)
        c = self.dmactr.get(ck)
        if c is None:
            c = self.dmactr[ck] = Counter(f"d{len(self.dmactr)}")
        c.count += 16
        o.ctr = c
        o.val = c.count
        o.deps = self._deps(o, reads, writes)
        self.eng_ops[q].append(o)
        o.pos = len(self.eng_ops[q])
        self.all_dma.append(o)
        return o

    def barrier(self):
        if not hasattr(self, '_bar_t'):
            self._bar_t = {e: self.sb([1, 8], F32, name=f"bar{e}") for e in ENGS}
            self._bar_ps = self.ps([1, 8], F32, name="barps")
            self._bar_tok = {e: f"__bar_{e}" for e in ENGS}
            self.keep.append(self._bar_tok)
        bt = self._bar_t
        if not self._bar_init:
            self._bar_init = True
            for e_ in ('pe', 'act'):
                self.op('dve', (lambda eng, e_=e_: eng.memset(bt[e_][:], 0.0)), writes=[bt[e_]])
        dmas = [d for d in self.all_dma]
        self.all_dma = []
        first = []
        for e in ENGS:
            if e == 'pe':
                fn = lambda eng: eng.matmul(self._bar_ps[0:1, 0:1], bt['pe'][0:1, 0:1], bt['pe'][0:1, 1:2], start=True, stop=True)
            elif e == 'sp':
                fn = lambda eng: eng.nop()
            elif e == 'act':
                fn = lambda eng: eng.activation(out=bt['act'][0:1, 2:3], in_=bt['act'][0:1, 3:4], func=AF.Copy)
            elif e == 'dve':
                fn = lambda eng: eng.memset(bt['dve'][0:1, 2:3], 0.0)
            else:
                fn = lambda eng: eng.memset(bt['pool'][0:1, 2:3], 0.0)
            o = self.op(e, fn, reads=([bt[e]] if e in ('pe', 'act') else []), writes=[(self._bar_tok[e], 0)])
            if e in self._bar_last:
                o.deps.append((self._bar_last[e], 'waw'))
            self._bar_last[e] = o
            if e == 'sp':
                o.deps += [(d, 'raw') for d in dmas]
            first.append(o)
        for e in ENGS:
            if e == 'pe':
                fn = lambda eng: eng.matmul(self._bar_ps[0:1, 4:5], bt['pe'][0:1, 0:1], bt['pe'][0:1, 1:2], start=True, stop=True)
            elif e == 'sp':
                fn = lambda eng: eng.nop()
            elif e == 'act':
                fn = lambda eng: eng.activation(out=bt['act'][0:1, 4:5], in_=bt['act'][0:1, 3:4], func=AF.Copy)
            elif e == 'dve':
                fn = lambda eng: eng.memset(bt['dve'][0:1, 4:5], 0.0)
            else:
                fn = lambda eng: eng.memset(bt['pool'][0:1, 4:5], 0.0)
            o = self.op(e, fn, reads=([bt[e]] if e in ('pe', 'act') else []))
            o.deps += [(f, 'raw') for f in first if f.eng != e]
            o.deps.append((self._bar_last[e], 'waw'))
            self._bar_last[e] = o
        self.res.clear()

    def init_bar(self):
        self.barrier_ready = True

    def emit(self, final_wait_ops=()):
        fin = self.op('sp', lambda eng: eng.nop())
        fin.deps += [(d, 'raw') for d in final_wait_ops]
        for e in ENGS:
            waited = {}
            for op in self.eng_ops[e]:
                op.waits = []
                best = {}
                for d, kind in op.deps:
                    if d.is_dma:
                        key = ('c', id(d.ctr))
                        v = d.val
                    else:
                        if d.eng == e and kind != 'raw' and e == 'pe':
                            continue
                        key = ('e', d.eng)
                        v = d.pos
                    if waited.get(key, 0) >= v:
                        continue
                    if key not in best or best[key][0] < v:
                        best[key] = (v, d)
                for key, (v, d) in best.items():
                    waited[key] = v
                    d.signal = True
                    op.waits.append(d)
        for e in ENGS:
            seq = 0
            for op in self.eng_ops[e]:
                if op.is_dma:
                    op.ctr.need(self, op.val)
                elif op.signal:
                    seq += 1
                    op.seq = seq
                    self.engctr[e].need(self, seq)
        nc = self.nc

        def run(e, eng):
            for op in self.eng_ops[e]:
                for d in op.waits:
                    if d.is_dma:
                        sem, v = d.ctr.sem_for(d.val)
                    else:
                        sem, v = self.engctr[d.eng].sem_for(d.seq)
                    eng.wait_ge(sem, v)
                ins = op.fn(eng)
                if op.is_dma:
                    sem, _ = op.ctr.sem_for(op.val)
                    ins.then_inc(sem, 16)
                elif op.signal:
                    sem, _ = self.engctr[e].sem_for(op.seq)
                    ins.then_inc(sem, 1)

        with nc.Block() as block:
            @block.tensor
            def _(eng):
                run('pe', eng)

            @block.vector
            def _(eng):
                run('dve', eng)

            @block.scalar
            def _(eng):
                run('act', eng)

            @block.gpsimd
            def _(eng):
                run('pool', eng)

            @block.sync
            def _(eng):
                run('sp', eng)

    def stats(self):
        return {e: len(v) for e, v in self.eng_ops.items()}, self.nsem


def _apname(a):
    try:
        return a.name
    except Exception:
        return a.tensor.name


def I(P, eng, meth, wsub=None, rsub=None, extra_reads=(), extra_writes=(), **kw):
    reads, writes = list(extra_reads), list(extra_writes)
    for k, v in kw.items():
        if isinstance(v, bass.AP):
            nm = _apname(v)
            if k in ('out', 'accum_out'):
                writes.append((nm, wsub.get(nm) if isinstance(wsub, dict) else wsub))
            else:
                reads.append((nm, rsub.get(nm) if isinstance(rsub, dict) else rsub))
    return P.op(eng, (lambda e: getattr(e, meth)(**kw)), reads=reads, writes=writes)


def _key2(r):
    if isinstance(r, tuple):
        a, s = r
    else:
        a, s = r, None
    if isinstance(a, str):
        return a, s
    try:
        return a.name, s
    except Exception:
        return a.tensor.name, s


Prog._key = staticmethod(_key2)


D = 1024
DFF = 4096
RMS_EPS = 1e-6


def bcast_rows(ap1d, n):
    return ap1d.partition_broadcast(n)


def phase_mod(P, nc, cT, w_ada, b_ada, g_mix, g_ffn, mod_d, NB):
    with contextlib.ExitStack() as st:
        ct = P.sb([128, 8, NB], F32, 'ct', st)
        sil = P.sb([128, 8, NB], F32, 'sil', st)
        ones = P.sb([1, NB], F32, 'ones', st)
        mod = P.sb([NB, 6 * D], F32, 'mod', st)
        gb = P.sb([NB, 2, D], F32, 'gb', st)
        wa = [P.sb([128, 8, 512], F32, 'wa', st) for _ in range(2)]
        ba = [P.sb([1, 512], F32, 'ba', st) for _ in range(2)]
        pm = [P.ps([128, 512], F32, 'pm', st) for _ in range(2)]
        P.dma('sp', ct[:], cT, writes=[ct], ctr=ct)
        P.op('act', lambda e: e.activation(out=sil[:], in_=ct[:], func=AF.Silu), reads=[ct], writes=[sil])
        P.op('dve', lambda e: e.memset(ones[:], 1.0), writes=[ones])
        P.dma('sp', gb[:, 0, :], g_mix.partition_broadcast(NB), writes=[(gb, 0)], ctr=gb)
        P.dma('sp', gb[:, 1, :], g_ffn.partition_broadcast(NB), writes=[(gb, 1)], ctr=gb)
        for cg in range(12):
            w = wa[cg % 2]
            bb = ba[cg % 2]
            pp = pm[cg % 2]
            P.dma('sp', w[:], w_ada[:, cg * 512:(cg + 1) * 512].rearrange("(k p) e -> p k e", p=128),
                  writes=[w], ctr=w)
            P.dma('sp', bb[:], b_ada[cg * 512:(cg + 1) * 512].partition_broadcast(1), writes=[bb], ctr=bb)
            for k in range(8):
                P.op('pe', (lambda e, k=k, w=w, pp=pp: e.matmul(pp[0:NB, :], sil[:, k, :], w[:, k, :], start=(k == 0), stop=False)),
                     reads=[sil, w], writes=[pp])
            P.op('pe', (lambda e, bb=bb, pp=pp: e.matmul(pp[0:NB, :], ones[:, :], bb[:, :], start=False, stop=True)),
                 reads=[ones, bb], writes=[pp])
            P.op('act', (lambda e, pp=pp, cg=cg: e.activation(out=mod[:, cg * 512:(cg + 1) * 512], in_=pp[0:NB, :], func=AF.Copy)),
                 reads=[pp], writes=[(mod, cg)])
        for r, gi in ((1, 0), (4, 1)):
            P.op('dve', (lambda e, r=r, gi=gi: e.scalar_tensor_tensor(out=mod[:, r * D:(r + 1) * D], in0=mod[:, r * D:(r + 1) * D],
                                                                     scalar=1.0, in1=gb[:, gi, :], op0=ALU.add, op1=ALU.mult)),
                 reads=[mod, gb], writes=[mod])
        o = P.dma('sp', mod_d.rearrange("b r d -> b (r d)"), mod[:], reads=[mod], writes=[mod_d], ctr=mod)
    return o


def phase_ffn(P, nc, xin, xout, w_ff1, w_ff2, mod_d, ident_d, NB, S):
    NT = NB * S
    TG = 256
    outs = []
    with contextlib.ExitStack() as st:
        w1 = P.sb([128, 8, DFF], BF16, 'w1', st)
        w2 = P.sb([128, 32, D], BF16, 'w2', st)
        ident = P.sb([128, 128], BF16, 'ident', st)
        sh2f = P.sb([128, 8, NB], F32, 'sh2f', st)
        sh2b = P.sb([128, 8, NB], BF16, 'sh2b', st)
        bias1 = P.sb([128, 32, NB], F32, 'bias1', st)
        G2bc = P.sb([128, D], F32, 'G2bc', st)
        gt2bc = P.sb([128, D], F32, 'gt2bc', st)
        xt = [P.sb([128, D], F32, 'xt', st) for _ in range(4)]
        xn = [P.sb([128, D], BF16, 'xn', st) for _ in range(2)]
        junk = P.sb([128, D], BF16, 'junk', st)
        epsc = P.sb([128, 1], F32, 'epsc', st)
        P.op('dve', lambda e: e.memset(epsc[:], RMS_EPS), writes=[epsc])
        ss = [P.sb([128, 2], F32, 'ss', st) for _ in range(2)]
        h2T = [P.sb([128, 8, TG], BF16, 'h2T', st) for _ in range(2)]
        u2T = P.sb([128, 32, TG], BF16, 'u2T', st)
        rr = [P.sb([128, TG], BF16, 'rr', st) for _ in range(2)]
        tmp = [P.sb([128, 512], F32, 'tmp', st) for _ in range(2)]
        tp = [P.ps([128, 1024], BF16, 'tp', st) for _ in range(2)]
        ps1 = [P.ps([128, 512], F32, 'ps1', st) for _ in range(2)]
        ps2 = [P.ps([128, 512], F32, 'ps2', st) for _ in range(2)]

        P.dma('pool', ident[:], ident_d, writes=[ident], ctr=ident)
        for k in range(8):
            P.dma('pool', w1[:, k, :], w_ff1[k * 128:(k + 1) * 128, :], writes=[(w1, k)], ctr=w1)
        for c in range(32):
            P.dma('pool', w2[:, c, :], w_ff2[c * 128:(c + 1) * 128, :], writes=[(w2, c)], ctr=w2)
        for b_ in range(NB):
            P.dma('sp', sh2f[:, :, b_], mod_d[b_, 3, :].rearrange("(k p) -> p k", p=128), writes=[(sh2f, b_)], ctr=sh2f,
                  allow_slow_non_contiguous=True)
        P.op('dve', lambda e: e.tensor_copy(out=sh2b[:], in_=sh2f[:]), reads=[sh2f], writes=[sh2b])
        pb = ps1[0]
        for c in range(32):
            for k in range(8):
                P.op('pe', (lambda e, c=c, k=k: e.matmul(pb[:, c * NB:(c + 1) * NB], w1[:, k, c * 128:(c + 1) * 128], sh2b[:, k, :],
                                                        start=(k == 0), stop=(k == 7))),
                     reads=[w1, sh2b], writes=[pb])
        P.op('act', lambda e: e.activation(out=bias1[:].rearrange("p c b -> p (c b)"), in_=pb[:, 0:32 * NB], func=AF.Copy),
             reads=[pb], writes=[bias1])

        ng = NT // TG
        cur_b = -1
        for g in range(ng):
            b = (g * TG) // S
            if b != cur_b:
                cur_b = b
                P.dma('sp', G2bc[:], mod_d[b, 4, :].partition_broadcast(128), writes=[G2bc], ctr=G2bc)
                P.dma('sp', gt2bc[:], mod_d[b, 5, :].partition_broadcast(128), writes=[gt2bc], ctr=gt2bc)
            hT = h2T[g % 2]
            xs = [xt[(g % 2) * 2 + j] for j in range(2)]
            for j in range(2):
                r0 = g * TG + j * 128
                x = xs[j]
                P.dma('sp', x[:], xin[r0:r0 + 128, :], writes=[x], ctr=x)
                s_ = ss[j]
                xb = xn[j]
                tpp = tp[j]
                P.op('act', (lambda e, x=x, s_=s_: e.activation(out=junk[:], in_=x[:], func=AF.Square, accum_out=s_[:, 0:1])),
                     reads=[x], writes=[junk, s_])
                P.op('act', (lambda e, s_=s_: e.activation(out=s_[:, 1:2], in_=s_[:, 0:1], func=AF.Sqrt, scale=1.0 / D, bias=epsc[:, 0:1])),
                     reads=[s_, epsc], writes=[s_])
                P.op('dve', (lambda e, s_=s_: e.reciprocal(out=s_[:, 1:2], in_=s_[:, 1:2])), reads=[s_], writes=[s_])
                P.op('dve', (lambda e, x=x, s_=s_, xb=xb: e.scalar_tensor_tensor(out=xb[:], in0=x[:], scalar=s_[:, 1:2], in1=G2bc[:],
                                                                               op0=ALU.mult, op1=ALU.mult)),
                     reads=[x, s_, G2bc], writes=[xb])
                for k in range(8):
                    P.op('pe', (lambda e, k=k, xb=xb, tpp=tpp: e.transpose(out=tpp[:, k * 128:(k + 1) * 128], in_=xb[:, k * 128:(k + 1) * 128],
                                                                        identity=ident[:])),
                         reads=[xb, ident], writes=[tpp])
                P.op('act', (lambda e, tpp=tpp, hT=hT, j=j: e.activation(out=hT[:, :, j * 128:(j + 1) * 128],
                                                                       in_=tpp[:].rearrange("p (k t) -> p k t", k=8), func=AF.Copy)),
                     reads=[tpp], writes=[(hT, j)])
            for c in range(32):
                pp = ps1[c % 2]
                r_ = rr[c % 2]
                for k in range(8):
                    P.op('pe', (lambda e, c=c, k=k, pp=pp, hT=hT: e.matmul(pp[:, 0:TG], w1[:, k, c * 128:(c + 1) * 128], hT[:, k, :],
                                                                        start=(k == 0), stop=(k == 7))),
                         reads=[w1, hT], writes=[pp])
                P.op('act', (lambda e, c=c, pp=pp, r_=r_, b=b: e.activation(out=r_[:], in_=pp[:, 0:TG], func=AF.Relu, bias=bias1[:, c, b:b + 1])),
                     reads=[pp, bias1], writes=[r_])
                P.op('dve', (lambda e, c=c, r_=r_: e.tensor_tensor(out=u2T[:, c, :], in0=r_[:], in1=r_[:], op=ALU.mult)),
                     reads=[r_], writes=[(u2T, c)])
            for j in range(2):
                x = xs[j]
                for hf in range(2):
                    pp = ps2[hf]
                    tm = tmp[hf]
                    for c in range(32):
                        P.op('pe', (lambda e, c=c, j=j, hf=hf, pp=pp: e.matmul(pp[:], u2T[:, c, j * 128:(j + 1) * 128], w2[:, c, hf * 512:(hf + 1) * 512],
                                                                            start=(c == 0), stop=(c == 31))),
                             reads=[(u2T, c), w2], writes=[pp])
                    P.op('dve', (lambda e, pp=pp, tm=tm, hf=hf: e.tensor_tensor(out=tm[:], in0=pp[:], in1=gt2bc[:, hf * 512:(hf + 1) * 512], op=ALU.mult)),
                         reads=[pp, gt2bc], writes=[tm])
                    P.op('dve', (lambda e, x=x, tm=tm, hf=hf: e.tensor_tensor(out=x[:, hf * 512:(hf + 1) * 512], in0=tm[:], in1=x[:, hf * 512:(hf + 1) * 512], op=ALU.add)),
                         reads=[tm, x], writes=[x])
                r0 = g * TG + j * 128
                outs.append(P.dma('sp', xout[r0:r0 + 128, :], x[:], reads=[x], writes=[(xout, r0)], ctr=x))
    return outs


NPROJ = 3072


def phase_inproj(P, nc, x, projT, widxT, w_inp, w_widx, mu_d, mod_d, ident_d, NB, S):
    NT = NB * S
    TG = 512
    with contextlib.ExitStack() as st:
        win = P.sb([128, 8, NPROJ], BF16, 'win', st)
        wwx = P.sb([128, 8, 8], BF16, 'wwx', st)
        ident = P.sb([128, 128], BF16, 'ident', st)
        sh1f = P.sb([128, 8, NB], F32, 'sh1f', st)
        sh1b = P.sb([128, 8, NB], BF16, 'sh1b', st)
        bias = P.sb([128, 25, NB], F32, 'bias', st)
        mu = P.sb([128, 14], F32, 'mu', st)
        G1bc = P.sb([128, D], F32, 'G1bc', st)
        epsc = P.sb([128, 1], F32, 'epsc', st)
        xt = [P.sb([128, D], F32, 'xt', st) for _ in range(2)]
        xn = [P.sb([128, D], BF16, 'xn', st) for _ in range(2)]
        junk = P.sb([128, D], BF16, 'junk', st)
        ss = [P.sb([128, 2], F32, 'ss', st) for _ in range(2)]
        hT = [P.sb([128, 8, TG], BF16, 'hT', st) for _ in range(2)]
        pb = [P.sb([128, TG + 1], BF16, 'pb', st) for _ in range(14)]
        tmpd = [P.sb([128, TG], BF16, 'tmpd', st) for _ in range(2)]
        stg = [P.sb([128, TG], BF16, 'stg', st) for _ in range(3)]
        stw = [P.sb([8, TG], F32, 'stw', st) for _ in range(2)]
        tp = [P.ps([128, 1024], BF16, 'tp', st) for _ in range(2)]
        pp = [P.ps([128, 512], F32, 'pp', st) for _ in range(3)]

        I(P, 'dve', 'memset', ap=epsc[:], constant=RMS_EPS, extra_writes=[epsc])
        P.dma('pool', ident[:], ident_d, writes=[ident], ctr=ident)
        for k in range(8):
            P.dma('pool', win[:, k, :], w_inp[k * 128:(k + 1) * 128, :], writes=[(win, k)], ctr=win)
        P.dma('pool', wwx[:], w_widx.rearrange("(k p) e -> p k e", p=128), writes=[wwx], ctr=wwx)
        P.dma('sp', mu[:], mu_d, writes=[mu], ctr=mu)
        for b_ in range(NB):
            P.dma('sp', sh1f[:, :, b_], mod_d[b_, 0, :].rearrange("(k p) -> p k", p=128), writes=[(sh1f, b_)], ctr=sh1f,
                  allow_slow_non_contiguous=True)
        I(P, 'dve', 'tensor_copy', out=sh1b[:], in_=sh1f[:])
        pbm = pp[0]
        for cg in range(25):
            for k in range(8):
                if cg < 24:
                    I(P, 'pe', 'matmul', out=pbm[:, cg * NB:(cg + 1) * NB], lhsT=win[:, k, cg * 128:(cg + 1) * 128], rhs=sh1b[:, k, :],
                      start=(k == 0), stop=(k == 7))
                else:
                    I(P, 'pe', 'matmul', out=pbm[0:8, cg * NB:(cg + 1) * NB], lhsT=wwx[:, k, :], rhs=sh1b[:, k, :],
                      start=(k == 0), stop=(k == 7))
        I(P, 'act', 'activation', out=bias[:, 0:24, :].rearrange("p c b -> p (c b)"), in_=pbm[:, 0:24 * NB], func=AF.Copy)
        I(P, 'act', 'activation', out=bias[0:8, 24, :], in_=pbm[0:8, 24 * NB:25 * NB], func=AF.Copy)

        ng = NT // TG
        cur_b = -1
        si = 0
        for g in range(ng):
            tok0 = g * TG
            b = tok0 // S
            seq_start = (tok0 % S == 0)
            if b != cur_b:
                cur_b = b
                P.dma('sp', G1bc[:], mod_d[b, 1, :].partition_broadcast(128), writes=[G1bc], ctr=G1bc)
            h = hT[g % 2]
            for j in range(4):
                r0 = tok0 + j * 128
                xx = xt[j % 2]
                s_ = ss[j % 2]
                xb = xn[j % 2]
                tpp = tp[j % 2]
                P.dma('sp', xx[:], x[r0:r0 + 128, :], writes=[xx], ctr=xx)
                I(P, 'act', 'activation', out=junk[:], in_=xx[:], func=AF.Square, accum_out=s_[:, 0:1])
                I(P, 'act', 'activation', out=s_[:, 1:2], in_=s_[:, 0:1], func=AF.Sqrt, scale=1.0 / D, bias=epsc[:, 0:1])
                I(P, 'dve', 'reciprocal', out=s_[:, 1:2], in_=s_[:, 1:2])
                I(P, 'dve', 'scalar_tensor_tensor', out=xb[:], in0=xx[:], scalar=s_[:, 1:2], in1=G1bc[:], op0=ALU.mult, op1=ALU.mult)
                for k in range(8):
                    I(P, 'pe', 'transpose', out=tpp[:, k * 128:(k + 1) * 128], in_=xb[:, k * 128:(k + 1) * 128], identity=ident[:])
                I(P, 'act', 'activation', out=h[:, :, j * 128:(j + 1) * 128], in_=tpp[:].rearrange("p (k t) -> p k t", k=8), func=AF.Copy,
                  wsub=j)
            for cg in range(24):
                pq = pp[cg % 3]
                for k in range(8):
                    I(P, 'pe', 'matmul', out=pq[:], lhsT=win[:, k, cg * 128:(cg + 1) * 128], rhs=h[:, k, :], start=(k == 0), stop=(k == 7))
                sg = stg[si % 3]
                si += 1
                if cg < 10:
                    I(P, 'act', 'activation', out=sg[:], in_=pq[:], func=AF.Identity, bias=bias[:, cg, b:b + 1])
                else:
                    gi = cg - 10
                    pbb = pb[gi]
                    td = tmpd[gi % 2]
                    if seq_start:
                        I(P, 'dve', 'memset', ap=pbb[:, 0:1], constant=0.0, extra_writes=[pbb])
                    else:
                        I(P, 'dve', 'tensor_copy', out=pbb[:, 0:1], in_=pbb[:, TG:TG + 1])
                    I(P, 'act', 'activation', out=pbb[:, 1:TG + 1], in_=pq[:], func=AF.Identity, bias=bias[:, cg, b:b + 1])
                    I(P, 'dve', 'tensor_tensor', out=td[:], in0=pbb[:, 0:TG], in1=pbb[:, 1:TG + 1], op=ALU.subtract)
                    I(P, 'dve', 'scalar_tensor_tensor', out=sg[:], in0=td[:], scalar=mu[:, gi:gi + 1], in1=pbb[:, 1:TG + 1],
                      op0=ALU.mult, op1=ALU.add)
                P.dma('sp', projT[cg * 128:(cg + 1) * 128, tok0:tok0 + TG], sg[:], reads=[sg], writes=[(projT, (cg, g))], ctr=sg)
            pq = pp[0]
            for k in range(8):
                I(P, 'pe', 'matmul', out=pq[0:8, :], lhsT=wwx[:, k, :], rhs=h[:, k, :], start=(k == 0), stop=(k == 7))
            sw = stw[g % 2]
            I(P, 'act', 'activation', out=sw[:], in_=pq[0:8, :], func=AF.Identity, bias=bias[0:8, 24, b:b + 1])
            P.dma('sp', widxT[:, tok0:tok0 + TG], sw[:], reads=[sw], writes=[(widxT, g)], ctr=sw)


def phase_outproj(P, nc, x, xout, mixedT, w_out, mod_d, NB, S):
    NT = NB * S
    with contextlib.ExitStack() as st:
        wo = P.sb([128, 8, D], BF16, 'wo', st)
        gt1bc = P.sb([128, D], F32, 'gt1bc', st)
        mt = [P.sb([128, 8, 128], BF16, 'mt', st) for _ in range(2)]
        xt = [P.sb([128, D], F32, 'xt', st) for _ in range(2)]
        tmp = [P.sb([128, 512], F32, 'tmp', st) for _ in range(2)]
        pp = [P.ps([128, 512], F32, 'pp', st) for _ in range(2)]
        for k in range(8):
            P.dma('pool', wo[:, k, :], w_out[k * 128:(k + 1) * 128, :], writes=[(wo, k)], ctr=wo)
        cur_b = -1
        for i in range(NT // 128):
            r0 = i * 128
            b = r0 // S
            if b != cur_b:
                cur_b = b
                P.dma('sp', gt1bc[:], mod_d[b, 2, :].partition_broadcast(128), writes=[gt1bc], ctr=gt1bc)
            m = mt[i % 2]
            xx = xt[i % 2]
            P.dma('sp', m[:], mixedT[:, r0:r0 + 128].rearrange("(k p) t -> p k t", p=128), writes=[m], ctr=m)
            P.dma('sp', xx[:], x[r0:r0 + 128, :], writes=[xx], ctr=xx)
            for hf in range(2):
                pq = pp[hf]
                tm = tmp[hf]
                for k in range(8):
                    I(P, 'pe', 'matmul', out=pq[:], lhsT=m[:, k, :], rhs=wo[:, k, hf * 512:(hf + 1) * 512], start=(k == 0), stop=(k == 7))
                I(P, 'dve', 'tensor_tensor', out=tm[:], in0=pq[:], in1=gt1bc[:, hf * 512:(hf + 1) * 512], op=ALU.mult)
                I(P, 'dve', 'tensor_tensor', out=xx[:, hf * 512:(hf + 1) * 512], in0=tm[:], in1=xx[:, hf * 512:(hf + 1) * 512], op=ALU.add)
            P.dma('sp', xout[r0:r0 + 128, :], xx[:], reads=[xx], writes=[(xout, i)], ctr=xx)

STOP = 99

NEG = -30000.0


def phase_dsa(P, nc, projT, widxT, mixedT, C, NB, S, TOPK):
    NTL = S // 128
    NBLK = S // 512
    with contextlib.ExitStack() as st:
        sb = lambda shp, dt, nm: P.sb(shp, dt, nm, st)
        wuk = sb([128, 512], BF16, 'wuk'); wuv = sb([128, 512], BF16, 'wuv')
        ident = sb([128, 128], BF16, 'ident'); identf = sb([128, 128], F32, 'identf')
        ones = sb([128, 128], BF16, 'ones'); sel65 = sb([65, 64], BF16, 'sel65')
        ccorr = sb([128, 8, 128], BF16, 'ccorr'); dmask = sb([128, 128], F32, 'dmask')
        gq = sb([64, 1], F32, 'gq'); gk = sb([64, 1], F32, 'gk'); gqk = sb([64, 1], F32, 'gqk'); gkv = sb([128, 1], F32, 'gkv')
        epsc = sb([128, 1], F32, 'epsc')
        qaug = sb([68, 8, S], BF16, 'qaug'); kaug = sb([68, 8, S], BF16, 'kaug')
        qidx = sb([128, 4, S], BF16, 'qidx'); kidx = sb([128, S], BF16, 'kidx')
        ckv = sb([128, S], BF16, 'ckv'); vaug = sb([128, NTL, 8, 65], BF16, 'vaug')
        MbT2 = [sb([128, NTL, 512], BF16, 'MbT') for _ in range(2)]; scs = [sb([128, S], F32, 'sc') for _ in range(4)]; junk = [sb([128, S], BF16, 'junkS')] * 2
        lo = sb([128, 4], F32, 'lo'); negt = sb([128, 4], F32, 'negt'); Sg = sb([128, 4], F32, 'Sg'); dd = sb([128, 4], F32, 'dd')
        Mb = sb([128, S], BF16, 'Mb'); mx = sb([128, 8], F32, 'mx'); thr = sb([128, 1], F32, 'thr')
        clat = sb([128, 512], BF16, 'clat'); sqc = sb([128, 512], BF16, 'sqc')
        qraw = scs[3][:].bitcast(BF16)[0:64, :].rearrange("p (h t) -> p h t", h=8)
        sqk = [sb([64, 512], BF16, 'sqk') for _ in range(2)]
        wT = sb([8, 128], F32, 'wT'); wv = sb([128, 8], F32, 'wv'); rl = [sb([128, 512], F32, 'rl') for _ in range(2)]
        rcb = rl[0]; rr = [rl[1][0:64, :], sb([64, 512], F32, 'rr')]
        Pt = [sb([128, 512], BF16, 'Pt') for _ in range(3)]
        Osb = sb([65, 512], BF16, 'Osb'); rden = sb([64, 512], F32, 'rden'); oa = [sb([64, 512], BF16, 'oa') for _ in range(2)]
        PB = [P.ps([128, 512], F32, 'PB', st) for _ in range(7)]
        psA = PB[0:3]
        psB = PB[3:5]
        I(P, 'dve', 'memset', ap=epsc[:], constant=RMS_EPS, extra_writes=[epsc])
        for t_, d_ in ((wuk, 'w_uk'), (wuv, 'w_uv'), (ident, 'ident'), (ones, 'ones128'), (sel65, 'sel65'), (ccorr, 'ccorr')):
            P.dma('pool', t_[:], C[d_], writes=[t_], ctr=t_)
        for t_, d_ in ((identf, 'ident'), (dmask, 'dmask'), (gq, 'g_q'), (gk, 'g_k'), (gkv, 'g_kv')):
            P.dma('sp', t_[:], C[d_], writes=[t_], ctr=t_)
        I(P, 'dve', 'tensor_tensor', out=gqk[:], in0=gq[:], in1=gk[:], op=ALU.mult)
        I(P, 'dve', 'tensor_scalar', out=gqk[:], in0=gqk[:], scalar1=0.125, scalar2=None, op0=ALU.mult)
        for h in range(8):
            P.dma('pool', kaug[64:68, h, :], C['kaug'], writes=[(kaug, ('a', h))], ctr=(kaug, 'a'))
            P.dma('pool', qaug[64:68, h, :], C['qaug'][h], writes=[(qaug, ('a', h))], ctr=(qaug, 'a'))
        I(P, 'dve', 'memset', ap=vaug[:, :, :, 64:65], constant=1.0, extra_writes=[(vaug, 'one')])
        for b in range(NB):
            T0 = b * S
            P.dma('sp', qidx[:], projT[512:1024, T0:T0 + S].rearrange("(m p) t -> p m t", p=128), writes=[qidx], ctr=qidx)
            P.dma('sp', kidx[:], projT[1152:1280, T0:T0 + S], writes=[kidx], ctr=kidx)
            for blk in range(NBLK):
                c0 = blk * 512
                P.dma('sp', clat[:], projT[1024:1152, T0 + c0:T0 + c0 + 512], writes=[clat], ctr=clat)
                P.dma('sp', qraw[:], projT[0:512, T0 + c0:T0 + c0 + 512].rearrange("(h p) t -> p h t", p=64), writes=[qraw], ctr=qraw)
                I(P, 'dve', 'tensor_tensor', out=sqc[:], in0=clat[:], in1=clat[:], op=ALU.mult)
                pa = psA[0]
                I(P, 'pe', 'matmul', out=pa[:], lhsT=ones[:], rhs=sqc[:], start=True, stop=True)
                I(P, 'act', 'activation', out=rcb[:], in_=pa[:], func=AF.Sqrt, scale=1.0 / 128, bias=epsc[:, 0:1])
                I(P, 'dve', 'reciprocal', out=rcb[:], in_=rcb[:])
                I(P, 'dve', 'scalar_tensor_tensor', out=ckv[:, c0:c0 + 512], in0=clat[:], scalar=gkv[:, 0:1], in1=rcb[:],
                  op0=ALU.mult, op1=ALU.mult, wsub=blk)
                for j in range(4):
                    ti = blk * 4 + j
                    pv = psB[j % 2]
                    I(P, 'pe', 'matmul', out=pv[:], lhsT=ckv[:, ti * 128:(ti + 1) * 128], rhs=wuv[:], start=True, stop=True)
                    I(P, 'act', 'activation', out=vaug[:, ti, :, 0:64], in_=pv[:].rearrange("p (h d) -> p h d", h=8), func=AF.Copy,
                      wsub=('v', ti))
                for h in range(8):
                    pk = psA[1 + h % 2]; p2 = psB[h % 2]; sq = sqk[h % 2]; r_ = rr[h % 2]
                    I(P, 'pe', 'matmul', out=pk[0:64, :], lhsT=wuk[:, h * 64:(h + 1) * 64], rhs=ckv[:, c0:c0 + 512], start=True, stop=True)
                    I(P, 'act', 'activation', out=sq[:], in_=pk[0:64, :], func=AF.Square)
                    I(P, 'pe', 'matmul', out=p2[0:64, :], lhsT=ones[0:64, 0:64], rhs=sq[:], start=True, stop=True)
                    I(P, 'act', 'activation', out=r_[:], in_=p2[0:64, :], func=AF.Sqrt, scale=1.0 / 64, bias=epsc[0:64, 0:1])
                    I(P, 'dve', 'reciprocal', out=r_[:], in_=r_[:])
                    I(P, 'dve', 'tensor_tensor', out=kaug[0:64, h, c0:c0 + 512], in0=pk[0:64, :], in1=r_[:], op=ALU.mult, wsub=('k', h, blk))
                    p3 = psB[h % 2]
                    I(P, 'act', 'activation', out=sq[:], in_=qraw[:, h, :], func=AF.Square)
                    I(P, 'pe', 'matmul', out=p3[0:64, :], lhsT=ones[0:64, 0:64], rhs=sq[:], start=True, stop=True)
                    I(P, 'act', 'activation', out=r_[:], in_=p3[0:64, :], func=AF.Sqrt, scale=1.0 / 64, bias=epsc[0:64, 0:1])
                    I(P, 'dve', 'reciprocal', out=r_[:], in_=r_[:])
                    I(P, 'dve', 'scalar_tensor_tensor', out=qaug[0:64, h, c0:c0 + 512], in0=qraw[:, h, :], scalar=gqk[:, 0:1], in1=r_[:],
                      op0=ALU.mult, op1=ALU.mult, wsub=('q', h, blk))
            if STOP <= 1:
                continue
            if STOP <= 1:
                continue
            def prep(q, T0=T0):
                MbT = MbT2[q % 2]
                I(P, 'dve', 'memset', ap=MbT[:], constant=NEG, extra_writes=[MbT])
                RNG = 1024.0
                NIT = 26
                for ti in range(4):
                    i = q * 4 + ti
                    L = 128 * (i + 1)
                    sc = scs[ti]
                    P.dma('sp', wT[:], widxT[:, T0 + i * 128:T0 + (i + 1) * 128], writes=[wT], ctr=wT)
                    pw = PB[4]
                    I(P, 'pe', 'transpose', out=pw[:, 0:8], in_=wT[:], identity=identf[0:8, 0:8])
                    I(P, 'act', 'activation', out=wv[:], in_=pw[:, 0:8], func=AF.Copy)
                    nkb = (L + 511) // 512
                    for kb in range(nkb):
                        cols = min(512, L - kb * 512)
                        for h in range(8):
                            base = (h % 2) * 64; m = h // 2
                            pi = PB[2 + h % 2]; r_ = rl[h % 2]
                            I(P, 'pe', 'matmul', out=pi[:, 0:cols], lhsT=qidx[base:base + 64, m, i * 128:(i + 1) * 128],
                              rhs=kidx[base:base + 64, kb * 512:kb * 512 + cols], start=True, stop=True)
                            I(P, 'act', 'activation', out=r_[:, 0:cols], in_=pi[:, 0:cols], func=AF.Relu)
                            if h == 0:
                                I(P, 'dve', 'tensor_scalar', out=sc[:, kb * 512:kb * 512 + cols], in0=r_[:, 0:cols], scalar1=wv[:, 0:1],
                                  scalar2=None, op0=ALU.mult)
                            else:
                                I(P, 'dve', 'scalar_tensor_tensor', out=sc[:, kb * 512:kb * 512 + cols], in0=r_[:, 0:cols],
                                  scalar=wv[:, h:h + 1], in1=sc[:, kb * 512:kb * 512 + cols], op0=ALU.mult, op1=ALU.add)
                    I(P, 'dve', 'tensor_tensor', out=sc[:, L - 128:L], in0=sc[:, L - 128:L], in1=dmask[:], op=ALU.add)
                    yield 1
                act_t = [ti for ti in range(4) if 128 * (q * 4 + ti + 1) > TOPK]
                if act_t:
                    I(P, 'dve', 'memset', ap=lo[:], constant=-RNG, extra_writes=[lo])
                    for k in range(NIT):
                        s_k = RNG / (2.0 ** k)
                        for ti in act_t:
                            L = 128 * (q * 4 + ti + 1)
                            I(P, 'dve', 'tensor_scalar', out=negt[:, ti:ti + 1], in0=lo[:, ti:ti + 1], scalar1=-1.0, scalar2=-s_k,
                              op0=ALU.mult, op1=ALU.add, wsub=ti, rsub=ti)
                            I(P, 'act', 'activation', out=junk[ti % 2][:, 0:L], in_=scs[ti][:, 0:L], func=AF.Sign, bias=negt[:, ti:ti + 1],
                              accum_out=Sg[:, ti:ti + 1], wsub={Sg.name: ti}, rsub=ti)
                        if k % 3 == 2:
                            yield 1
                        for ti in act_t:
                            L = 128 * (q * 4 + ti + 1)
                            I(P, 'dve', 'tensor_scalar', out=dd[:, ti:ti + 1], in0=Sg[:, ti:ti + 1], scalar1=float(2 * TOPK - L), scalar2=None,
                              op0=ALU.is_ge, wsub=ti, rsub=ti)
                            I(P, 'dve', 'scalar_tensor_tensor', out=lo[:, ti:ti + 1], in0=dd[:, ti:ti + 1], scalar=s_k, in1=lo[:, ti:ti + 1],
                              op0=ALU.mult, op1=ALU.add, wsub=ti, rsub=ti)
                for ti in range(4):
                    i = q * 4 + ti
                    L = 128 * (i + 1)
                    sc = scs[ti]
                    if L > TOPK:
                        I(P, 'dve', 'tensor_scalar', out=Mb[:, 0:L], in0=sc[:, 0:L], scalar1=lo[:, ti:ti + 1], scalar2=NEG, op0=ALU.is_le, op1=ALU.mult)
                    else:
                        I(P, 'dve', 'tensor_scalar', out=Mb[:, 0:L], in0=sc[:, 0:L], scalar1=-1e29, scalar2=NEG, op0=ALU.is_lt, op1=ALU.mult)
                    for j0 in range(0, i + 1, 8):
                        j1 = min(i + 1, j0 + 8)
                        pt = PB[6]
                        ptb = pt[:].bitcast(BF16)
                        for j in range(j0, j1):
                            I(P, 'pe', 'transpose', out=ptb[:, (j - j0) * 128:(j - j0 + 1) * 128], in_=Mb[:, j * 128:(j + 1) * 128], identity=ident[:])
                        I(P, 'act', 'activation', out=MbT[:, j0:j1, ti * 128:(ti + 1) * 128],
                          in_=ptb[:, 0:(j1 - j0) * 128].rearrange("p (j t) -> p j t", t=128), func=AF.Copy, wsub=('t', i, j0))
                    yield 1

            def attn(q, T0=T0, b=b):
                MbT = MbT2[q % 2]
                nsb = 4 * q + 4
                for h in range(8):
                    po = PB[5]
                    for j in range(nsb):
                        pl = PB[j % 2]; pt_ = Pt[j % 3]
                        I(P, 'pe', 'matmul', out=pl[:], lhsT=kaug[0:68, h, j * 128:(j + 1) * 128], rhs=qaug[0:68, h, q * 512:(q + 1) * 512],
                          start=True, stop=False)
                        diag = j >= 4 * q
                        I(P, 'pe', 'matmul', out=pl[:], lhsT=ident[:], rhs=MbT[:, j, :], start=False, stop=not diag)
                        if diag:
                            dj = j - 4 * q
                            I(P, 'pe', 'matmul', out=pl[:, dj * 128:(dj + 1) * 128], lhsT=ident[:], rhs=ccorr[:, h, :], start=False, stop=True)
                        I(P, 'act', 'activation', out=pt_[:], in_=pl[:], func=AF.Exp)
                        I(P, 'pe', 'matmul', out=po[0:65, :], lhsT=vaug[:, j, h, :], rhs=pt_[:], start=(j == 0), stop=(j == nsb - 1))
                        if j % 4 == 3 and j != nsb - 1:
                            yield 1
                    I(P, 'act', 'activation', out=Osb[:], in_=po[0:65, :], func=AF.Copy)
                    pd = PB[4]
                    I(P, 'pe', 'matmul', out=pd[0:64, :], lhsT=sel65[:], rhs=Osb[:], start=True, stop=True)
                    I(P, 'dve', 'reciprocal', out=rden[:], in_=pd[0:64, :])
                    o_ = oa[h % 2]
                    I(P, 'dve', 'tensor_tensor', out=o_[:], in0=Osb[0:64, :], in1=rden[:], op=ALU.mult)
                    P.dma('sp', mixedT[h * 64:(h + 1) * 64, T0 + q * 512:T0 + (q + 1) * 512], o_[:], reads=[o_], writes=[(mixedT, (h, b, q))], ctr=o_)
                    yield 1

            for _ in prep(0):
                pass
            for q in range(NBLK):
                ga = attn(q)
                gp = prep(q + 1) if q + 1 < NBLK else iter(())
                da = dp = False
                while not (da and dp):
                    if not dp and next(gp, None) is None:
                        dp = True
                    if not da and next(ga, None) is None:
                        da = True

RSTOP = 99

GN_EPS = 64e-5


def phase_rwkv(P, nc, projT, mixedT, C, NB, S):
    TGR = 256
    NCH = 4
    NU = NCH * 8
    c0 = -float(np.exp(-0.5))
    with contextlib.ExitStack() as st:
        sb = lambda shp, dt, nm: P.sb(shp, dt, nm, st)
        w2b = sb([64, 512], BF16, 'w2b'); a2b = sb([64, 512], BF16, 'a2b'); g2b = sb([128, 512], BF16, 'g2b')
        vec = sb([64, 5, 8], F32, 'vec'); omk = sb([64, 8], F32, 'omk')
        lnw = sb([64, 512], F32, 'lnw'); lnb = sb([64, 512], F32, 'lnb'); rkr = sb([64, 512], F32, 'rkr')
        ones = sb([128, 128], BF16, 'onesr'); ident = sb([128, 128], BF16, 'identr')
        mask1 = sb([64, 128], F32, 'mask1'); mask3 = sb([64, 64], F32, 'mask3'); rst = sb([64, TGR], F32, 'rst')
        gneps = sb([64, 1], F32, 'gneps')
        F3 = [64, 8, TGR]
        rT = sb(F3, BF16, 'rT'); kT = sb(F3, BF16, 'kT'); vT = sb(F3, BF16, 'vT')
        xw = sb([64, TGR], BF16, 'xw'); xa = sb([64, TGR], BF16, 'xa'); xg = sb([128, TGR], BF16, 'xg')
        thb = sb([64, TGR], BF16, 'thb'); sgx = sb([128, TGR], BF16, 'sgx')
        sg = sb(F3, F32, 'sg'); cs = sb(F3, F32, 'cs'); Ea = sb(F3, F32, 'Ea'); Eb = sb(F3, F32, 'Eb')
        a_ = sb(F3, BF16, 'a_'); kkr = sb(F3, F32, 'kkr'); sq = sb(F3, BF16, 'sq')
        kk = sb(F3, F32, 'kk'); t1 = sb(F3, F32, 't1'); nrm = t1
        kmod = sb(F3, F32, 'kmod'); beta = kkr; gT = sb(F3, BF16, 'gT')
        bp = sb(F3, BF16, 'bp'); kp = sb(F3, BF16, 'kp'); kmb = sq
        AR = sb([64, 8, NCH, 2, 64], BF16, 'AR'); BK = sb([64, 8, NCH, 2, 64], BF16, 'BK'); GCt = sb([64, 8, NCH], F32, 'GCt')
        T3 = [64, NCH, 512]
        bpTM = sb(T3, BF16, 'bpTM'); kpTM = sb(T3, BF16, 'kpTM'); vTM = sb(T3, BF16, 'vTM')
        rTM = sb(T3, BF16, 'rTM'); kmTM = sb(T3, BF16, 'kmTM'); gTM = sb(T3, BF16, 'gTM')
        NM1 = [sb([64, 8, 128], BF16, 'NM1') for _ in range(NCH)]; NM2 = [sb([64, 8, 128], BF16, 'NM2') for _ in range(NCH)]
        NTt = [sb([64, 8, 64], BF16, 'NTt') for _ in range(NCH)]; X = [sb([64, 8, 64], BF16, 'X') for _ in range(NCH)]
        Pm = [[sb([64, 8, 64], BF16, 'Pm')] * 2 for _ in range(NCH)]; PTm = [[sb([64, 8, 64], BF16, 'PTm')] * 2 for _ in range(NCH)]
        Ysb = sb([64, 512], BF16, 'Ysb'); Usb = sb([64, 512], BF16, 'Usb')
        hst = sb([64, 8, 64], F32, 'hst'); hs = sb([64, 8, 64], F32, 'hs'); hbf = sb([64, 8, 64], BF16, 'hbf')
        OTM = sb(T3, F32, 'OTM')
        s1 = sb([64, NU], F32, 's1'); s2 = sb([64, NU], F32, 's2'); bs = sb([64, NU], F32, 'bs')
        ob = sb(T3, BF16, 'ob'); obT = [sb(F3, BF16, 'obT')] * 2
        PS = [P.ps([128, 512], F32, 'PSr', st) for _ in range(7)]
        cnt = [0]

        def nxt():
            cnt[0] += 1
            return PS[cnt[0] % 7]

        for t_, d_ in ((w2b, 'w2'), (a2b, 'a2'), (g2b, 'g2'), (ones, 'ones128'), (ident, 'ident')):
            P.dma('pool', t_[:], C[d_], writes=[t_], ctr=t_)
        for t_, d_ in ((vec, 'vec8'), (mask1, 'mask1'), (mask3, 'mask3')):
            P.dma('sp', t_[:], C[d_], writes=[t_], ctr=t_)
        P.dma('sp', rst[:], C['rst'][0:64, :], writes=[rst], ctr=rst)
        for t_, d_ in ((lnw, 'ln_w'), (lnb, 'ln_b'), (rkr, 'r_k1')):
            P.dma('sp', t_[:], C[d_].partition_broadcast(64), writes=[t_], ctr=t_)
        I(P, 'dve', 'memset', ap=gneps[:], constant=GN_EPS, extra_writes=[gneps])
        I(P, 'dve', 'tensor_scalar', out=omk[:], in0=vec[:, 3, :], scalar1=-1.0, scalar2=1.0, op0=ALU.mult, op1=ALU.add)

        def v3(t, h):
            return t[:, h, :].rearrange("p (n c) -> p n c", c=64)

        def fm_load(t_, r0, tok):
            P.dma('sp', t_[:], projT[r0:r0 + 512, tok].rearrange("(h p) t -> p h t", p=64), writes=[t_], ctr=t_)

        gi = 0
        for b in range(NB):
            I(P, 'dve', 'memset', ap=hst[:], constant=0.0, extra_writes=[hst])
            I(P, 'dve', 'memset', ap=hbf[:], constant=0.0, extra_writes=[hbf])
            for g in range(S // TGR):
                tok = b * S + g * TGR
                sl = slice(tok, tok + TGR)
                fm_load(rT, 1280, sl); fm_load(kT, 1792, sl); fm_load(vT, 2304, sl)
                P.dma('sp', xw[:], projT[2816:2880, sl], writes=[xw], ctr=xw)
                P.dma('sp', xa[:], projT[2880:2944, sl], writes=[xa], ctr=xa)
                P.dma('sp', xg[:], projT[2944:3072, sl], writes=[xg], ctr=xg)
                I(P, 'act', 'activation', out=thb[:], in_=xw[:], func=AF.Tanh)
                I(P, 'act', 'activation', out=sgx[:], in_=xg[:], func=AF.Sigmoid)
                for hp in range(4):
                    p1, p2, p3 = nxt(), nxt(), nxt()
                    for e2 in range(2):
                        h = hp * 2 + e2
                        hc = slice(h * 64, (h + 1) * 64)
                        oc = slice(e2 * TGR, (e2 + 1) * TGR)
                        I(P, 'pe', 'matmul', out=p1[0:64, oc], lhsT=w2b[:, hc], rhs=thb[:], start=True, stop=True)
                        I(P, 'pe', 'matmul', out=p2[0:64, oc], lhsT=a2b[:, hc], rhs=xa[:], start=True, stop=True)
                        I(P, 'pe', 'matmul', out=p3[0:64, oc], lhsT=g2b[:, hc], rhs=sgx[:], start=True, stop=True)
                    for e2 in range(2):
                        h = hp * 2 + e2
                        oc = slice(e2 * TGR, (e2 + 1) * TGR)
                        I(P, 'act', 'activation', out=sg[:, h, :], in_=p1[0:64, oc], func=AF.Sigmoid, bias=vec[:, 0, h:h + 1], wsub=h)
                        I(P, 'act', 'activation', out=a_[:, h, :], in_=p2[0:64, oc], func=AF.Sigmoid, bias=vec[:, 1, h:h + 1], wsub=h)
                    I(P, 'act', 'activation', out=gT[:, hp * 2:hp * 2 + 2, :], in_=p3[0:64, :].rearrange("p (e t) -> p e t", e=2), func=AF.Copy, wsub=hp)
                for h in range(8):
                    I(P, 'dve', 'tensor_scalar', out=kkr[:, h, :], in0=kT[:, h, :], scalar1=vec[:, 2, h:h + 1], scalar2=None, op0=ALU.mult, wsub=h)
                I(P, 'dve', 'tensor_tensor', out=sq[:], in0=kkr[:], in1=kkr[:], op=ALU.mult)
                for hp in range(4):
                    p1 = nxt()
                    for e2 in range(2):
                        h = hp * 2 + e2
                        I(P, 'pe', 'matmul', out=p1[0:64, e2 * TGR:(e2 + 1) * TGR], lhsT=ones[0:64, 0:64], rhs=sq[:, h, :], start=True, stop=True)
                    I(P, 'act', 'activation', out=nrm[:, hp * 2:hp * 2 + 2, :], in_=p1[0:64, :].rearrange("p (e t) -> p e t", e=2), func=AF.Sqrt, wsub=hp)
                I(P, 'dve', 'tensor_scalar', out=nrm[:], in0=nrm[:], scalar1=1e-12, scalar2=None, op0=ALU.max)
                I(P, 'dve', 'reciprocal', out=nrm[:], in_=nrm[:])
                I(P, 'dve', 'tensor_tensor', out=kk[:], in0=kkr[:], in1=nrm[:], op=ALU.mult)
                for h in range(8):
                    I(P, 'dve', 'tensor_scalar', out=t1[:, h, :], in0=a_[:, h, :], scalar1=vec[:, 3, h:h + 1], scalar2=omk[:, h:h + 1],
                      op0=ALU.mult, op1=ALU.add, wsub=h)
                I(P, 'dve', 'tensor_tensor', out=kmod[:], in0=kT[:], in1=t1[:], op=ALU.mult)
                I(P, 'dve', 'tensor_copy', out=kmb[:], in_=kmod[:])
                I(P, 'dve', 'tensor_tensor', out=beta[:], in0=kk[:], in1=a_[:], op=ALU.mult)
                for h in range(8):
                    I(P, 'dve', 'tensor_tensor_scan', out=cs[:, h, :], data0=rst[:], data1=sg[:, h, :], initial=0.0, op0=ALU.mult, op1=ALU.add,
                      wsub=h)
                I(P, 'act', 'activation', out=Ea[:], in_=cs[:], func=AF.Exp, scale=c0)
                for h in range(8):
                    I(P, 'dve', 'tensor_tensor', out=AR[:, h, :, 1, :], in0=v3(rT, h), in1=v3(Ea, h), op=ALU.mult, wsub=('r', h))
                I(P, 'dve', 'tensor_copy', out=GCt[:], in_=Ea[:].rearrange("p h (n c) -> p h n c", c=64)[:, :, :, 63])
                I(P, 'act', 'activation', out=Eb[:], in_=cs[:], func=AF.Exp, scale=-c0)
                for h in range(8):
                    I(P, 'dve', 'tensor_tensor', out=BK[:, h, :, 0, :], in0=v3(beta, h), in1=v3(Eb, h), op=ALU.mult, wsub=('b', h))
                    I(P, 'dve', 'tensor_tensor', out=BK[:, h, :, 1, :], in0=v3(kmod, h), in1=v3(Eb, h), op=ALU.mult, wsub=('k', h))
                I(P, 'dve', 'tensor_tensor', out=Ea[:], in0=cs[:], in1=sg[:], op=ALU.subtract)
                I(P, 'act', 'activation', out=Ea[:], in_=Ea[:], func=AF.Exp, scale=c0)
                for h in range(8):
                    I(P, 'dve', 'scalar_tensor_tensor', out=AR[:, h, :, 0, :], in0=v3(kk, h), scalar=-1.0, in1=v3(Ea, h), op0=ALU.mult, op1=ALU.mult,
                      wsub=('a', h))
                csv = cs[:].rearrange("p h (n c) -> p (h n) c", c=64)
                I(P, 'dve', 'tensor_tensor', out=Eb[:].rearrange("p h (n c) -> p (h n) c", c=64), in0=csv[:, :, 63:64].to_broadcast([64, 8 * NCH, 64]),
                  in1=csv, op=ALU.subtract)
                I(P, 'act', 'activation', out=Eb[:], in_=Eb[:], func=AF.Exp, scale=c0)
                I(P, 'dve', 'tensor_tensor', out=bp[:], in0=beta[:], in1=Eb[:], op=ALU.mult)
                I(P, 'dve', 'tensor_tensor', out=kp[:], in0=kmod[:], in1=Eb[:], op=ALU.mult)
                if RSTOP <= 1:
                    continue
                tcnt = 0
                for Xf, Xt in ((bp, bpTM), (kp, kpTM), (vT, vTM), (rT, rTM), (kmb, kmTM), (gT, gTM)):
                    for n in range(NCH):
                        ps = nxt()
                        pb_ = ps[:].bitcast(BF16)
                        for h in range(8):
                            I(P, 'pe', 'transpose', out=pb_[0:64, h * 64:(h + 1) * 64], in_=Xf[:, h, n * 64:(n + 1) * 64], identity=ident[0:64, 0:64])
                        tcnt += 1
                        if tcnt % 2:
                            I(P, 'act', 'activation', out=Xt[:, n, :], in_=pb_[0:64, 0:512], func=AF.Copy, wsub=n)
                        else:
                            I(P, 'dve', 'tensor_copy', out=Xt[:, n, :], in_=pb_[0:64, 0:512], wsub=n)
                if RSTOP <= 2:
                    continue
                Pcs = {}
                PTcs = {}
                for n in range(NCH):
                    nm1 = NM1[n]; nm2 = NM2[n]; ntt = NTt[n]; x_ = X[n]
                    for hq in range(2):
                        pa, pb2, pc = nxt(), nxt(), nxt()
                        for hh in range(4):
                            h = hq * 4 + hh
                            arv = AR[:, h, n, :, :].rearrange("p a c -> p (a c)")
                            I(P, 'pe', 'matmul', out=pa[0:64, hh * 128:(hh + 1) * 128], lhsT=BK[:, h, n, 0, :], rhs=arv, start=True, stop=True)
                            I(P, 'pe', 'matmul', out=pb2[0:64, hh * 128:(hh + 1) * 128], lhsT=BK[:, h, n, 1, :], rhs=arv, start=True, stop=True)
                            I(P, 'pe', 'matmul', out=pc[0:64, hh * 64:(hh + 1) * 64], lhsT=AR[:, h, n, 0, :], rhs=BK[:, h, n, 0, :], start=True, stop=True)
                        m1b = mask1[:].unsqueeze(1).to_broadcast([64, 4, 128])
                        I(P, 'dve', 'tensor_tensor', out=nm1[:, hq * 4:(hq + 1) * 4, :], in0=pa[0:64, :].rearrange("p (h c) -> p h c", c=128), in1=m1b,
                          op=ALU.mult, wsub=hq)
                        I(P, 'dve', 'tensor_tensor', out=nm2[:, hq * 4:(hq + 1) * 4, :], in0=pb2[0:64, :].rearrange("p (h c) -> p h c", c=128), in1=m1b,
                          op=ALU.mult, wsub=hq)
                        I(P, 'dve', 'tensor_tensor', out=ntt[:, hq * 4:(hq + 1) * 4, :], in0=pc[0:64, 0:256].rearrange("p (h c) -> p h c", c=64),
                          in1=mask3[:].unsqueeze(1).to_broadcast([64, 4, 64]), op=ALU.mult, wsub=hq)
                    I(P, 'dve', 'tensor_tensor', out=x_[:], in0=nm1[:, :, 0:64], in1=ident[0:64, 0:64].unsqueeze(1).to_broadcast([64, 8, 64]), op=ALU.add)
                    Pcs[n] = (lambda u, nm1=nm1: nm1[:, u, 0:64])
                    PTcs[n] = (lambda u, ntt=ntt: ntt[:, u, :])
                for k in range(6):
                    for n in range(NCH):
                        x_ = X[n]; Pc = Pcs[n]; PTc = PTcs[n]
                        if k >= 1:
                            pX = nxt()
                            for u in range(8):
                                I(P, 'pe', 'matmul', out=pX[0:64, u * 64:(u + 1) * 64], lhsT=PTc(u), rhs=x_[:, u, :], start=True, stop=True)
                        if k < 5:
                            pP, pPT = nxt(), nxt()
                            for u in range(8):
                                I(P, 'pe', 'matmul', out=pP[0:64, u * 64:(u + 1) * 64], lhsT=PTc(u), rhs=Pc(u), start=True, stop=True)
                                I(P, 'pe', 'matmul', out=pPT[0:64, u * 64:(u + 1) * 64], lhsT=Pc(u), rhs=PTc(u), start=True, stop=True)
                        if k >= 1:
                            I(P, 'dve', 'tensor_tensor', out=x_[:], in0=x_[:], in1=pX[0:64, :].rearrange("p (u c) -> p u c", c=64), op=ALU.add)
                        if k < 5:
                            pn = Pm[n][k % 2]; ptn = PTm[n][k % 2]
                            I(P, 'act', 'activation', out=pn[:], in_=pP[0:64, :].rearrange("p (u c) -> p u c", c=64), func=AF.Copy)
                            I(P, 'dve', 'tensor_copy', out=ptn[:], in_=pPT[0:64, :].rearrange("p (u c) -> p u c", c=64))
                            Pcs[n] = (lambda u, pn=pn: pn[:, u, :])
                            PTcs[n] = (lambda u, ptn=ptn: ptn[:, u, :])
                for n in range(NCH):
                    nm1 = NM1[n]; nm2 = NM2[n]; x_ = X[n]
                    if RSTOP <= 3:
                        continue
                    pY = nxt()
                    for h in range(8):
                        hc = slice(h * 64, (h + 1) * 64)
                        I(P, 'pe', 'matmul', out=pY[0:64, hc], lhsT=AR[:, h, n, 0, :], rhs=hbf[:, h, :], start=True, stop=False)
                        I(P, 'pe', 'matmul', out=pY[0:64, hc], lhsT=nm2[:, h, 0:64], rhs=vTM[:, n, hc], start=False, stop=True)
                    I(P, 'act', 'activation', out=Ysb[:], in_=pY[0:64, :], func=AF.Copy)
                    pU = nxt()
                    for h in range(8):
                        hc = slice(h * 64, (h + 1) * 64)
                        I(P, 'pe', 'matmul', out=pU[0:64, hc], lhsT=x_[:, h, :], rhs=Ysb[:, hc], start=True, stop=True)
                    I(P, 'dve', 'tensor_copy', out=Usb[:], in_=pU[0:64, :])
                    pO = nxt()
                    for h in range(8):
                        hc = slice(h * 64, (h + 1) * 64)
                        I(P, 'pe', 'matmul', out=pO[0:64, hc], lhsT=AR[:, h, n, 1, :], rhs=hbf[:, h, :], start=True, stop=False)
                        I(P, 'pe', 'matmul', out=pO[0:64, hc], lhsT=nm1[:, h, 64:128], rhs=Usb[:, hc], start=False, stop=False)
                        I(P, 'pe', 'matmul', out=pO[0:64, hc], lhsT=nm2[:, h, 64:128], rhs=vTM[:, n, hc], start=False, stop=True)
                    I(P, 'act', 'activation', out=OTM[:, n, :], in_=pO[0:64, :], func=AF.Copy, wsub=n)
                    pH = nxt()
                    for h in range(8):
                        hc = slice(h * 64, (h + 1) * 64)
                        I(P, 'pe', 'matmul', out=pH[0:64, hc], lhsT=bpTM[:, n, hc], rhs=Usb[:, hc], start=True, stop=False)
                        I(P, 'pe', 'matmul', out=pH[0:64, hc], lhsT=kpTM[:, n, hc], rhs=vTM[:, n, hc], start=False, stop=True)
                    I(P, 'dve', 'tensor_tensor', out=hs[:], in0=hst[:], in1=GCt[:, :, n:n + 1].to_broadcast([64, 8, 64]), op=ALU.mult)
                    I(P, 'dve', 'tensor_tensor', out=hst[:], in0=hs[:], in1=pH[0:64, :].rearrange("p (h c) -> p h c", c=64), op=ALU.add)
                    I(P, 'dve', 'tensor_copy', out=hbf[:], in_=hst[:])
                if RSTOP <= 4:
                    continue
                O3 = OTM[:].rearrange("p n (h d) -> p (n h) d", d=64)
                OcA = Ea[:].rearrange("p h t -> p (h t)").rearrange("p (n c) -> p n c", c=512)
                sqA = Eb[:].rearrange("p h t -> p (h t)").rearrange("p (n c) -> p n c", c=512)
                Oc3 = OcA.rearrange("p n (h d) -> p (n h) d", d=64)
                sq3 = sqA.rearrange("p n (h d) -> p (n h) d", d=64)
                bc = lambda t: t[:, :].unsqueeze(2).to_broadcast([64, NU, 64])
                I(P, 'dve', 'tensor_reduce', out=s1[:], in_=O3, axis=AX.X, op=ALU.add)
                I(P, 'dve', 'tensor_scalar', out=s1[:], in0=s1[:], scalar1=1.0 / 64, scalar2=None, op0=ALU.mult)
                I(P, 'dve', 'tensor_tensor', out=Oc3, in0=O3, in1=bc(s1), op=ALU.subtract)
                I(P, 'dve', 'tensor_tensor', out=sqA, in0=OcA, in1=OcA, op=ALU.mult)
                I(P, 'dve', 'tensor_reduce', out=s2[:], in_=sq3, axis=AX.X, op=ALU.add)
                I(P, 'act', 'activation', out=s2[:], in_=s2[:], func=AF.Sqrt, scale=1.0 / 64, bias=gneps[:, 0:1])
                I(P, 'dve', 'reciprocal', out=s2[:], in_=s2[:])
                I(P, 'dve', 'tensor_tensor', out=Oc3, in0=Oc3, in1=bc(s2), op=ALU.mult)
                I(P, 'dve', 'tensor_tensor', out=OcA, in0=OcA, in1=lnw[:].unsqueeze(1).to_broadcast([64, NCH, 512]), op=ALU.mult)
                I(P, 'dve', 'tensor_tensor', out=OcA, in0=OcA, in1=lnb[:].unsqueeze(1).to_broadcast([64, NCH, 512]), op=ALU.add)
                I(P, 'dve', 'tensor_tensor', out=sqA, in0=rTM[:], in1=kmTM[:], op=ALU.mult)
                I(P, 'dve', 'tensor_tensor', out=sqA, in0=sqA, in1=rkr[:].unsqueeze(1).to_broadcast([64, NCH, 512]), op=ALU.mult)
                I(P, 'dve', 'tensor_reduce', out=bs[:], in_=sq3, axis=AX.X, op=ALU.add)
                I(P, 'dve', 'tensor_tensor', out=sq3, in0=vTM[:].rearrange("p n (h d) -> p (n h) d", d=64), in1=bc(bs), op=ALU.mult)
                I(P, 'dve', 'tensor_tensor', out=OcA, in0=OcA, in1=sqA, op=ALU.add)
                I(P, 'dve', 'tensor_tensor', out=ob[:], in0=OcA, in1=gTM[:], op=ALU.mult)
                oT = obT[g % 2]
                for n in range(NCH):
                    ps = nxt()
                    pb_ = ps[:].bitcast(BF16)
                    for h in range(8):
                        I(P, 'pe', 'transpose', out=pb_[0:64, h * 64:(h + 1) * 64], in_=ob[:, n, h * 64:(h + 1) * 64], identity=ident[0:64, 0:64])
                    I(P, 'act', 'activation', out=oT[:, :, n * 64:(n + 1) * 64], in_=pb_[0:64, 0:512].rearrange("p (h c) -> p h c", c=64), func=AF.Copy, wsub=n)
                P.dma('sp', mixedT[512:1024, sl].rearrange("(h p) t -> p h t", p=64), oT[:], reads=[oT], writes=[(mixedT, ('ob', b, g))], ctr=oT)


A_W, KVL, IH, ID_ = 512, 128, 8, 64
N_IN_A = 1224


def perm_cols():
    q = list(range(0, 512))
    clat = list(range(512, 640))
    qidx = list(range(640, 1152))
    kidx = list(range(1152, 1216))
    B0 = N_IN_A
    r = list(range(B0, B0 + 512))
    k = list(range(B0 + 512, B0 + 1024))
    v = list(range(B0 + 1024, B0 + 1536))
    xwxa = list(range(B0 + 1536, B0 + 1664))
    xg = list(range(B0 + 1664, B0 + 1792))
    return q + qidx + clat + kidx + kidx + r + k + v + xwxa + xg


def host_consts(S):
    f = np.float32
    t = np.arange(S)
    slopes = 2.0 ** (-np.arange(1, 9, dtype=np.float64))
    kaug = np.stack([np.ones(S), np.ones(S), 64.0 * (t // 64), (t % 64) * 1.0]).astype(f)
    qaug = np.stack([np.stack([-s * 64.0 * (t // 64), -s * (t % 64), s * np.ones(S), s * np.ones(S)]) for s in slopes]).astype(f)
    ss, tt = np.meshgrid(np.arange(128), np.arange(128), indexing='ij')
    same = (ss // 64) == (tt // 64)
    ccorr = np.stack([np.where(same, -2.0 * s * np.maximum(ss - tt, 0), 0.0) for s in slopes], axis=1).astype(f)
    tq, sk = np.meshgrid(np.arange(128), np.arange(128), indexing='ij')
    dmask = np.where(sk < (tq // 64 + 1) * 64, 0.0, -1e30).astype(f)
    sel65 = np.zeros((65, 64), f)
    sel65[64, :] = 1.0
    return dict(kaug=kaug, qaug=qaug, ccorr=ccorr, dmask=dmask, sel65=sel65,
                ident=np.eye(128, dtype=f), ones128=np.ones((128, 128), f))


def rwkv_consts():
    f = np.float32
    i = np.arange(128)
    bones = ((i[:, None] // 64) == (i[None, :] // 64)).astype(f)
    s_, t_ = np.meshgrid(np.arange(64), np.arange(64), indexing='ij')
    mask1 = np.concatenate([(s_ < t_), (s_ <= t_)], axis=1).astype(f)
    mask3 = (t_ < s_).astype(f)
    rst = np.ones((128, 256), f)
    rst[:, 0::64] = 0.0
    return dict(bones=bones, mask1=mask1, mask3=mask3, rst=rst)


def rwkv_params(w0, w2, a0, a2, g2, k_k, k_a, r_k, ln_w, ln_b):
    f = np.float32
    col = lambda v: np.asarray(v, f).reshape(8, 64).T
    vec8 = np.ascontiguousarray(np.stack([col(w0), col(a0), col(k_k), col(k_a), col(np.asarray(r_k, f).reshape(512))], axis=1))
    return dict(w2=np.ascontiguousarray(np.asarray(w2, f)), a2=np.ascontiguousarray(np.asarray(a2, f)), g2=np.ascontiguousarray(np.asarray(g2, f)),
                vec8=vec8, ln_w=np.ascontiguousarray(np.asarray(ln_w, f).reshape(512)), ln_b=np.ascontiguousarray(np.asarray(ln_b, f).reshape(512)),
                r_k1=np.ascontiguousarray(np.asarray(r_k, f).reshape(512)))


NCORES = 8
NB_ = 4
S_ = 2048


def build_nc(shapes):
    NB, S = NB_, S_
    NT = NB * S
    nc = bass.Bass("TRN2", target_bir_lowering=False)
    A = {k: nc.dram_tensor(k, list(shp), F32, kind="ExternalInput").ap() for k, shp in shapes.items()}
    out = nc.dram_tensor("out", [NT, D], F32, kind="ExternalOutput").ap()
    mod_d = nc.dram_tensor("mod_d", [NB, 6, D], F32, kind="Internal").ap()
    projT = nc.dram_tensor("projT", [NPROJ, NT], BF16, kind="Internal").ap()
    widxT = nc.dram_tensor("widxT", [8, NT], F32, kind="Internal").ap()
    mixedT = nc.dram_tensor("mixedT", [1024, NT], BF16, kind="Internal").ap()
    with contextlib.ExitStack() as stack:
        P = Prog(nc, stack)
        phase_mod(P, nc, A['cT'], A['w_ada'], A['b_ada'], A['g_mix'], A['g_ffn'], mod_d, NB)
        P.barrier()
        phase_inproj(P, nc, A['x'], projT, widxT, A['w_inp'], A['w_widx'], A['mu'], mod_d, A['ident'], NB, S)
        P.barrier()
        phase_dsa(P, nc, projT, widxT, mixedT, A, NB, S, min(256, S // 4))
        P.barrier()
        phase_rwkv(P, nc, projT, mixedT, A, NB, S)
        P.barrier()
        phase_outproj(P, nc, A['x'], out, mixedT, A['w_out'], mod_d, NB, S)
        P.barrier()
        outs = phase_ffn(P, nc, out, out, A['w_ff1'], A['w_ff2'], mod_d, A['ident'], NB, S)
        P.emit(final_wait_ops=outs)
    return nc


def kernel(x, c, w_ada, b_ada, g_mix, g_ffn, w_in, g_q, g_k, g_kv, w_uk, w_uv, mu_shift, w0, w2, a0, a2, g2,
           k_k, k_a, r_k, ln_w, ln_b, w_out, w_ff1, w_ff2):
    f = np.float32
    x = np.asarray(x, f); c = np.asarray(c, f)
    B, S, Dm = x.shape
    NB = B // NCORES
    pc = perm_cols()
    hc = host_consts(S)
    w_in0 = np.asarray(w_in, f)[0]
    mu_full = np.zeros(w_in0.shape[1], f)
    mu_full[N_IN_A:] = np.asarray(mu_shift, f)[0]
    mu_perm = mu_full[pc]
    l0 = lambda a: np.asarray(a, f)[0]
    shared = dict(
        w_ada=np.ascontiguousarray(l0(w_ada)), b_ada=np.ascontiguousarray(l0(b_ada)),
        g_mix=np.ascontiguousarray(l0(g_mix)), g_ffn=np.ascontiguousarray(l0(g_ffn)),
        w_inp=np.ascontiguousarray(w_in0[:, pc]), w_widx=np.ascontiguousarray(w_in0[:, 1216:1224]),
        mu=np.ascontiguousarray(mu_perm[1280:].reshape(14, 128).T),
        ident=hc['ident'],
        w_uk=np.ascontiguousarray(l0(w_uk).reshape(128, 512)), w_uv=np.ascontiguousarray(l0(w_uv).reshape(128, 512)),
        g_q=np.ascontiguousarray(l0(g_q).reshape(64, 1)), g_k=np.ascontiguousarray(l0(g_k).reshape(64, 1)),
        g_kv=np.ascontiguousarray(l0(g_kv).reshape(128, 1)),
        kaug=hc['kaug'], qaug=hc['qaug'], ccorr=hc['ccorr'], dmask=hc['dmask'], sel65=hc['sel65'], ones128=hc['ones128'],
        w_out=np.ascontiguousarray(l0(w_out)), w_ff1=np.ascontiguousarray(l0(w_ff1)), w_ff2=np.ascontiguousarray(l0(w_ff2)),
        **rwkv_consts(),
        **rwkv_params(l0(w0), l0(w2), l0(a0), l0(a2), l0(g2), l0(k_k), l0(k_a), l0(r_k), l0(ln_w), l0(ln_b)),
    )
    in_maps = []
    for i in range(NCORES):
        m = dict(shared)
        m['x'] = np.ascontiguousarray(x[i * NB:(i + 1) * NB].reshape(NB * S, Dm))
        ci = c[i * NB:(i + 1) * NB]
        m['cT'] = np.ascontiguousarray(ci.T.reshape(8, 128, NB).transpose(1, 0, 2))
        in_maps.append(m)
    nc = build_nc({k: v.shape for k, v in in_maps[0].items()})
    res = run_bass_kernel_spmd(nc, in_maps, core_ids=list(range(NCORES)))
    outs = [np.asarray(r["out"], f).reshape(NB, S, Dm) for r in res.results]
    return np.concatenate(outs, axis=0)
```

```python
import contextlib
import os
import numpy as np
import concourse.bass as bass
import concourse.mybir as mybir
from concourse.bass_utils import run_bass_kernel_spmd


F32 = mybir.dt.float32
BF16 = mybir.dt.bfloat16
AF = mybir.ActivationFunctionType
ALU = mybir.AluOpType
AX = mybir.AxisListType

EPOCH = 16000
ENGS = ['pe', 'dve', 'act', 'pool', 'sp']


class Op:
    __slots__ = ('eng', 'fn', 'deps', 'pos', 'signal', 'seq', 'is_dma', 'ctr', 'val', 'waits', 'tag')


class Counter:
    def __init__(self, name):
        self.name = name
        self.sems = []
        self.count = 0

    def need(self, prog, v):
        ep = (v - 1) // EPOCH
        while len(self.sems) <= ep:
            self.sems.append(prog.new_sem())

    def sem_for(self, v):
        ep = (v - 1) // EPOCH
        return self.sems[ep], v - ep * EPOCH


class Prog:
    def __init__(self, nc, stack):
        self.nc = nc
        self.stack = stack
        self.eng_ops = {e: [] for e in ENGS}
        self.res = {}
        self.engctr = {e: Counter(e) for e in ENGS}
        self.dmactr = {}
        self.nsem = 0
        self.ntile = 0
        self.all_dma = []
        self.keep = []
        self._bar_t = {e: self.sb([1, 8], F32, name=f"bar{e}") for e in ENGS}
        self._bar_ps = self.ps([1, 8], F32, name="barps")
        self._bar_tok = {e: f"__bar_{e}" for e in ENGS}
        self._bar_init = False
        self._bar_last = {}

    def new_sem(self):
        self.nsem += 1
        return self.stack.enter_context(self.nc.semaphore(f"sm{self.nsem}"))

    def sb(self, shape, dtype, name=None, stack=None):
        self.ntile += 1
        nm = f"{name or 't'}_{self.ntile}"
        t = (stack or self.stack).enter_context(self.nc.sbuf_tensor(nm, list(shape), dtype))
        self.keep.append(t)
        return t

    def ps(self, shape, dtype=F32, name=None, stack=None):
        self.ntile += 1
        nm = f"{name or 'p'}_{self.ntile}"
        t = (stack or self.stack).enter_context(self.nc.psum_tensor(nm, list(shape), dtype))
        self.keep.append(t)
        return t

    @staticmethod
    def _key(r):
        if isinstance(r, tuple):
            return id(r[0]), r[1]
        return id(r), None

    def _deps(self, op, reads, writes):
        deps = []
        for r in reads:
            tk, sk = self._key(r)
            ent = self.res.setdefault(tk, {})
            for k2, e2 in ent.items():
                if sk is None or k2 is None or k2 == sk:
                    if e2[0] is not None:
                        deps.append((e2[0], 'raw'))
            e = ent.setdefault(sk, [None, []])
            if not op.is_dma:
                e[1] = [o for o in e[1] if o.is_dma or o.eng != op.eng]
            e[1].append(op)
        for w in writes:
            tk, sk = self._key(w)
            ent = self.res.setdefault(tk, {})
            for k2, e2 in ent.items():
                if sk is None or k2 is None or k2 == sk:
                    if e2[0] is not None:
                        deps.append((e2[0], 'waw'))
                    for o in e2[1]:
                        if o is not op:
                            deps.append((o, 'war'))
            if sk is None:
                ent.clear()
            ent[sk] = [op, []]
        return deps

    def op(self, eng, fn, reads=(), writes=(), tag=None):
        o = Op()
        o.eng = eng
        o.fn = fn
        o.is_dma = False
        o.signal = False
        o.seq = 0
        o.ctr = None
        o.val = 0
        o.tag = tag
        o.deps = self._deps(o, reads, writes)
        self.eng_ops[eng].append(o)
        o.pos = len(self.eng_ops[eng])
        return o

    def dma(self, q, out, in_, reads=(), writes=(), ctr=None, tag=None, **kw):
        o = Op()
        o.eng = q
        o.is_dma = True
        o.signal = True
        o.seq = 0
        o.tag = tag
        o.fn = lambda e: e.dma_start(out=out, in_=in_, **kw)
        ck = self._key(ctr)
        c = self.dmactr.get(ck)
        if c is None:
            c = self.dmactr[ck] = Counter(f"d{len(self.dmactr)}")
        c.count += 16
        o.ctr = c
        o.val = c.count
        o.deps = self._deps(o, reads, writes)
        self.eng_ops[q].append(o)
        o.pos = len(self.eng_ops[q])
        self.all_dma.append(o)
        return o

    def barrier(self):
        if not hasattr(self, '_bar_t'):
            self._bar_t = {e: self.sb([1, 8], F32, name=f"bar{e}") for e in ENGS}
            self._bar_ps = self.ps([1, 8], F32, name="barps")
            self._bar_tok = {e: f"__bar_{e}" for e in ENGS}
            self.keep.append(self._bar_tok)
        bt = self._bar_t
        if not self._bar_init:
            self._bar_init = True
            for e_ in ('pe', 'act'):
                self.op('dve', (lambda eng, e_=e_: eng.memset(bt[e_][:], 0.0)), writes=[bt[e_]])
        dmas = [d for d in self.all_dma]
        self.all_dma = []
        first = []
        for e in ENGS:
            if e == 'pe':
                fn = lambda eng: eng.matmul(self._bar_ps[0:1, 0:1], bt['pe'][0:1, 0:1], bt['pe'][0:1, 1:2], start=True, stop=True)
            elif e == 'sp':
                fn = lambda eng: eng.nop()
            elif e == 'act':
                fn = lambda eng: eng.activation(out=bt['act'][0:1, 2:3], in_=bt['act'][0:1, 3:4], func=AF.Copy)
            elif e == 'dve':
                fn = lambda eng: eng.memset(bt['dve'][0:1, 2:3], 0.0)
            else:
                fn = lambda eng: eng.memset(bt['pool'][0:1, 2:3], 0.0)
            o = self.op(e, fn, reads=([bt[e]] if e in ('pe', 'act') else []), writes=[(self._bar_tok[e], 0)])
            if e in self._bar_last:
                o.deps.append((self._bar_last[e], 'waw'))
            self._bar_last[e] = o
            if e == 'sp':
                o.deps += [(d, 'raw') for d in dmas]
            first.append(o)
        for e in ENGS:
            if e == 'pe':
                fn = lambda eng: eng.matmul(self._bar_ps[0:1, 4:5], bt['pe'][0:1, 0:1], bt['pe'][0:1, 1:2], start=True, stop=True)
            elif e == 'sp':
                fn = lambda eng: eng.nop()
            elif e == 'act':
                fn = lambda eng: eng.activation(out=bt['act'][0:1, 4:5], in_=bt['act'][0:1, 3:4], func=AF.Copy)
            elif e == 'dve':
                fn = lambda eng: eng.memset(bt['dve'][0:1, 4:5], 0.0)
            else:
                fn = lambda eng: eng.memset(bt['pool'][0:1, 4:5], 0.0)
            o = self.op(e, fn, reads=([bt[e]] if e in ('pe', 'act') else []))
            o.deps += [(f, 'raw') for f in first if f.eng != e]
            o.deps.append((self._bar_last[e], 'waw'))
            self._bar_last[e] = o
        self.res.clear()

    def init_bar(self):
        self.barrier_ready = True

    def emit(self, final_wait_ops=()):
        fin = self.op('sp', lambda eng: eng.nop())
        fin.deps += [(d, 'raw') for d in final_wait_ops]
        for e in ENGS:
            waited = {}
            for op in self.eng_ops[e]:
                op.waits = []
                best = {}
                for d, kind in op.deps:
                    if d.is_dma:
                        key = ('c', id(d.ctr))
                        v = d.val
                    else:
                        if d.eng == e and kind != 'raw' and e == 'pe':
                            continue
                        key = ('e', d.eng)
                        v = d.pos
                    if waited.get(key, 0) >= v:
                        continue
                    if key not in best or best[key][0] < v:
                        best[key] = (v, d)
                for key, (v, d) in best.items():
                    waited[key] = v
                    d.signal = True
                    op.waits.append(d)
        for e in ENGS:
            seq = 0
            for op in self.eng_ops[e]:
                if op.is_dma:
                    op.ctr.need(self, op.val)
                elif op.signal:
                    seq += 1
                    op.seq = seq
                    self.engctr[e].need(self, seq)
        nc = self.nc

        def run(e, eng):
            for op in self.eng_ops[e]:
                for d in op.waits:
                    if d.is_dma:
                        sem, v = d.ctr.sem_for(d.val)
                    else:
                        sem, v = self.engctr[d.eng].sem_for(d.seq)
                    eng.wait_ge(sem, v)
                ins = op.fn(eng)
                if op.is_dma:
                    sem, _ = op.ctr.sem_for(op.val)
                    ins.then_inc(sem, 16)
                elif op.signal:
                    sem, _ = self.engctr[e].sem_for(op.seq)
                    ins.then_inc(sem, 1)

        with nc.Block() as block:
            @block.tensor
            def _(eng):
                run('pe', eng)

            @block.vector
            def _(eng):
                run('dve', eng)

            @block.scalar
            def _(eng):
                run('act', eng)

            @block.gpsimd
            def _(eng):
                run('pool', eng)

            @block.sync
            def _(eng):
                run('sp', eng)

    def stats(self):
        return {e: len(v) for e, v in self.eng_ops.items()}, self.nsem


def _apname(a):
    try:
        return a.name
    except Exception:
        return a.tensor.name


def I(P, eng, meth, wsub=None, rsub=None, extra_reads=(), extra_writes=(), **kw):
    reads, writes = list(extra_reads), list(extra_writes)
    for k, v in kw.items():
        if isinstance(v, bass.AP):
            nm = _apname(v)
            if k in ('out', 'accum_out'):
                writes.append((nm, wsub.get(nm) if isinstance(wsub, dict) else wsub))
            else:
                reads.append((nm, rsub.get(nm) if isinstance(rsub, dict) else rsub))
    return P.op(eng, (lambda e: getattr(e, meth)(**kw)), reads=reads, writes=writes)


def _key2(r):
    if isinstance(r, tuple):
        a, s = r
    else:
        a, s = r, None
    if isinstance(a, str):
        return a, s
    try:
        return a.name, s
    except Exception:
        return a.tensor.name, s


Prog._key = staticmethod(_key2)


D = 1024
DFF = 4096
RMS_EPS = 1e-6


def bcast_rows(ap1d, n):
    return ap1d.partition_broadcast(n)


def phase_mod(P, nc, cT, w_ada, b_ada, g_mix, g_ffn, mod_d, NB):
    with contextlib.ExitStack() as st:
        ct = P.sb([128, 8, NB], F32, 'ct', st)
        sil = P.sb([128, 8, NB], F32, 'sil', st)
        ones = P.sb([1, NB], F32, 'ones', st)
        mod = P.sb([NB, 6 * D], F32, 'mod', st)
        gb = P.sb([NB, 2, D], F32, 'gb', st)
        wa = [P.sb([128, 8, 512], F32, 'wa', st) for _ in range(2)]
        ba = [P.sb([1, 512], F32, 'ba', st) for _ in range(2)]
        pm = [P.ps([128, 512], F32, 'pm', st) for _ in range(2)]
        P.dma('sp', ct[:], cT, writes=[ct], ctr=ct)
        P.op('act', lambda e: e.activation(out=sil[:], in_=ct[:], func=AF.Silu), reads=[ct], writes=[sil])
        P.op('dve', lambda e: e.memset(ones[:], 1.0), writes=[ones])
        P.dma('sp', gb[:, 0, :], g_mix.partition_broadcast(NB), writes=[(gb, 0)], ctr=gb)
        P.dma('sp', gb[:, 1, :], g_ffn.partition_broadcast(NB), writes=[(gb, 1)], ctr=gb)
        for cg in range(12):
            w = wa[cg % 2]
            bb = ba[cg % 2]
            pp = pm[cg % 2]
            P.dma('sp', w[:], w_ada[:, cg * 512:(cg + 1) * 512].rearrange("(k p) e -> p k e", p=128),
                  writes=[w], ctr=w)
            P.dma('sp', bb[:], b_ada[cg * 512:(cg + 1) * 512].partition_broadcast(1), writes=[bb], ctr=bb)
            for k in range(8):
                P.op('pe', (lambda e, k=k, w=w, pp=pp: e.matmul(pp[0:NB, :], sil[:, k, :], w[:, k, :], start=(k == 0), stop=False)),
                     reads=[sil, w], writes=[pp])
            P.op('pe', (lambda e, bb=bb, pp=pp: e.matmul(pp[0:NB, :], ones[:, :], bb[:, :], start=False, stop=True)),
                 reads=[ones, bb], writes=[pp])
            P.op('act', (lambda e, pp=pp, cg=cg: e.activation(out=mod[:, cg * 512:(cg + 1) * 512], in_=pp[0:NB, :], func=AF.Copy)),
                 reads=[pp], writes=[(mod, cg)])
        for r, gi in ((1, 0), (4, 1)):
            P.op('dve', (lambda e, r=r, gi=gi: e.scalar_tensor_tensor(out=mod[:, r * D:(r + 1) * D], in0=mod[:, r * D:(r + 1) * D],
                                                                     scalar=1.0, in1=gb[:, gi, :], op0=ALU.add, op1=ALU.mult)),
                 reads=[mod, gb], writes=[mod])
        o = P.dma('sp', mod_d.rearrange("b r d -> b (r d)"), mod[:], reads=[mod], writes=[mod_d], ctr=mod)
    return o


def phase_ffn(P, nc, xin, xout, w_ff1, w_ff2, mod_d, ident_d, NB, S):
    NT = NB * S
    TG = 256
    outs = []
    with contextlib.ExitStack() as st:
        w1 = P.sb([128, 8, DFF], BF16, 'w1', st)
        w2 = P.sb([128, 32, D], BF16, 'w2', st)
        ident = P.sb([128, 128], BF16, 'ident', st)
        sh2f = P.sb([128, 8, NB], F32, 'sh2f', st)
        sh2b = P.sb([128, 8, NB], BF16, 'sh2b', st)
        bias1 = P.sb([128, 32, NB], F32, 'bias1', st)
        G2bc = P.sb([128, D], F32, 'G2bc', st)
        gt2bc = P.sb([128, D], F32, 'gt2bc', st)
        xt = [P.sb([128, D], F32, 'xt', st) for _ in range(4)]
        xn = [P.sb([128, D], BF16, 'xn', st) for _ in range(2)]
        junk = P.sb([128, D], BF16, 'junk', st)
        epsc = P.sb([128, 1], F32, 'epsc', st)
        P.op('dve', lambda e: e.memset(epsc[:], RMS_EPS), writes=[epsc])
        ss = [P.sb([128, 2], F32, 'ss', st) for _ in range(2)]
        h2T = [P.sb([128, 8, TG], BF16, 'h2T', st) for _ in range(2)]
        u2T = P.sb([128, 32, TG], BF16, 'u2T', st)
        rr = [P.sb([128, TG], BF16, 'rr', st) for _ in range(2)]
        tmp = [P.sb([128, 512], F32, 'tmp', st) for _ in range(2)]
        tp = [P.ps([128, 1024], BF16, 'tp', st) for _ in range(2)]
        ps1 = [P.ps([128, 512], F32, 'ps1', st) for _ in range(2)]
        ps2 = [P.ps([128, 512], F32, 'ps2', st) for _ in range(2)]

        P.dma('pool', ident[:], ident_d, writes=[ident], ctr=ident)
        for k in range(8):
            P.dma('pool', w1[:, k, :], w_ff1[k * 128:(k + 1) * 128, :], writes=[(w1, k)], ctr=w1)
        for c in range(32):
            P.dma('pool', w2[:, c, :], w_ff2[c * 128:(c + 1) * 128, :], writes=[(w2, c)], ctr=w2)
        for b_ in range(NB):
            P.dma('sp', sh2f[:, :, b_], mod_d[b_, 3, :].rearrange("(k p) -> p k", p=128), writes=[(sh2f, b_)], ctr=sh2f,
                  allow_slow_non_contiguous=True)
        P.op('dve', lambda e: e.tensor_copy(out=sh2b[:], in_=sh2f[:]), reads=[sh2f], writes=[sh2b])
        pb = ps1[0]
        for c in range(32):
            for k in range(8):
                P.op('pe', (lambda e, c=c, k=k: e.matmul(pb[:, c * NB:(c + 1) * NB], w1[:, k, c * 128:(c + 1) * 128], sh2b[:, k, :],
                                                        start=(k == 0), stop=(k == 7))),
                     reads=[w1, sh2b], writes=[pb])
        P.op('act', lambda e: e.activation(out=bias1[:].rearrange("p c b -> p (c b)"), in_=pb[:, 0:32 * NB], func=AF.Copy),
             reads=[pb], writes=[bias1])

        ng = NT // TG
        cur_b = -1
        for g in range(ng):
            b = (g * TG) // S
            if b != cur_b:
                cur_b = b
                P.dma('sp', G2bc[:], mod_d[b, 4, :].partition_broadcast(128), writes=[G2bc], ctr=G2bc)
                P.dma('sp', gt2bc[:], mod_d[b, 5, :].partition_broadcast(128), writes=[gt2bc], ctr=gt2bc)
            hT = h2T[g % 2]
            xs = [xt[(g % 2) * 2 + j] for j in range(2)]
            for j in range(2):
                r0 = g * TG + j * 128
                x = xs[j]
                P.dma('sp', x[:], xin[r0:r0 + 128, :], writes=[x], ctr=x)
                s_ = ss[j]
                xb = xn[j]
                tpp = tp[j]
                P.op('act', (lambda e, x=x, s_=s_: e.activation(out=junk[:], in_=x[:], func=AF.Square, accum_out=s_[:, 0:1])),
                     reads=[x], writes=[junk, s_])
                P.op('act', (lambda e, s_=s_: e.activation(out=s_[:, 1:2], in_=s_[:, 0:1], func=AF.Sqrt, scale=1.0 / D, bias=epsc[:, 0:1])),
                     reads=[s_, epsc], writes=[s_])
                P.op('dve', (lambda e, s_=s_: e.reciprocal(out=s_[:, 1:2], in_=s_[:, 1:2])), reads=[s_], writes=[s_])
                P.op('dve', (lambda e, x=x, s_=s_, xb=xb: e.scalar_tensor_tensor(out=xb[:], in0=x[:], scalar=s_[:, 1:2], in1=G2bc[:],
                                                                               op0=ALU.mult, op1=ALU.mult)),
                     reads=[x, s_, G2bc], writes=[xb])
                for k in range(8):
                    P.op('pe', (lambda e, k=k, xb=xb, tpp=tpp: e.transpose(out=tpp[:, k * 128:(k + 1) * 128], in_=xb[:, k * 128:(k + 1) * 128],
                                                                        identity=ident[:])),
                         reads=[xb, ident], writes=[tpp])
                P.op('act', (lambda e, tpp=tpp, hT=hT, j=j: e.activation(out=hT[:, :, j * 128:(j + 1) * 128],
                                                                       in_=tpp[:].rearrange("p (k t) -> p k t", k=8), func=AF.Copy)),
                     reads=[tpp], writes=[(hT, j)])
            for c in range(32):
                pp = ps1[c % 2]
                r_ = rr[c % 2]
                for k in range(8):
                    P.op('pe', (lambda e, c=c, k=k, pp=pp, hT=hT: e.matmul(pp[:, 0:TG], w1[:, k, c * 128:(c + 1) * 128], hT[:, k, :],
                                                                        start=(k == 0), stop=(k == 7))),
                         reads=[w1, hT], writes=[pp])
                P.op('act', (lambda e, c=c, pp=pp, r_=r_, b=b: e.activation(out=r_[:], in_=pp[:, 0:TG], func=AF.Relu, bias=bias1[:, c, b:b + 1])),
                     reads=[pp, bias1], writes=[r_])
                P.op('dve', (lambda e, c=c, r_=r_: e.tensor_tensor(out=u2T[:, c, :], in0=r_[:], in1=r_[:], op=ALU.mult)),
                     reads=[r_], writes=[(u2T, c)])
            for j in range(2):
                x = xs[j]
                for hf in range(2):
                    pp = ps2[hf]
                    tm = tmp[hf]
                    for c in range(32):
                        P.op('pe', (lambda e, c=c, j=j, hf=hf, pp=pp: e.matmul(pp[:], u2T[:, c, j * 128:(j + 1) * 128], w2[:, c, hf * 512:(hf + 1) * 512],
                                                                            start=(c == 0), stop=(c == 31))),
                             reads=[(u2T, c), w2], writes=[pp])
                    P.op('dve', (lambda e, pp=pp, tm=tm, hf=hf: e.tensor_tensor(out=tm[:], in0=pp[:], in1=gt2bc[:, hf * 512:(hf + 1) * 512], op=ALU.mult)),
                         reads=[pp, gt2bc], writes=[tm])
                    P.op('dve', (lambda e, x=x, tm=tm, hf=hf: e.tensor_tensor(out=x[:, hf * 512:(hf + 1) * 512], in0=tm[:], in1=x[:, hf * 512:(hf + 1) * 512], op=ALU.add)),
                         reads=[tm, x], writes=[x])
                r0 = g * TG + j * 128
                outs.append(P.dma('pool', xout[r0:r0 + 128, :], x[:], reads=[x], writes=[(xout, r0)], ctr=x))
    return outs


NPROJ = 3072


def phase_inproj(P, nc, x, projT, widxT, w_inp, w_widx, mu_d, mod_d, ident_d, NB, S):
    NT = NB * S
    TG = 512
    with contextlib.ExitStack() as st:
        win = P.sb([128, 8, NPROJ], BF16, 'win', st)
        wwx = P.sb([128, 8, 8], BF16, 'wwx', st)
        ident = P.sb([128, 128], BF16, 'ident', st)
        sh1f = P.sb([128, 8, NB], F32, 'sh1f', st)
        sh1b = P.sb([128, 8, NB], BF16, 'sh1b', st)
        bias = P.sb([128, 25, NB], F32, 'bias', st)
        mu = P.sb([128, 14], F32, 'mu', st)
        G1bc = P.sb([128, D], F32, 'G1bc', st)
        epsc = P.sb([128, 1], F32, 'epsc', st)
        xt = [P.sb([128, D], F32, 'xt', st) for _ in range(2)]
        xn = [P.sb([128, D], BF16, 'xn', st) for _ in range(2)]
        junk = P.sb([128, D], BF16, 'junk', st)
        ss = [P.sb([128, 2], F32, 'ss', st) for _ in range(2)]
        hT = [P.sb([128, 8, TG], BF16, 'hT', st) for _ in range(2)]
        pb = [P.sb([128, TG + 1], BF16, 'pb', st) for _ in range(14)]
        tmpd = [P.sb([128, TG], BF16, 'tmpd', st) for _ in range(2)]
        stg = [P.sb([128, TG], BF16, 'stg', st) for _ in range(3)]
        stw = [P.sb([8, TG], F32, 'stw', st) for _ in range(2)]
        tp = [P.ps([128, 1024], BF16, 'tp', st) for _ in range(2)]
        pp = [P.ps([128, 512], F32, 'pp', st) for _ in range(3)]

        I(P, 'dve', 'memset', ap=epsc[:], constant=RMS_EPS, extra_writes=[epsc])
        P.dma('pool', ident[:], ident_d, writes=[ident], ctr=ident)
        for k in range(8):
            P.dma('pool', win[:, k, :], w_inp[k * 128:(k + 1) * 128, :], writes=[(win, k)], ctr=win)
        P.dma('pool', wwx[:], w_widx.rearrange("(k p) e -> p k e", p=128), writes=[wwx], ctr=wwx)
        P.dma('sp', mu[:], mu_d, writes=[mu], ctr=mu)
        for b_ in range(NB):
            P.dma('sp', sh1f[:, :, b_], mod_d[b_, 0, :].rearrange("(k p) -> p k", p=128), writes=[(sh1f, b_)], ctr=sh1f,
                  allow_slow_non_contiguous=True)
        I(P, 'dve', 'tensor_copy', out=sh1b[:], in_=sh1f[:])
        pbm = pp[0]
        for cg in range(25):
            for k in range(8):
                if cg < 24:
                    I(P, 'pe', 'matmul', out=pbm[:, cg * NB:(cg + 1) * NB], lhsT=win[:, k, cg * 128:(cg + 1) * 128], rhs=sh1b[:, k, :],
                      start=(k == 0), stop=(k == 7))
                else:
                    I(P, 'pe', 'matmul', out=pbm[0:8, cg * NB:(cg + 1) * NB], lhsT=wwx[:, k, :], rhs=sh1b[:, k, :],
                      start=(k == 0), stop=(k == 7))
        I(P, 'act', 'activation', out=bias[:, 0:24, :].rearrange("p c b -> p (c b)"), in_=pbm[:, 0:24 * NB], func=AF.Copy)
        I(P, 'act', 'activation', out=bias[0:8, 24, :], in_=pbm[0:8, 24 * NB:25 * NB], func=AF.Copy)

        ng = NT // TG
        cur_b = -1
        si = 0
        for g in range(ng):
            tok0 = g * TG
            b = tok0 // S
            seq_start = (tok0 % S == 0)
            if b != cur_b:
                cur_b = b
                P.dma('sp', G1bc[:], mod_d[b, 1, :].partition_broadcast(128), writes=[G1bc], ctr=G1bc)
            h = hT[g % 2]
            for j in range(4):
                r0 = tok0 + j * 128
                xx = xt[j % 2]
                s_ = ss[j % 2]
                xb = xn[j % 2]
                tpp = tp[j % 2]
                P.dma('sp', xx[:], x[r0:r0 + 128, :], writes=[xx], ctr=xx)
                I(P, 'act', 'activation', out=junk[:], in_=xx[:], func=AF.Square, accum_out=s_[:, 0:1])
                I(P, 'act', 'activation', out=s_[:, 1:2], in_=s_[:, 0:1], func=AF.Sqrt, scale=1.0 / D, bias=epsc[:, 0:1])
                I(P, 'dve', 'reciprocal', out=s_[:, 1:2], in_=s_[:, 1:2])
                I(P, 'dve', 'scalar_tensor_tensor', out=xb[:], in0=xx[:], scalar=s_[:, 1:2], in1=G1bc[:], op0=ALU.mult, op1=ALU.mult)
                for k in range(8):
                    I(P, 'pe', 'transpose', out=tpp[:, k * 128:(k + 1) * 128], in_=xb[:, k * 128:(k + 1) * 128], identity=ident[:])
                I(P, 'act', 'activation', out=h[:, :, j * 128:(j + 1) * 128], in_=tpp[:].rearrange("p (k t) -> p k t", k=8), func=AF.Copy,
                  wsub=j)
            for cg in range(24):
                pq = pp[cg % 3]
                for k in range(8):
                    I(P, 'pe', 'matmul', out=pq[:], lhsT=win[:, k, cg * 128:(cg + 1) * 128], rhs=h[:, k, :], start=(k == 0), stop=(k == 7))
                sg = stg[si % 3]
                si += 1
                if cg < 10:
                    I(P, 'act', 'activation', out=sg[:], in_=pq[:], func=AF.Identity, bias=bias[:, cg, b:b + 1])
                else:
                    gi = cg - 10
                    pbb = pb[gi]
                    td = tmpd[gi % 2]
                    if seq_start:
                        I(P, 'dve', 'memset', ap=pbb[:, 0:1], constant=0.0, extra_writes=[pbb])
                    else:
                        I(P, 'dve', 'tensor_copy', out=pbb[:, 0:1], in_=pbb[:, TG:TG + 1])
                    I(P, 'act', 'activation', out=pbb[:, 1:TG + 1], in_=pq[:], func=AF.Identity, bias=bias[:, cg, b:b + 1])
                    I(P, 'dve', 'tensor_tensor', out=td[:], in0=pbb[:, 0:TG], in1=pbb[:, 1:TG + 1], op=ALU.subtract)
                    I(P, 'dve', 'scalar_tensor_tensor', out=sg[:], in0=td[:], scalar=mu[:, gi:gi + 1], in1=pbb[:, 1:TG + 1],
                      op0=ALU.mult, op1=ALU.add)
                P.dma('pool', projT[cg * 128:(cg + 1) * 128, tok0:tok0 + TG], sg[:], reads=[sg], writes=[(projT, (cg, g))], ctr=sg)
            pq = pp[0]
            for k in range(8):
                I(P, 'pe', 'matmul', out=pq[0:8, :], lhsT=wwx[:, k, :], rhs=h[:, k, :], start=(k == 0), stop=(k == 7))
            sw = stw[g % 2]
            I(P, 'act', 'activation', out=sw[:], in_=pq[0:8, :], func=AF.Identity, bias=bias[0:8, 24, b:b + 1])
            P.dma('pool', widxT[:, tok0:tok0 + TG], sw[:], reads=[sw], writes=[(widxT, g)], ctr=sw)


def phase_outproj(P, nc, x, xout, mixedT, w_out, mod_d, NB, S):
    NT = NB * S
    with contextlib.ExitStack() as st:
        wo = P.sb([128, 8, D], BF16, 'wo', st)
        gt1bc = P.sb([128, D], F32, 'gt1bc', st)
        mt = [P.sb([128, 8, 128], BF16, 'mt', st) for _ in range(2)]
        xt = [P.sb([128, D], F32, 'xt', st) for _ in range(2)]
        tmp = [P.sb([128, 512], F32, 'tmp', st) for _ in range(2)]
        pp = [P.ps([128, 512], F32, 'pp', st) for _ in range(2)]
        for k in range(8):
            P.dma('pool', wo[:, k, :], w_out[k * 128:(k + 1) * 128, :], writes=[(wo, k)], ctr=wo)
        cur_b = -1
        for i in range(NT // 128):
            r0 = i * 128
            b = r0 // S
            if b != cur_b:
                cur_b = b
                P.dma('sp', gt1bc[:], mod_d[b, 2, :].partition_broadcast(128), writes=[gt1bc], ctr=gt1bc)
            m = mt[i % 2]
            xx = xt[i % 2]
            P.dma('sp', m[:], mixedT[:, r0:r0 + 128].rearrange("(k p) t -> p k t", p=128), writes=[m], ctr=m)
            P.dma('sp', xx[:], x[r0:r0 + 128, :], writes=[xx], ctr=xx)
            for hf in range(2):
                pq = pp[hf]
                tm = tmp[hf]
                for k in range(8):
                    I(P, 'pe', 'matmul', out=pq[:], lhsT=m[:, k, :], rhs=wo[:, k, hf * 512:(hf + 1) * 512], start=(k == 0), stop=(k == 7))
                I(P, 'dve', 'tensor_tensor', out=tm[:], in0=pq[:], in1=gt1bc[:, hf * 512:(hf + 1) * 512], op=ALU.mult)
                I(P, 'dve', 'tensor_tensor', out=xx[:, hf * 512:(hf + 1) * 512], in0=tm[:], in1=xx[:, hf * 512:(hf + 1) * 512], op=ALU.add)
            P.dma('pool', xout[r0:r0 + 128, :], xx[:], reads=[xx], writes=[(xout, i)], ctr=xx)

STOP = 99

NEG = -30000.0


def phase_dsa(P, nc, projT, widxT, mixedT, C, NB, S, TOPK):
    NTL = S // 128
    NBLK = S // 512
    with contextlib.ExitStack() as st:
        sb = lambda shp, dt, nm: P.sb(shp, dt, nm, st)
        wuk = sb([128, 512], BF16, 'wuk'); wuv = sb([128, 512], BF16, 'wuv')
        ident = sb([128, 128], BF16, 'ident'); identf = sb([128, 128], F32, 'identf')
        ones = sb([128, 128], BF16, 'ones'); sel65 = sb([65, 64], BF16, 'sel65')
        ccorr = sb([128, 8, 128], BF16, 'ccorr'); dmask = sb([128, 128], F32, 'dmask')
        gq = sb([64, 1], F32, 'gq'); gk = sb([64, 1], F32, 'gk'); gqk = sb([64, 1], F32, 'gqk'); gkv = sb([128, 1], F32, 'gkv')
        epsc = sb([128, 1], F32, 'epsc')
        qaug = sb([68, 8, S], BF16, 'qaug'); kaug = sb([68, 8, S], BF16, 'kaug')
        qidx = sb([128, 4, S], BF16, 'qidx'); kidx = sb([128, S], BF16, 'kidx')
        ckv = sb([128, S], BF16, 'ckv'); vaug = sb([128, NTL, 8, 65], BF16, 'vaug')
        MbT2 = [sb([128, NTL, 512], BF16, 'MbT') for _ in range(2)]; scs = [sb([128, S], F32, 'sc') for _ in range(4)]; junk = [sb([128, S], BF16, 'junkS')] * 2
        lo = sb([128, 4], F32, 'lo'); negt = sb([128, 4], F32, 'negt'); Sg = sb([128, 4], F32, 'Sg'); dd = sb([128, 4], F32, 'dd')
        Mb = sb([128, S], BF16, 'Mb'); mx = sb([128, 8], F32, 'mx'); thr = sb([128, 1], F32, 'thr')
        clat = sb([128, 512], BF16, 'clat'); sqc = sb([128, 512], BF16, 'sqc')
        qraw = scs[3][:].bitcast(BF16)[0:64, :].rearrange("p (h t) -> p h t", h=8)
        sqk = [sb([64, 512], BF16, 'sqk') for _ in range(2)]
        wT = sb([8, 128], F32, 'wT'); wv = sb([128, 8], F32, 'wv'); rl = [sb([128, 512], F32, 'rl') for _ in range(2)]
        rcb = rl[0]; rr = [rl[1][0:64, :], sb([64, 512], F32, 'rr')]
        Pt = [sb([128, 512], BF16, 'Pt') for _ in range(3)]
        Osb = sb([65, 512], BF16, 'Osb'); rden = sb([64, 512], F32, 'rden'); oa = [sb([64, 512], BF16, 'oa') for _ in range(2)]
        PB = [P.ps([128, 512], F32, 'PB', st) for _ in range(7)]
        psA = PB[0:3]
        psB = PB[3:5]
        I(P, 'dve', 'memset', ap=epsc[:], constant=RMS_EPS, extra_writes=[epsc])
        for t_, d_ in ((wuk, 'w_uk'), (wuv, 'w_uv'), (ident, 'ident'), (ones, 'ones128'), (sel65, 'sel65'), (ccorr, 'ccorr')):
            P.dma('pool', t_[:], C[d_], writes=[t_], ctr=t_)
        for t_, d_ in ((identf, 'ident'), (dmask, 'dmask'), (gq, 'g_q'), (gk, 'g_k'), (gkv, 'g_kv')):
            P.dma('sp', t_[:], C[d_], writes=[t_], ctr=t_)
        I(P, 'dve', 'tensor_tensor', out=gqk[:], in0=gq[:], in1=gk[:], op=ALU.mult)
        I(P, 'dve', 'tensor_scalar', out=gqk[:], in0=gqk[:], scalar1=0.125, scalar2=None, op0=ALU.mult)
        for h in range(8):
            P.dma('pool', kaug[64:68, h, :], C['kaug'], writes=[(kaug, ('a', h))], ctr=(kaug, 'a'))
            P.dma('pool', qaug[64:68, h, :], C['qaug'][h], writes=[(qaug, ('a', h))], ctr=(qaug, 'a'))
        I(P, 'dve', 'memset', ap=vaug[:, :, :, 64:65], constant=1.0, extra_writes=[(vaug, 'one')])
        for b in range(NB):
            T0 = b * S
            P.dma('sp', qidx[:], projT[512:1024, T0:T0 + S].rearrange("(m p) t -> p m t", p=128), writes=[qidx], ctr=qidx)
            P.dma('sp', kidx[:], projT[1152:1280, T0:T0 + S], writes=[kidx], ctr=kidx)
            for blk in range(NBLK):
                c0 = blk * 512
                P.dma('sp', clat[:], projT[1024:1152, T0 + c0:T0 + c0 + 512], writes=[clat], ctr=clat)
                P.dma('sp', qraw[:], projT[0:512, T0 + c0:T0 + c0 + 512].rearrange("(h p) t -> p h t", p=64), writes=[qraw], ctr=qraw)
                I(P, 'dve', 'tensor_tensor', out=sqc[:], in0=clat[:], in1=clat[:], op=ALU.mult)
                pa = psA[0]
                I(P, 'pe', 'matmul', out=pa[:], lhsT=ones[:], rhs=sqc[:], start=True, stop=True)
                I(P, 'act', 'activation', out=rcb[:], in_=pa[:], func=AF.Sqrt, scale=1.0 / 128, bias=epsc[:, 0:1])
                I(P, 'dve', 'reciprocal', out=rcb[:], in_=rcb[:])
                I(P, 'dve', 'scalar_tensor_tensor', out=ckv[:, c0:c0 + 512], in0=clat[:], scalar=gkv[:, 0:1], in1=rcb[:],
                  op0=ALU.mult, op1=ALU.mult, wsub=blk)
                for j in range(4):
                    ti = blk * 4 + j
                    pv = psB[j % 2]
                    I(P, 'pe', 'matmul', out=pv[:], lhsT=ckv[:, ti * 128:(ti + 1) * 128], rhs=wuv[:], start=True, stop=True)
                    I(P, 'act', 'activation', out=vaug[:, ti, :, 0:64], in_=pv[:].rearrange("p (h d) -> p h d", h=8), func=AF.Copy,
                      wsub=('v', ti))
                for h in range(8):
                    pk = psA[1 + h % 2]; p2 = psB[h % 2]; sq = sqk[h % 2]; r_ = rr[h % 2]
                    I(P, 'pe', 'matmul', out=pk[0:64, :], lhsT=wuk[:, h * 64:(h + 1) * 64], rhs=ckv[:, c0:c0 + 512], start=True, stop=True)
                    I(P, 'act', 'activation', out=sq[:], in_=pk[0:64, :], func=AF.Square)
                    I(P, 'pe', 'matmul', out=p2[0:64, :], lhsT=ones[0:64, 0:64], rhs=sq[:], start=True, stop=True)
                    I(P, 'act', 'activation', out=r_[:], in_=p2[0:64, :], func=AF.Sqrt, scale=1.0 / 64, bias=epsc[0:64, 0:1])
                    I(P, 'dve', 'reciprocal', out=r_[:], in_=r_[:])
                    I(P, 'dve', 'tensor_tensor', out=kaug[0:64, h, c0:c0 + 512], in0=pk[0:64, :], in1=r_[:], op=ALU.mult, wsub=('k', h, blk))
                    p3 = psB[h % 2]
                    I(P, 'act', 'activation', out=sq[:], in_=qraw[:, h, :], func=AF.Square)
                    I(P, 'pe', 'matmul', out=p3[0:64, :], lhsT=ones[0:64, 0:64], rhs=sq[:], start=True, stop=True)
                    I(P, 'act', 'activation', out=r_[:], in_=p3[0:64, :], func=AF.Sqrt, scale=1.0 / 64, bias=epsc[0:64, 0:1])
                    I(P, 'dve', 'reciprocal', out=r_[:], in_=r_[:])
                    I(P, 'dve', 'scalar_tensor_tensor', out=qaug[0:64, h, c0:c0 + 512], in0=qraw[:, h, :], scalar=gqk[:, 0:1], in1=r_[:],
                      op0=ALU.mult, op1=ALU.mult, wsub=('q', h, blk))
            if STOP <= 1:
                continue
            if STOP <= 1:
                continue
            def prep(q, T0=T0):
                MbT = MbT2[q % 2]
                I(P, 'dve', 'memset', ap=MbT[:], constant=NEG, extra_writes=[MbT])
                RNG = 1024.0
                NIT = 26
                for ti in range(4):
                    i = q * 4 + ti
                    L = 128 * (i + 1)
                    sc = scs[ti]
                    P.dma('sp', wT[:], widxT[:, T0 + i * 128:T0 + (i + 1) * 128], writes=[wT], ctr=wT)
                    pw = PB[4]
                    I(P, 'pe', 'transpose', out=pw[:, 0:8], in_=wT[:], identity=identf[0:8, 0:8])
                    I(P, 'act', 'activation', out=wv[:], in_=pw[:, 0:8], func=AF.Copy)
                    nkb = (L + 511) // 512
                    for kb in range(nkb):
                        cols = min(512, L - kb * 512)
                        for h in range(8):
                            base = (h % 2) * 64; m = h // 2
                            pi = PB[2 + h % 2]; r_ = rl[h % 2]
                            I(P, 'pe', 'matmul', out=pi[:, 0:cols], lhsT=qidx[base:base + 64, m, i * 128:(i + 1) * 128],
                              rhs=kidx[base:base + 64, kb * 512:kb * 512 + cols], start=True, stop=True)
                            I(P, 'act', 'activation', out=r_[:, 0:cols], in_=pi[:, 0:cols], func=AF.Relu)
                            if h == 0:
                                I(P, 'dve', 'tensor_scalar', out=sc[:, kb * 512:kb * 512 + cols], in0=r_[:, 0:cols], scalar1=wv[:, 0:1],
                                  scalar2=None, op0=ALU.mult)
                            else:
                                I(P, 'dve', 'scalar_tensor_tensor', out=sc[:, kb * 512:kb * 512 + cols], in0=r_[:, 0:cols],
                                  scalar=wv[:, h:h + 1], in1=sc[:, kb * 512:kb * 512 + cols], op0=ALU.mult, op1=ALU.add)
                    I(P, 'dve', 'tensor_tensor', out=sc[:, L - 128:L], in0=sc[:, L - 128:L], in1=dmask[:], op=ALU.add)
                    yield 1
                act_t = [ti for ti in range(4) if 128 * (q * 4 + ti + 1) > TOPK]
                if act_t:
                    I(P, 'dve', 'memset', ap=lo[:], constant=-RNG, extra_writes=[lo])
                    for k in range(NIT):
                        s_k = RNG / (2.0 ** k)
                        for ti in act_t:
                            L = 128 * (q * 4 + ti + 1)
                            I(P, 'dve', 'tensor_scalar', out=negt[:, ti:ti + 1], in0=lo[:, ti:ti + 1], scalar1=-1.0, scalar2=-s_k,
                              op0=ALU.mult, op1=ALU.add, wsub=ti, rsub=ti)
                            I(P, 'act', 'activation', out=junk[ti % 2][:, 0:L], in_=scs[ti][:, 0:L], func=AF.Sign, bias=negt[:, ti:ti + 1],
                              accum_out=Sg[:, ti:ti + 1], wsub={Sg.name: ti}, rsub=ti)
                        if k % 3 == 2:
                            yield 1
                        for ti in act_t:
                            L = 128 * (q * 4 + ti + 1)
                            I(P, 'dve', 'tensor_scalar', out=dd[:, ti:ti + 1], in0=Sg[:, ti:ti + 1], scalar1=float(2 * TOPK - L), scalar2=None,
                              op0=ALU.is_ge, wsub=ti, rsub=ti)
                            I(P, 'dve', 'scalar_tensor_tensor', out=lo[:, ti:ti + 1], in0=dd[:, ti:ti + 1], scalar=s_k, in1=lo[:, ti:ti + 1],
                              op0=ALU.mult, op1=ALU.add, wsub=ti, rsub=ti)
                for ti in range(4):
                    i = q * 4 + ti
                    L = 128 * (i + 1)
                    sc = scs[ti]
                    if L > TOPK:
                        I(P, 'dve', 'tensor_scalar', out=Mb[:, 0:L], in0=sc[:, 0:L], scalar1=lo[:, ti:ti + 1], scalar2=NEG, op0=ALU.is_le, op1=ALU.mult)
                    else:
                        I(P, 'dve', 'tensor_scalar', out=Mb[:, 0:L], in0=sc[:, 0:L], scalar1=-1e29, scalar2=NEG, op0=ALU.is_lt, op1=ALU.mult)
                    for j0 in range(0, i + 1, 8):
                        j1 = min(i + 1, j0 + 8)
                        pt = PB[6]
                        ptb = pt[:].bitcast(BF16)
                        for j in range(j0, j1):
                            I(P, 'pe', 'transpose', out=ptb[:, (j - j0) * 128:(j - j0 + 1) * 128], in_=Mb[:, j * 128:(j + 1) * 128], identity=ident[:])
                        I(P, 'act', 'activation', out=MbT[:, j0:j1, ti * 128:(ti + 1) * 128],
                          in_=ptb[:, 0:(j1 - j0) * 128].rearrange("p (j t) -> p j t", t=128), func=AF.Copy, wsub=('t', i, j0))
                    yield 1

            def attn(q, T0=T0, b=b):
                MbT = MbT2[q % 2]
                nsb = 4 * q + 4
                for h in range(8):
                    po = PB[5]
                    for j in range(nsb):
                        pl = PB[j % 2]; pt_ = Pt[j % 3]
                        I(P, 'pe', 'matmul', out=pl[:], lhsT=kaug[0:68, h, j * 128:(j + 1) * 128], rhs=qaug[0:68, h, q * 512:(q + 1) * 512],
                          start=True, stop=False)
                        diag = j >= 4 * q
                        I(P, 'pe', 'matmul', out=pl[:], lhsT=ident[:], rhs=MbT[:, j, :], start=False, stop=not diag)
                        if diag:
                            dj = j - 4 * q
                            I(P, 'pe', 'matmul', out=pl[:, dj * 128:(dj + 1) * 128], lhsT=ident[:], rhs=ccorr[:, h, :], start=False, stop=True)
                        I(P, 'act', 'activation', out=pt_[:], in_=pl[:], func=AF.Exp)
                        I(P, 'pe', 'matmul', out=po[0:65, :], lhsT=vaug[:, j, h, :], rhs=pt_[:], start=(j == 0), stop=(j == nsb - 1))
                        if j % 4 == 3 and j != nsb - 1:
                            yield 1
                    I(P, 'act', 'activation', out=Osb[:], in_=po[0:65, :], func=AF.Copy)
                    pd = PB[4]
                    I(P, 'pe', 'matmul', out=pd[0:64, :], lhsT=sel65[:], rhs=Osb[:], start=True, stop=True)
                    I(P, 'dve', 'reciprocal', out=rden[:], in_=pd[0:64, :])
                    o_ = oa[h % 2]
                    I(P, 'dve', 'tensor_tensor', out=o_[:], in0=Osb[0:64, :], in1=rden[:], op=ALU.mult)
                    P.dma('sp', mixedT[h * 64:(h + 1) * 64, T0 + q * 512:T0 + (q + 1) * 512], o_[:], reads=[o_], writes=[(mixedT, (h, b, q))], ctr=o_)
                    yield 1

            for _ in prep(0):
                pass
            for q in range(NBLK):
                ga = attn(q)
                gp = prep(q + 1) if q + 1 < NBLK else iter(())
                da = dp = False
                while not (da and dp):
                    if not dp and next(gp, None) is None:
                        dp = True
                    if not da and next(ga, None) is None:
                        da = True

RSTOP = 99

GN_EPS = 64e-5


def phase_rwkv(P, nc, projT, mixedT, C, NB, S):
    TGR = 256
    NCH = 4
    NU = NCH * 8
    c0 = -float(np.exp(-0.5))
    with contextlib.ExitStack() as st:
        sb = lambda shp, dt, nm: P.sb(shp, dt, nm, st)
        w2b = sb([64, 512], BF16, 'w2b'); a2b = sb([64, 512], BF16, 'a2b'); g2b = sb([128, 512], BF16, 'g2b')
        vec = sb([64, 5, 8], F32, 'vec'); omk = sb([64, 8], F32, 'omk')
        lnw = sb([64, 512], F32, 'lnw'); lnb = sb([64, 512], F32, 'lnb'); rkr = sb([64, 512], F32, 'rkr')
        ones = sb([128, 128], BF16, 'onesr'); ident = sb([128, 128], BF16, 'identr')
        mask1 = sb([64, 128], F32, 'mask1'); mask3 = sb([64, 64], F32, 'mask3'); rst = sb([64, TGR], F32, 'rst')
        gneps = sb([64, 1], F32, 'gneps')
        F3 = [64, 8, TGR]
        rT = sb(F3, BF16, 'rT'); kT = sb(F3, BF16, 'kT'); vT = sb(F3, BF16, 'vT')
        xw = sb([64, TGR], BF16, 'xw'); xa = sb([64, TGR], BF16, 'xa'); xg = sb([128, TGR], BF16, 'xg')
        thb = sb([64, TGR], BF16, 'thb'); sgx = sb([128, TGR], BF16, 'sgx')
        sg = sb(F3, F32, 'sg'); cs = sb(F3, F32, 'cs'); Ea = sb(F3, F32, 'Ea'); Eb = sb(F3, F32, 'Eb')
        a_ = sb(F3, BF16, 'a_'); kkr = sb(F3, F32, 'kkr'); sq = sb(F3, BF16, 'sq')
        kk = sb(F3, F32, 'kk'); t1 = sb(F3, F32, 't1'); nrm = t1
        kmod = sb(F3, F32, 'kmod'); beta = kkr; gT = sb(F3, BF16, 'gT')
        bp = sb(F3, BF16, 'bp'); kp = sb(F3, BF16, 'kp'); kmb = sq
        AR = sb([64, 8, NCH, 2, 64], BF16, 'AR'); BK = sb([64, 8, NCH, 2, 64], BF16, 'BK'); GCt = sb([64, 8, NCH], F32, 'GCt')
        T3 = [64, NCH, 512]
        bpTM = sb(T3, BF16, 'bpTM'); kpTM = sb(T3, BF16, 'kpTM'); vTM = sb(T3, BF16, 'vTM')
        rTM = sb(T3, BF16, 'rTM'); kmTM = sb(T3, BF16, 'kmTM'); gTM = sb(T3, BF16, 'gTM')
        NM1 = [sb([64, 8, 128], BF16, 'NM1') for _ in range(NCH)]; NM2 = [sb([64, 8, 128], BF16, 'NM2') for _ in range(NCH)]
        NTt = [sb([64, 8, 64], BF16, 'NTt') for _ in range(NCH)]; X = [sb([64, 8, 64], BF16, 'X') for _ in range(NCH)]
        Pm = [[sb([64, 8, 64], BF16, 'Pm')] * 2 for _ in range(NCH)]; PTm = [[sb([64, 8, 64], BF16, 'PTm')] * 2 for _ in range(NCH)]
        Ysb = sb([64, 512], BF16, 'Ysb'); Usb = sb([64, 512], BF16, 'Usb')
        hst = sb([64, 8, 64], F32, 'hst'); hs = sb([64, 8, 64], F32, 'hs'); hbf = sb([64, 8, 64], BF16, 'hbf')
        OTM = sb(T3, F32, 'OTM')
        s1 = sb([64, NU], F32, 's1'); s2 = sb([64, NU], F32, 's2'); bs = sb([64, NU], F32, 'bs')
        ob = sb(T3, BF16, 'ob'); obT = [sb(F3, BF16, 'obT')] * 2
        PS = [P.ps([128, 512], F32, 'PSr', st) for _ in range(7)]
        cnt = [0]

        def nxt():
            cnt[0] += 1
            return PS[cnt[0] % 7]

        for t_, d_ in ((w2b, 'w2'), (a2b, 'a2'), (g2b, 'g2'), (ones, 'ones128'), (ident, 'ident')):
            P.dma('pool', t_[:], C[d_], writes=[t_], ctr=t_)
        for t_, d_ in ((vec, 'vec8'), (mask1, 'mask1'), (mask3, 'mask3')):
            P.dma('sp', t_[:], C[d_], writes=[t_], ctr=t_)
        P.dma('sp', rst[:], C['rst'][0:64, :], writes=[rst], ctr=rst)
        for t_, d_ in ((lnw, 'ln_w'), (lnb, 'ln_b'), (rkr, 'r_k1')):
            P.dma('sp', t_[:], C[d_].partition_broadcast(64), writes=[t_], ctr=t_)
        I(P, 'dve', 'memset', ap=gneps[:], constant=GN_EPS, extra_writes=[gneps])
        I(P, 'dve', 'tensor_scalar', out=omk[:], in0=vec[:, 3, :], scalar1=-1.0, scalar2=1.0, op0=ALU.mult, op1=ALU.add)

        def v3(t, h):
            return t[:, h, :].rearrange("p (n c) -> p n c", c=64)

        def fm_load(t_, r0, tok):
            P.dma('sp', t_[:], projT[r0:r0 + 512, tok].rearrange("(h p) t -> p h t", p=64), writes=[t_], ctr=t_)

        gi = 0
        for b in range(NB):
            I(P, 'dve', 'memset', ap=hst[:], constant=0.0, extra_writes=[hst])
            I(P, 'dve', 'memset', ap=hbf[:], constant=0.0, extra_writes=[hbf])
            for g in range(S // TGR):
                tok = b * S + g * TGR
                sl = slice(tok, tok + TGR)
                fm_load(rT, 1280, sl); fm_load(kT, 1792, sl); fm_load(vT, 2304, sl)
                P.dma('sp', xw[:], projT[2816:2880, sl], writes=[xw], ctr=xw)
                P.dma('sp', xa[:], projT[2880:2944, sl], writes=[xa], ctr=xa)
                P.dma('sp', xg[:], projT[2944:3072, sl], writes=[xg], ctr=xg)
                I(P, 'act', 'activation', out=thb[:], in_=xw[:], func=AF.Tanh)
                I(P, 'act', 'activation', out=sgx[:], in_=xg[:], func=AF.Sigmoid)
                for hp in range(4):
                    p1, p2, p3 = nxt(), nxt(), nxt()
                    for e2 in range(2):
                        h = hp * 2 + e2
                        hc = slice(h * 64, (h + 1) * 64)
                        oc = slice(e2 * TGR, (e2 + 1) * TGR)
                        I(P, 'pe', 'matmul', out=p1[0:64, oc], lhsT=w2b[:, hc], rhs=thb[:], start=True, stop=True)
                        I(P, 'pe', 'matmul', out=p2[0:64, oc], lhsT=a2b[:, hc], rhs=xa[:], start=True, stop=True)
                        I(P, 'pe', 'matmul', out=p3[0:64, oc], lhsT=g2b[:, hc], rhs=sgx[:], start=True, stop=True)
                    for e2 in range(2):
                        h = hp * 2 + e2
                        oc = slice(e2 * TGR, (e2 + 1) * TGR)
                        I(P, 'act', 'activation', out=sg[:, h, :], in_=p1[0:64, oc], func=AF.Sigmoid, bias=vec[:, 0, h:h + 1], wsub=h)
                        I(P, 'act', 'activation', out=a_[:, h, :], in_=p2[0:64, oc], func=AF.Sigmoid, bias=vec[:, 1, h:h + 1], wsub=h)
                    I(P, 'act', 'activation', out=gT[:, hp * 2:hp * 2 + 2, :], in_=p3[0:64, :].rearrange("p (e t) -> p e t", e=2), func=AF.Copy, wsub=hp)
                for h in range(8):
                    I(P, 'dve', 'tensor_scalar', out=kkr[:, h, :], in0=kT[:, h, :], scalar1=vec[:, 2, h:h + 1], scalar2=None, op0=ALU.mult, wsub=h)
                I(P, 'dve', 'tensor_tensor', out=sq[:], in0=kkr[:], in1=kkr[:], op=ALU.mult)
                for hp in range(4):
                    p1 = nxt()
                    for e2 in range(2):
                        h = hp * 2 + e2
                        I(P, 'pe', 'matmul', out=p1[0:64, e2 * TGR:(e2 + 1) * TGR], lhsT=ones[0:64, 0:64], rhs=sq[:, h, :], start=True, stop=True)
                    I(P, 'act', 'activation', out=nrm[:, hp * 2:hp * 2 + 2, :], in_=p1[0:64, :].rearrange("p (e t) -> p e t", e=2), func=AF.Sqrt, wsub=hp)
                I(P, 'dve', 'tensor_scalar', out=nrm[:], in0=nrm[:], scalar1=1e-12, scalar2=None, op0=ALU.max)
                I(P, 'dve', 'reciprocal', out=nrm[:], in_=nrm[:])
                I(P, 'dve', 'tensor_tensor', out=kk[:], in0=kkr[:], in1=nrm[:], op=ALU.mult)
                for h in range(8):
                    I(P, 'dve', 'tensor_scalar', out=t1[:, h, :], in0=a_[:, h, :], scalar1=vec[:, 3, h:h + 1], scalar2=omk[:, h:h + 1],
                      op0=ALU.mult, op1=ALU.add, wsub=h)
                I(P, 'dve', 'tensor_tensor', out=kmod[:], in0=kT[:], in1=t1[:], op=ALU.mult)
                I(P, 'dve', 'tensor_copy', out=kmb[:], in_=kmod[:])
                I(P, 'dve', 'tensor_tensor', out=beta[:], in0=kk[:], in1=a_[:], op=ALU.mult)
                for h in range(8):
                    I(P, 'dve', 'tensor_tensor_scan', out=cs[:, h, :], data0=rst[:], data1=sg[:, h, :], initial=0.0, op0=ALU.mult, op1=ALU.add,
                      wsub=h)
                I(P, 'act', 'activation', out=Ea[:], in_=cs[:], func=AF.Exp, scale=c0)
                for h in range(8):
                    I(P, 'dve', 'tensor_tensor', out=AR[:, h, :, 1, :], in0=v3(rT, h), in1=v3(Ea, h), op=ALU.mult, wsub=('r', h))
                I(P, 'dve', 'tensor_copy', out=GCt[:], in_=Ea[:].rearrange("p h (n c) -> p h n c", c=64)[:, :, :, 63])
                I(P, 'act', 'activation', out=Eb[:], in_=cs[:], func=AF.Exp, scale=-c0)
                for h in range(8):
                    I(P, 'dve', 'tensor_tensor', out=BK[:, h, :, 0, :], in0=v3(beta, h), in1=v3(Eb, h), op=ALU.mult, wsub=('b', h))
                    I(P, 'dve', 'tensor_tensor', out=BK[:, h, :, 1, :], in0=v3(kmod, h), in1=v3(Eb, h), op=ALU.mult, wsub=('k', h))
                I(P, 'dve', 'tensor_tensor', out=Ea[:], in0=cs[:], in1=sg[:], op=ALU.subtract)
                I(P, 'act', 'activation', out=Ea[:], in_=Ea[:], func=AF.Exp, scale=c0)
                for h in range(8):
                    I(P, 'dve', 'scalar_tensor_tensor', out=AR[:, h, :, 0, :], in0=v3(kk, h), scalar=-1.0, in1=v3(Ea, h), op0=ALU.mult, op1=ALU.mult,
                      wsub=('a', h))
                csv = cs[:].rearrange("p h (n c) -> p (h n) c", c=64)
                I(P, 'dve', 'tensor_tensor', out=Eb[:].rearrange("p h (n c) -> p (h n) c", c=64), in0=csv[:, :, 63:64].to_broadcast([64, 8 * NCH, 64]),
                  in1=csv, op=ALU.subtract)
                I(P, 'act', 'activation', out=Eb[:], in_=Eb[:], func=AF.Exp, scale=c0)
                I(P, 'dve', 'tensor_tensor', out=bp[:], in0=beta[:], in1=Eb[:], op=ALU.mult)
                I(P, 'dve', 'tensor_tensor', out=kp[:], in0=kmod[:], in1=Eb[:], op=ALU.mult)
                if RSTOP <= 1:
                    continue
                tcnt = 0
                for Xf, Xt in ((bp, bpTM), (kp, kpTM), (vT, vTM), (rT, rTM), (kmb, kmTM), (gT, gTM)):
                    for n in range(NCH):
                        ps = nxt()
                        pb_ = ps[:].bitcast(BF16)
                        for h in range(8):
                            I(P, 'pe', 'transpose', out=pb_[0:64, h * 64:(h + 1) * 64], in_=Xf[:, h, n * 64:(n + 1) * 64], identity=ident[0:64, 0:64])
                        tcnt += 1
                        if tcnt % 2:
                            I(P, 'act', 'activation', out=Xt[:, n, :], in_=pb_[0:64, 0:512], func=AF.Copy, wsub=n)
                        else:
                            I(P, 'dve', 'tensor_copy', out=Xt[:, n, :], in_=pb_[0:64, 0:512], wsub=n)
                if RSTOP <= 2:
                    continue
                Pcs = {}
                PTcs = {}
                for n in range(NCH):
                    nm1 = NM1[n]; nm2 = NM2[n]; ntt = NTt[n]; x_ = X[n]
                    for hq in range(2):
                        pa, pb2, pc = nxt(), nxt(), nxt()
                        for hh in range(4):
                            h = hq * 4 + hh
                            arv = AR[:, h, n, :, :].rearrange("p a c -> p (a c)")
                            I(P, 'pe', 'matmul', out=pa[0:64, hh * 128:(hh + 1) * 128], lhsT=BK[:, h, n, 0, :], rhs=arv, start=True, stop=True)
                            I(P, 'pe', 'matmul', out=pb2[0:64, hh * 128:(hh + 1) * 128], lhsT=BK[:, h, n, 1, :], rhs=arv, start=True, stop=True)
                            I(P, 'pe', 'matmul', out=pc[0:64, hh * 64:(hh + 1) * 64], lhsT=AR[:, h, n, 0, :], rhs=BK[:, h, n, 0, :], start=True, stop=True)
                        m1b = mask1[:].unsqueeze(1).to_broadcast([64, 4, 128])
                        I(P, 'dve', 'tensor_tensor', out=nm1[:, hq * 4:(hq + 1) * 4, :], in0=pa[0:64, :].rearrange("p (h c) -> p h c", c=128), in1=m1b,
                          op=ALU.mult, wsub=hq)
                        I(P, 'dve', 'tensor_tensor', out=nm2[:, hq * 4:(hq + 1) * 4, :], in0=pb2[0:64, :].rearrange("p (h c) -> p h c", c=128), in1=m1b,
                          op=ALU.mult, wsub=hq)
                        I(P, 'dve', 'tensor_tensor', out=ntt[:, hq * 4:(hq + 1) * 4, :], in0=pc[0:64, 0:256].rearrange("p (h c) -> p h c", c=64),
                          in1=mask3[:].unsqueeze(1).to_broadcast([64, 4, 64]), op=ALU.mult, wsub=hq)
                    I(P, 'dve', 'tensor_tensor', out=x_[:], in0=nm1[:, :, 0:64], in1=ident[0:64, 0:64].unsqueeze(1).to_broadcast([64, 8, 64]), op=ALU.add)
                    Pcs[n] = (lambda u, nm1=nm1: nm1[:, u, 0:64])
                    PTcs[n] = (lambda u, ntt=ntt: ntt[:, u, :])
                for k in range(6):
                    for n in range(NCH):
                        x_ = X[n]; Pc = Pcs[n]; PTc = PTcs[n]
                        if k >= 1:
                            pX = nxt()
                            for u in range(8):
                                I(P, 'pe', 'matmul', out=pX[0:64, u * 64:(u + 1) * 64], lhsT=PTc(u), rhs=x_[:, u, :], start=True, stop=True)
                        if k < 5:
                            pP, pPT = nxt(), nxt()
                            for u in range(8):
                                I(P, 'pe', 'matmul', out=pP[0:64, u * 64:(u + 1) * 64], lhsT=PTc(u), rhs=Pc(u), start=True, stop=True)
                                I(P, 'pe', 'matmul', out=pPT[0:64, u * 64:(u + 1) * 64], lhsT=Pc(u), rhs=PTc(u), start=True, stop=True)
                        if k >= 1:
                            I(P, 'dve', 'tensor_tensor', out=x_[:], in0=x_[:], in1=pX[0:64, :].rearrange("p (u c) -> p u c", c=64), op=ALU.add)
                        if k < 5:
                            pn = Pm[n][k % 2]; ptn = PTm[n][k % 2]
                            I(P, 'act', 'activation', out=pn[:], in_=pP[0:64, :].rearrange("p (u c) -> p u c", c=64), func=AF.Copy)
                            I(P, 'dve', 'tensor_copy', out=ptn[:], in_=pPT[0:64, :].rearrange("p (u c) -> p u c", c=64))
                            Pcs[n] = (lambda u, pn=pn: pn[:, u, :])
                            PTcs[n] = (lambda u, ptn=ptn: ptn[:, u, :])
                for n in range(NCH):
                    nm1 = NM1[n]; nm2 = NM2[n]; x_ = X[n]
                    if RSTOP <= 3:
                        continue
                    pY = nxt()
                    for h in range(8):
                        hc = slice(h * 64, (h + 1) * 64)
                        I(P, 'pe', 'matmul', out=pY[0:64, hc], lhsT=AR[:, h, n, 0, :], rhs=hbf[:, h, :], start=True, stop=False)
                        I(P, 'pe', 'matmul', out=pY[0:64, hc], lhsT=nm2[:, h, 0:64], rhs=vTM[:, n, hc], start=False, stop=True)
                    I(P, 'act', 'activation', out=Ysb[:], in_=pY[0:64, :], func=AF.Copy)
                    pU = nxt()
                    for h in range(8):
                        hc = slice(h * 64, (h + 1) * 64)
                        I(P, 'pe', 'matmul', out=pU[0:64, hc], lhsT=x_[:, h, :], rhs=Ysb[:, hc], start=True, stop=True)
                    I(P, 'dve', 'tensor_copy', out=Usb[:], in_=pU[0:64, :])
                    pO = nxt()
                    for h in range(8):
                        hc = slice(h * 64, (h + 1) * 64)
                        I(P, 'pe', 'matmul', out=pO[0:64, hc], lhsT=AR[:, h, n, 1, :], rhs=hbf[:, h, :], start=True, stop=False)
                        I(P, 'pe', 'matmul', out=pO[0:64, hc], lhsT=nm1[:, h, 64:128], rhs=Usb[:, hc], start=False, stop=False)
                        I(P, 'pe', 'matmul', out=pO[0:64, hc], lhsT=nm2[:, h, 64:128], rhs=vTM[:, n, hc], start=False, stop=True)
                    I(P, 'act', 'activation', out=OTM[:, n, :], in_=pO[0:64, :], func=AF.Copy, wsub=n)
                    pH = nxt()
                    for h in range(8):
                        hc = slice(h * 64, (h + 1) * 64)
                        I(P, 'pe', 'matmul', out=pH[0:64, hc], lhsT=bpTM[:, n, hc], rhs=Usb[:, hc], start=True, stop=False)
                        I(P, 'pe', 'matmul', out=pH[0:64, hc], lhsT=kpTM[:, n, hc], rhs=vTM[:, n, hc], start=False, stop=True)
                    I(P, 'dve', 'tensor_tensor', out=hs[:], in0=hst[:], in1=GCt[:, :, n:n + 1].to_broadcast([64, 8, 64]), op=ALU.mult)
                    I(P, 'dve', 'tensor_tensor', out=hst[:], in0=hs[:], in1=pH[0:64, :].rearrange("p (h c) -> p h c", c=64), op=ALU.add)
                    I(P, 'dve', 'tensor_copy', out=hbf[:], in_=hst[:])
                if RSTOP <= 4:
                    continue
                O3 = OTM[:].rearrange("p n (h d) -> p (n h) d", d=64)
                OcA = Ea[:].rearrange("p h t -> p (h t)").rearrange("p (n c) -> p n c", c=512)
                sqA = Eb[:].rearrange("p h t -> p (h t)").rearrange("p (n c) -> p n c", c=512)
                Oc3 = OcA.rearrange("p n (h d) -> p (n h) d", d=64)
                sq3 = sqA.rearrange("p n (h d) -> p (n h) d", d=64)
                bc = lambda t: t[:, :].unsqueeze(2).to_broadcast([64, NU, 64])
                I(P, 'dve', 'tensor_reduce', out=s1[:], in_=O3, axis=AX.X, op=ALU.add)
                I(P, 'dve', 'tensor_scalar', out=s1[:], in0=s1[:], scalar1=1.0 / 64, scalar2=None, op0=ALU.mult)
                I(P, 'dve', 'tensor_tensor', out=Oc3, in0=O3, in1=bc(s1), op=ALU.subtract)
                I(P, 'dve', 'tensor_tensor', out=sqA, in0=OcA, in1=OcA, op=ALU.mult)
                I(P, 'dve', 'tensor_reduce', out=s2[:], in_=sq3, axis=AX.X, op=ALU.add)
                I(P, 'act', 'activation', out=s2[:], in_=s2[:], func=AF.Sqrt, scale=1.0 / 64, bias=gneps[:, 0:1])
                I(P, 'dve', 'reciprocal', out=s2[:], in_=s2[:])
                I(P, 'dve', 'tensor_tensor', out=Oc3, in0=Oc3, in1=bc(s2), op=ALU.mult)
                I(P, 'dve', 'tensor_tensor', out=OcA, in0=OcA, in1=lnw[:].unsqueeze(1).to_broadcast([64, NCH, 512]), op=ALU.mult)
                I(P, 'dve', 'tensor_tensor', out=OcA, in0=OcA, in1=lnb[:].unsqueeze(1).to_broadcast([64, NCH, 512]), op=ALU.add)
                I(P, 'dve', 'tensor_tensor', out=sqA, in0=rTM[:], in1=kmTM[:], op=ALU.mult)
                I(P, 'dve', 'tensor_tensor', out=sqA, in0=sqA, in1=rkr[:].unsqueeze(1).to_broadcast([64, NCH, 512]), op=ALU.mult)
                I(P, 'dve', 'tensor_reduce', out=bs[:], in_=sq3, axis=AX.X, op=ALU.add)
                I(P, 'dve', 'tensor_tensor', out=sq3, in0=vTM[:].rearrange("p n (h d) -> p (n h) d", d=64), in1=bc(bs), op=ALU.mult)
                I(P, 'dve', 'tensor_tensor', out=OcA, in0=OcA, in1=sqA, op=ALU.add)
                I(P, 'dve', 'tensor_tensor', out=ob[:], in0=OcA, in1=gTM[:], op=ALU.mult)
                oT = obT[g % 2]
                for n in range(NCH):
                    ps = nxt()
                    pb_ = ps[:].bitcast(BF16)
                    for h in range(8):
                        I(P, 'pe', 'transpose', out=pb_[0:64, h * 64:(h + 1) * 64], in_=ob[:, n, h * 64:(h + 1) * 64], identity=ident[0:64, 0:64])
                    I(P, 'act', 'activation', out=oT[:, :, n * 64:(n + 1) * 64], in_=pb_[0:64, 0:512].rearrange("p (h c) -> p h c", c=64), func=AF.Copy, wsub=n)
                P.dma('sp', mixedT[512:1024, sl].rearrange("(h p) t -> p h t", p=64), oT[:], reads=[oT], writes=[(mixedT, ('ob', b, g))], ctr=oT)


A_W, KVL, IH, ID_ = 512, 128, 8, 64
N_IN_A = 1224


def perm_cols():
    q = list(range(0, 512))
    clat = list(range(512, 640))
    qidx = list(range(640, 1152))
    kidx = list(range(1152, 1216))
    B0 = N_IN_A
    r = list(range(B0, B0 + 512))
    k = list(range(B0 + 512, B0 + 1024))
    v = list(range(B0 + 1024, B0 + 1536))
    xwxa = list(range(B0 + 1536, B0 + 1664))
    xg = list(range(B0 + 1664, B0 + 1792))
    return q + qidx + clat + kidx + kidx + r + k + v + xwxa + xg


def host_consts(S):
    f = np.float32
    t = np.arange(S)
    slopes = 2.0 ** (-np.arange(1, 9, dtype=np.float64))
    kaug = np.stack([np.ones(S), np.ones(S), 64.0 * (t // 64), (t % 64) * 1.0]).astype(f)
    qaug = np.stack([np.stack([-s * 64.0 * (t // 64), -s * (t % 64), s * np.ones(S), s * np.ones(S)]) for s in slopes]).astype(f)
    ss, tt = np.meshgrid(np.arange(128), np.arange(128), indexing='ij')
    same = (ss // 64) == (tt // 64)
    ccorr = np.stack([np.where(same, -2.0 * s * np.maximum(ss - tt, 0), 0.0) for s in slopes], axis=1).astype(f)
    tq, sk = np.meshgrid(np.arange(128), np.arange(128), indexing='ij')
    dmask = np.where(sk < (tq // 64 + 1) * 64, 0.0, -1e30).astype(f)
    sel65 = np.zeros((65, 64), f)
    sel65[64, :] = 1.0
    return dict(kaug=kaug, qaug=qaug, ccorr=ccorr, dmask=dmask, sel65=sel65,
                ident=np.eye(128, dtype=f), ones128=np.ones((128, 128), f))


def rwkv_consts():
    f = np.float32
    i = np.arange(128)
    bones = ((i[:, None] // 64) == (i[None, :] // 64)).astype(f)
    s_, t_ = np.meshgrid(np.arange(64), np.arange(64), indexing='ij')
    mask1 = np.concatenate([(s_ < t_), (s_ <= t_)], axis=1).astype(f)
    mask3 = (t_ < s_).astype(f)
    rst = np.ones((128, 256), f)
    rst[:, 0::64] = 0.0
    return dict(bones=bones, mask1=mask1, mask3=mask3, rst=rst)


def rwkv_params(w0, w2, a0, a2, g2, k_k, k_a, r_k, ln_w, ln_b):
    f = np.float32
    col = lambda v: np.asarray(v, f).reshape(8, 64).T
    vec8 = np.ascontiguousarray(np.stack([col(w0), col(a0), col(k_k), col(k_a), col(np.asarray(r_k, f).reshape(512))], axis=1))
    return dict(w2=np.ascontiguousarray(np.asarray(w2, f)), a2=np.ascontiguousarray(np.asarray(a2, f)), g2=np.ascontiguousarray(np.asarray(g2, f)),
                vec8=vec8, ln_w=np.ascontiguousarray(np.asarray(ln_w, f).reshape(512)), ln_b=np.ascontiguousarray(np.asarray(ln_b, f).reshape(512)),
                r_k1=np.ascontiguousarray(np.asarray(r_k, f).reshape(512)))


NCORES = 8
NB_ = 4
S_ = 2048


def build_nc(shapes):
    NB, S = NB_, S_
    NT = NB * S
    nc = bass.Bass("TRN2", target_bir_lowering=False)
    A = {k: nc.dram_tensor(k, list(shp), F32, kind="ExternalInput").ap() for k, shp in shapes.items()}
    out = nc.dram_tensor("out", [NT, D], F32, kind="ExternalOutput").ap()
    mod_d = nc.dram_tensor("mod_d", [NB, 6, D], F32, kind="Internal").ap()
    projT = nc.dram_tensor("projT", [NPROJ, NT], BF16, kind="Internal").ap()
    widxT = nc.dram_tensor("widxT", [8, NT], F32, kind="Internal").ap()
    mixedT = nc.dram_tensor("mixedT", [1024, NT], BF16, kind="Internal").ap()
    with contextlib.ExitStack() as stack:
        P = Prog(nc, stack)
        phase_mod(P, nc, A['cT'], A['w_ada'], A['b_ada'], A['g_mix'], A['g_ffn'], mod_d, NB)
        P.barrier()
        phase_inproj(P, nc, A['x'], projT, widxT, A['w_inp'], A['w_widx'], A['mu'], mod_d, A['ident'], NB, S)
        P.barrier()
        phase_dsa(P, nc, projT, widxT, mixedT, A, NB, S, min(256, S // 4))
        P.barrier()
        phase_rwkv(P, nc, projT, mixedT, A, NB, S)
        P.barrier()
        phase_outproj(P, nc, A['x'], out, mixedT, A['w_out'], mod_d, NB, S)
        P.barrier()
        outs = phase_ffn(P, nc, out, out, A['w_ff1'], A['w_ff2'], mod_d, A['ident'], NB, S)
        P.emit(final_wait_ops=outs)
    return nc


def kernel(x, c, w_ada, b_ada, g_mix, g_ffn, w_in, g_q, g_k, g_kv, w_uk, w_uv, mu_shift, w0, w2, a0, a2, g2,
           k_k, k_a, r_k, ln_w, ln_b, w_out, w_ff1, w_ff2):
    f = np.float32
    x = np.asarray(x, f); c = np.asarray(c, f)
    B, S, Dm = x.shape
    NB = B // NCORES
    pc = perm_cols()
    hc = host_consts(S)
    w_in0 = np.asarray(w_in, f)[0]
    mu_full = np.zeros(w_in0.shape[1], f)
    mu_full[N_IN_A:] = np.asarray(mu_shift, f)[0]
    mu_perm = mu_full[pc]
    l0 = lambda a: np.asarray(a, f)[0]
    shared = dict(
        w_ada=np.ascontiguousarray(l0(w_ada)), b_ada=np.ascontiguousarray(l0(b_ada)),
        g_mix=np.ascontiguousarray(l0(g_mix)), g_ffn=np.ascontiguousarray(l0(g_ffn)),
        w_inp=np.ascontiguousarray(w_in0[:, pc]), w_widx=np.ascontiguousarray(w_in0[:, 1216:1224]),
        mu=np.ascontiguousarray(mu_perm[1280:].reshape(14, 128).T),
        ident=hc['ident'],
        w_uk=np.ascontiguousarray(l0(w_uk).reshape(128, 512)), w_uv=np.ascontiguousarray(l0(w_uv).reshape(128, 512)),
        g_q=np.ascontiguousarray(l0(g_q).reshape(64, 1)), g_k=np.ascontiguousarray(l0(g_k).reshape(64, 1)),
        g_kv=np.ascontiguousarray(l0(g_kv).reshape(128, 1)),
        kaug=hc['kaug'], qaug=hc['qaug'], ccorr=hc['ccorr'], dmask=hc['dmask'], sel65=hc['sel65'], ones128=hc['ones128'],
        w_out=np.ascontiguousarray(l0(w_out)), w_ff1=np.ascontiguousarray(l0(w_ff1)), w_ff2=np.ascontiguousarray(l0(w_ff2)),
        **rwkv_consts(),
        **rwkv_params(l0(w0), l0(w2), l0(a0), l0(a2), l0(g2), l0(k_k), l0(k_a), l0(r_k), l0(ln_w), l0(ln_b)),
    )
    in_maps = []
    for i in range(NCORES):
        m = dict(shared)
        m['x'] = np.ascontiguousarray(x[i * NB:(i + 1) * NB].reshape(NB * S, Dm))
        ci = c[i * NB:(i + 1) * NB]
        m['cT'] = np.ascontiguousarray(ci.T.reshape(8, 128, NB).transpose(1, 0, 2))
        in_maps.append(m)
    nc = build_nc({k: v.shape for k, v in in_maps[0].items()})
    res = run_bass_kernel_spmd(nc, in_maps, core_ids=list(range(NCORES)))
    outs = [np.asarray(r["out"], f).reshape(NB, S, Dm) for r in res.results]
    return np.concatenate(outs, axis=0)
```

```python
import contextlib
import os
import numpy as np
import concourse.bass as bass
import concourse.mybir as mybir
from concourse.bass_utils import run_bass_kernel_spmd


F32 = mybir.dt.float32
BF16 = mybir.dt.bfloat16
AF = mybir.ActivationFunctionType
ALU = mybir.AluOpType
AX = mybir.AxisListType

EPOCH = 16000
ENGS = ['pe', 'dve', 'act', 'pool', 'sp']


class Op:
    __slots__ = ('eng', 'fn', 'deps', 'pos', 'signal', 'seq', 'is_dma', 'ctr', 'val', 'waits', 'tag')


class Counter:
    def __init__(self, name):
        self.name = name
        self.sems = []
        self.count = 0

    def need(self, prog, v):
        ep = (v - 1) // EPOCH
        while len(self.sems) <= ep:
            self.sems.append(prog.new_sem())

    def sem_for(self, v):
        ep = (v - 1) // EPOCH
        return self.sems[ep], v - ep * EPOCH


class Prog:
    def __init__(self, nc, stack):
        self.nc = nc
        self.stack = stack
        self.eng_ops = {e: [] for e in ENGS}
        self.res = {}
        self.engctr = {e: Counter(e) for e in ENGS}
        self.dmactr = {}
        self.nsem = 0
        self.ntile = 0
        self.all_dma = []
        self.keep = []
        self._bar_t = {e: self.sb([1, 8], F32, name=f"bar{e}") for e in ENGS}
        self._bar_ps = self.ps([1, 8], F32, name="barps")
        self._bar_tok = {e: f"__bar_{e}" for e in ENGS}
        self._bar_init = False
        self._bar_last = {}

    def new_sem(self):
        self.nsem += 1
        return self.stack.enter_context(self.nc.semaphore(f"sm{self.nsem}"))

    def sb(self, shape, dtype, name=None, stack=None):
        self.ntile += 1
        nm = f"{name or 't'}_{self.ntile}"
        t = (stack or self.stack).enter_context(self.nc.sbuf_tensor(nm, list(shape), dtype))
        self.keep.append(t)
        return t

    def ps(self, shape, dtype=F32, name=None, stack=None):
        self.ntile += 1
        nm = f"{name or 'p'}_{self.ntile}"
        t = (stack or self.stack).enter_context(self.nc.psum_tensor(nm, list(shape), dtype))
        self.keep.append(t)
        return t

    @staticmethod
    def _key(r):
        if isinstance(r, tuple):
            return id(r[0]), r[1]
        return id(r), None

    def _deps(self, op, reads, writes):
        deps = []
        for r in reads:
            tk, sk = self._key(r)
            ent = self.res.setdefault(tk, {})
            for k2, e2 in ent.items():
                if sk is None or k2 is None or k2 == sk:
                    if e2[0] is not None:
                        deps.append((e2[0], 'raw'))
            e = ent.setdefault(sk, [None, []])
            if not op.is_dma:
                e[1] = [o for o in e[1] if o.is_dma or o.eng != op.eng]
            e[1].append(op)
        for w in writes:
            tk, sk = self._key(w)
            ent = self.res.setdefault(tk, {})
            for k2, e2 in ent.items():
                if sk is None or k2 is None or k2 == sk:
                    if e2[0] is not None:
                        deps.append((e2[0], 'waw'))
                    for o in e2[1]:
                        if o is not op:
                            deps.append((o, 'war'))
            if sk is None:
                ent.clear()
            ent[sk] = [op, []]
        return deps

    def op(self, eng, fn, reads=(), writes=(), tag=None):
        o = Op()
        o.eng = eng
        o.fn = fn
        o.is_dma = False
        o.signal = False
        o.seq = 0
        o.ctr = None
        o.val = 0
        o.tag = tag
        o.deps = self._deps(o, reads, writes)
        self.eng_ops[eng].append(o)
        o.pos = len(self.eng_ops[eng])
        return o

    def dma(self, q, out, in_, reads=(), writes=(), ctr=None, tag=None, **kw):
        o = Op()
        o.eng = q
        o.is_dma = True
        o.signal = True
        o.seq = 0
        o.tag = tag
        o.fn = lambda e: e.dma_start(out=out, in_=in_, **kw)
        ck = self._key(ctr)
        c = self.dmactr.get(ck)
        if c is None:
            c = self.dmactr[ck] = Counter(f"d{len(self.dmactr)}")
        c.count += 16
        o.ctr = c
        o.val = c.count
        o.deps = self._deps(o, reads, writes)
        self.eng_ops[q].append(o)
        o.pos = len(self.eng_ops[q])
        self.all_dma.append(o)
        return o

    def barrier(self):
        if not hasattr(self, '_bar_t'):
            self._bar_t = {e: self.sb([1, 8], F32, name=f"bar{e}") for e in ENGS}
            self._bar_ps = self.ps([1, 8], F32, name="barps")
            self._bar_tok = {e: f"__bar_{e}" for e in ENGS}
            self.keep.append(self._bar_tok)
        bt = self._bar_t
        if not self._bar_init:
            self._bar_init = True
            for e_ in ('pe', 'act'):
                self.op('dve', (lambda eng, e_=e_: eng.memset(bt[e_][:], 0.0)), writes=[bt[e_]])
        dmas = [d for d in self.all_dma]
        self.all_dma = []
        first = []
        for e in ENGS:
            if e == 'pe':
                fn = lambda eng: eng.matmul(self._bar_ps[0:1, 0:1], bt['pe'][0:1, 0:1], bt['pe'][0:1, 1:2], start=True, stop=True)
            elif e == 'sp':
                fn = lambda eng: eng.nop()
            elif e == 'act':
                fn = lambda eng: eng.activation(out=bt['act'][0:1, 2:3], in_=bt['act'][0:1, 3:4], func=AF.Copy)
            elif e == 'dve':
                fn = lambda eng: eng.memset(bt['dve'][0:1, 2:3], 0.0)
            else:
                fn = lambda eng: eng.memset(bt['pool'][0:1, 2:3], 0.0)
            o = self.op(e, fn, reads=([bt[e]] if e in ('pe', 'act') else []), writes=[(self._bar_tok[e], 0)])
            if e in self._bar_last:
                o.deps.append((self._bar_last[e], 'waw'))
            self._bar_last[e] = o
            if e == 'sp':
                o.deps += [(d, 'raw') for d in dmas]
            first.append(o)
        for e in ENGS:
            if e == 'pe':
                fn = lambda eng: eng.matmul(self._bar_ps[0:1, 4:5], bt['pe'][0:1, 0:1], bt['pe'][0:1, 1:2], start=True, stop=True)
            elif e == 'sp':
                fn = lambda eng: eng.nop()
            elif e == 'act':
                fn = lambda eng: eng.activation(out=bt['act'][0:1, 4:5], in_=bt['act'][0:1, 3:4], func=AF.Copy)
            elif e == 'dve':
                fn = lambda eng: eng.memset(bt['dve'][0:1, 4:5], 0.0)
            else:
                fn = lambda eng: eng.memset(bt['pool'][0:1, 4:5], 0.0)
            o = self.op(e, fn, reads=([bt[e]] if e in ('pe', 'act') else []))
            o.deps += [(f, 'raw') for f in first if f.eng != e]
            o.deps.append((self._bar_last[e], 'waw'))
            self._bar_last[e] = o
        self.res.clear()

    def init_bar(self):
        self.barrier_ready = True

    def emit(self, final_wait_ops=()):
        fin = self.op('sp', lambda eng: eng.nop())
        fin.deps += [(d, 'raw') for d in final_wait_ops]
        for e in ENGS:
            waited = {}
            for op in self.eng_ops[e]:
                op.waits = []
                best = {}
                for d, kind in op.deps:
                    if d.is_dma:
                        key = ('c', id(d.ctr))
                        v = d.val
                    else:
                        if d.eng == e and kind != 'raw' and e == 'pe':
                            continue
                        key = ('e', d.eng)
                        v = d.pos
                    if waited.get(key, 0) >= v:
                        continue
                    if key not in best or best[key][0] < v:
                        best[key] = (v, d)
                for key, (v, d) in best.items():
                    waited[key] = v
                    d.signal = True
                    op.waits.append(d)
        for e in ENGS:
            seq = 0
            for op in self.eng_ops[e]:
                if op.is_dma:
                    op.ctr.need(self, op.val)
                elif op.signal:
                    seq += 1
                    op.seq = seq
                    self.engctr[e].need(self, seq)
        nc = self.nc

        def run(e, eng):
            for op in self.eng_ops[e]:
                for d in op.waits:
                    if d.is_dma:
                        sem, v = d.ctr.sem_for(d.val)
                    else:
                        sem, v = self.engctr[d.eng].sem_for(d.seq)
                    eng.wait_ge(sem, v)
                ins = op.fn(eng)
                if op.is_dma:
                    sem, _ = op.ctr.sem_for(op.val)
                    ins.then_inc(sem, 16)
                elif op.signal:
                    sem, _ = self.engctr[e].sem_for(op.seq)
                    ins.then_inc(sem, 1)

        with nc.Block() as block:
            @block.tensor
            def _(eng):
                run('pe', eng)

            @block.vector
            def _(eng):
                run('dve', eng)

            @block.scalar
            def _(eng):
                run('act', eng)

            @block.gpsimd
            def _(eng):
                run('pool', eng)

            @block.sync
            def _(eng):
                run('sp', eng)

    def stats(self):
        return {e: len(v) for e, v in self.eng_ops.items()}, self.nsem


def _apname(a):
    try:
        return a.name
    except Exception:
        return a.tensor.name


def I(P, eng, meth, wsub=None, rsub=None, extra_reads=(), extra_writes=(), **kw):
    reads, writes = list(extra_reads), list(extra_writes)
    for k, v in kw.items():
        if isinstance(v, bass.AP):
            nm = _apname(v)
            if k in ('out', 'accum_out'):
                writes.append((nm, wsub.get(nm) if isinstance(wsub, dict) else wsub))
            else:
                reads.append((nm, rsub.get(nm) if isinstance(rsub, dict) else rsub))
    return P.op(eng, (lambda e: getattr(e, meth)(**kw)), reads=reads, writes=writes)


def _key2(r):
    if isinstance(r, tuple):
        a, s = r
    else:
        a, s = r, None
    if isinstance(a, str):
        return a, s
    try:
        return a.name, s
    except Exception:
        return a.tensor.name, s


Prog._key = staticmethod(_key2)


D = 1024
DFF = 4096
RMS_EPS = 1e-6


def bcast_rows(ap1d, n):
    return ap1d.partition_broadcast(n)


def phase_mod(P, nc, cT, w_ada, b_ada, g_mix, g_ffn, mod_d, NB):
    with contextlib.ExitStack() as st:
        ct = P.sb([128, 8, NB], F32, 'ct', st)
        sil = P.sb([128, 8, NB], F32, 'sil', st)
        ones = P.sb([1, NB], F32, 'ones', st)
        mod = P.sb([NB, 6 * D], F32, 'mod', st)
        gb = P.sb([NB, 2, D], F32, 'gb', st)
        wa = [P.sb([128, 8, 512], F32, 'wa', st) for _ in range(2)]
        ba = [P.sb([1, 512], F32, 'ba', st) for _ in range(2)]
        pm = [P.ps([128, 512], F32, 'pm', st) for _ in range(2)]
        P.dma('sp', ct[:], cT, writes=[ct], ctr=ct)
        P.op('act', lambda e: e.activation(out=sil[:], in_=ct[:], func=AF.Silu), reads=[ct], writes=[sil])
        P.op('dve', lambda e: e.memset(ones[:], 1.0), writes=[ones])
        P.dma('sp', gb[:, 0, :], g_mix.partition_broadcast(NB), writes=[(gb, 0)], ctr=gb)
        P.dma('sp', gb[:, 1, :], g_ffn.partition_broadcast(NB), writes=[(gb, 1)], ctr=gb)
        for cg in range(12):
            w = wa[cg % 2]
            bb = ba[cg % 2]
            pp = pm[cg % 2]
            P.dma('sp', w[:], w_ada[:, cg * 512:(cg + 1) * 512].rearrange("(k p) e -> p k e", p=128),
                  writes=[w], ctr=w)
            P.dma('sp', bb[:], b_ada[cg * 512:(cg + 1) * 512].partition_broadcast(1), writes=[bb], ctr=bb)
            for k in range(8):
                P.op('pe', (lambda e, k=k, w=w, pp=pp: e.matmul(pp[0:NB, :], sil[:, k, :], w[:, k, :], start=(k == 0), stop=False)),
                     reads=[sil, w], writes=[pp])
            P.op('pe', (lambda e, bb=bb, pp=pp: e.matmul(pp[0:NB, :], ones[:, :], bb[:, :], start=False, stop=True)),
                 reads=[ones, bb], writes=[pp])
            P.op('act', (lambda e, pp=pp, cg=cg: e.activation(out=mod[:, cg * 512:(cg + 1) * 512], in_=pp[0:NB, :], func=AF.Copy)),
                 reads=[pp], writes=[(mod, cg)])
        for r, gi in ((1, 0), (4, 1)):
            P.op('dve', (lambda e, r=r, gi=gi: e.scalar_tensor_tensor(out=mod[:, r * D:(r + 1) * D], in0=mod[:, r * D:(r + 1) * D],
                                                                     scalar=1.0, in1=gb[:, gi, :], op0=ALU.add, op1=ALU.mult)),
                 reads=[mod, gb], writes=[mod])
        o = P.dma('sp', mod_d.rearrange("b r d -> b (r d)"), mod[:], reads=[mod], writes=[mod_d], ctr=mod)
    return o


def phase_ffn(P, nc, xin, xout, w_ff1, w_ff2, mod_d, ident_d, NB, S):
    NT = NB * S
    TG = 256
    outs = []
    with contextlib.ExitStack() as st:
        w1 = P.sb([128, 8, DFF], BF16, 'w1', st)
        w2 = P.sb([128, 32, D], BF16, 'w2', st)
        ident = P.sb([128, 128], BF16, 'ident', st)
        sh2f = P.sb([128, 8, NB], F32, 'sh2f', st)
        sh2b = P.sb([128, 8, NB], BF16, 'sh2b', st)
        bias1 = P.sb([128, 32, NB], F32, 'bias1', st)
        G2bc = P.sb([128, D], F32, 'G2bc', st)
        gt2bc = P.sb([128, D], F32, 'gt2bc', st)
        xt = [P.sb([128, D], F32, 'xt', st) for _ in range(4)]
        xn = [P.sb([128, D], BF16, 'xn', st) for _ in range(2)]
        junk = P.sb([128, D], BF16, 'junk', st)
        epsc = P.sb([128, 1], F32, 'epsc', st)
        P.op('dve', lambda e: e.memset(epsc[:], RMS_EPS), writes=[epsc])
        ss = [P.sb([128, 2], F32, 'ss', st) for _ in range(2)]
        h2T = [P.sb([128, 8, TG], BF16, 'h2T', st) for _ in range(2)]
        u2T = P.sb([128, 32, TG], BF16, 'u2T', st)
        rr = [P.sb([128, TG], BF16, 'rr', st) for _ in range(2)]
        tmp = [P.sb([128, 512], F32, 'tmp', st) for _ in range(2)]
        tp = [P.ps([128, 1024], BF16, 'tp', st) for _ in range(2)]
        ps1 = [P.ps([128, 512], F32, 'ps1', st) for _ in range(2)]
        ps2 = [P.ps([128, 512], F32, 'ps2', st) for _ in range(2)]

        P.dma('pool', ident[:], ident_d, writes=[ident], ctr=ident)
        for k in range(8):
            P.dma('pool', w1[:, k, :], w_ff1[k * 128:(k + 1) * 128, :], writes=[(w1, k)], ctr=w1)
        for c in range(32):
            P.dma('pool', w2[:, c, :], w_ff2[c * 128:(c + 1) * 128, :], writes=[(w2, c)], ctr=w2)
        for b_ in range(NB):
            P.dma('sp', sh2f[:, :, b_], mod_d[b_, 3, :].rearrange("(k p) -> p k", p=128), writes=[(sh2f, b_)], ctr=sh2f,
                  allow_slow_non_contiguous=True)
        P.op('dve', lambda e: e.tensor_copy(out=sh2b[:], in_=sh2f[:]), reads=[sh2f], writes=[sh2b])
        pb = ps1[0]
        for c in range(32):
            for k in range(8):
                P.op('pe', (lambda e, c=c, k=k: e.matmul(pb[:, c * NB:(c + 1) * NB], w1[:, k, c * 128:(c + 1) * 128], sh2b[:, k, :],
                                                        start=(k == 0), stop=(k == 7))),
                     reads=[w1, sh2b], writes=[pb])
        P.op('act', lambda e: e.activation(out=bias1[:].rearrange("p c b -> p (c b)"), in_=pb[:, 0:32 * NB], func=AF.Copy),
             reads=[pb], writes=[bias1])

        ng = NT // TG
        cur_b = -1
        for g in range(ng):
            b = (g * TG) // S
            if b != cur_b:
                cur_b = b
                P.dma('sp', G2bc[:], mod_d[b, 4, :].partition_broadcast(128), writes=[G2bc], ctr=G2bc)
                P.dma('sp', gt2bc[:], mod_d[b, 5, :].partition_broadcast(128), writes=[gt2bc], ctr=gt2bc)
            hT = h2T[g % 2]
            xs = [xt[(g % 2) * 2 + j] for j in range(2)]
            for j in range(2):
                r0 = g * TG + j * 128
                x = xs[j]
                P.dma('sp', x[:], xin[r0:r0 + 128, :], writes=[x], ctr=x)
                s_ = ss[j]
                xb = xn[j]
                tpp = tp[j]
                P.op('act', (lambda e, x=x, s_=s_: e.activation(out=junk[:], in_=x[:], func=AF.Square, accum_out=s_[:, 0:1])),
                     reads=[x], writes=[junk, s_])
                P.op('act', (lambda e, s_=s_: e.activation(out=s_[:, 1:2], in_=s_[:, 0:1], func=AF.Sqrt, scale=1.0 / D, bias=epsc[:, 0:1])),
                     reads=[s_, epsc], writes=[s_])
                P.op('dve', (lambda e, s_=s_: e.reciprocal(out=s_[:, 1:2], in_=s_[:, 1:2])), reads=[s_], writes=[s_])
                P.op('dve', (lambda e, x=x, s_=s_, xb=xb: e.scalar_tensor_tensor(out=xb[:], in0=x[:], scalar=s_[:, 1:2], in1=G2bc[:],
                                                                               op0=ALU.mult, op1=ALU.mult)),
                     reads=[x, s_, G2bc], writes=[xb])
                for k in range(8):
                    P.op('pe', (lambda e, k=k, xb=xb, tpp=tpp: e.transpose(out=tpp[:, k * 128:(k + 1) * 128], in_=xb[:, k * 128:(k + 1) * 128],
                                                                        identity=ident[:])),
                         reads=[xb, ident], writes=[tpp])
                P.op('act', (lambda e, tpp=tpp, hT=hT, j=j: e.activation(out=hT[:, :, j * 128:(j + 1) * 128],
                                                                       in_=tpp[:].rearrange("p (k t) -> p k t", k=8), func=AF.Copy)),
                     reads=[tpp], writes=[(hT, j)])
            for c in range(32):
                pp = ps1[c % 2]
                r_ = rr[c % 2]
                for k in range(8):
                    P.op('pe', (lambda e, c=c, k=k, pp=pp, hT=hT: e.matmul(pp[:, 0:TG], w1[:, k, c * 128:(c + 1) * 128], hT[:, k, :],
                                                                        start=(k == 0), stop=(k == 7))),
                         reads=[w1, hT], writes=[pp])
                P.op('act', (lambda e, c=c, pp=pp, r_=r_, b=b: e.activation(out=r_[:], in_=pp[:, 0:TG], func=AF.Relu, bias=bias1[:, c, b:b + 1])),
                     reads=[pp, bias1], writes=[r_])
                P.op('dve', (lambda e, c=c, r_=r_: e.tensor_tensor(out=u2T[:, c, :], in0=r_[:], in1=r_[:], op=ALU.mult)),
                     reads=[r_], writes=[(u2T, c)])
            for j in range(2):
                x = xs[j]
                for hf in range(2):
                    pp = ps2[hf]
                    tm = tmp[hf]
                    for c in range(32):
                        P.op('pe', (lambda e, c=c, j=j, hf=hf, pp=pp: e.matmul(pp[:], u2T[:, c, j * 128:(j + 1) * 128], w2[:, c, hf * 512:(hf + 1) * 512],
                                                                            start=(c == 0), stop=(c == 31))),
                             reads=[(u2T, c), w2], writes=[pp])
                    P.op('dve', (lambda e, pp=pp, tm=tm, hf=hf: e.tensor_tensor(out=tm[:], in0=pp[:], in1=gt2bc[:, hf * 512:(hf + 1) * 512], op=ALU.mult)),
                         reads=[pp, gt2bc], writes=[tm])
                    P.op('dve', (lambda e, x=x, tm=tm, hf=hf: e.tensor_tensor(out=x[:, hf * 512:(hf + 1) * 512], in0=tm[:], in1=x[:, hf * 512:(hf + 1) * 512], op=ALU.add)),
                         reads=[tm, x], writes=[x])
                r0 = g * TG + j * 128
                outs.append(P.dma('pool', xout[r0:r0 + 128, :], x[:], reads=[x], writes=[(xout, r0)], ctr=x))
    return outs


NPROJ = 3072


def phase_inproj(P, nc, x, projT, widxT, w_inp, w_widx, mu_d, mod_d, ident_d, NB, S):
    NT = NB * S
    TG = 512
    with contextlib.ExitStack() as st:
        win = P.sb([128, 8, NPROJ], BF16, 'win', st)
        wwx = P.sb([128, 8, 8], BF16, 'wwx', st)
        ident = P.sb([128, 128], BF16, 'ident', st)
        sh1f = P.sb([128, 8, NB], F32, 'sh1f', st)
        sh1b = P.sb([128, 8, NB], BF16, 'sh1b', st)
        bias = P.sb([128, 25, NB], F32, 'bias', st)
        mu = P.sb([128, 14], F32, 'mu', st)
        G1bc = P.sb([128, D], F32, 'G1bc', st)
        epsc = P.sb([128, 1], F32, 'epsc', st)
        xt = [P.sb([128, D], F32, 'xt', st) for _ in range(2)]
        xn = [P.sb([128, D], BF16, 'xn', st) for _ in range(2)]
        junk = P.sb([128, D], BF16, 'junk', st)
        ss = [P.sb([128, 2], F32, 'ss', st) for _ in range(2)]
        hT = [P.sb([128, 8, TG], BF16, 'hT', st) for _ in range(2)]
        pb = [P.sb([128, TG + 1], BF16, 'pb', st) for _ in range(14)]
        tmpd = [P.sb([128, TG], BF16, 'tmpd', st) for _ in range(2)]
        stg = [P.sb([128, TG], BF16, 'stg', st) for _ in range(3)]
        stw = [P.sb([8, TG], F32, 'stw', st) for _ in range(2)]
        tp = [P.ps([128, 1024], BF16, 'tp', st) for _ in range(2)]
        pp = [P.ps([128, 512], F32, 'pp', st) for _ in range(3)]

        I(P, 'dve', 'memset', ap=epsc[:], constant=RMS_EPS, extra_writes=[epsc])
        P.dma('pool', ident[:], ident_d, writes=[ident], ctr=ident)
        for k in range(8):
            P.dma('pool', win[:, k, :], w_inp[k * 128:(k + 1) * 128, :], writes=[(win, k)], ctr=win)
        P.dma('pool', wwx[:], w_widx.rearrange("(k p) e -> p k e", p=128), writes=[wwx], ctr=wwx)
        P.dma('sp', mu[:], mu_d, writes=[mu], ctr=mu)
        for b_ in range(NB):
            P.dma('sp', sh1f[:, :, b_], mod_d[b_, 0, :].rearrange("(k p) -> p k", p=128), writes=[(sh1f, b_)], ctr=sh1f,
                  allow_slow_non_contiguous=True)
        I(P, 'dve', 'tensor_copy', out=sh1b[:], in_=sh1f[:])
        pbm = pp[0]
        for cg in range(25):
            for k in range(8):
                if cg < 24:
                    I(P, 'pe', 'matmul', out=pbm[:, cg * NB:(cg + 1) * NB], lhsT=win[:, k, cg * 128:(cg + 1) * 128], rhs=sh1b[:, k, :],
                      start=(k == 0), stop=(k == 7))
                else:
                    I(P, 'pe', 'matmul', out=pbm[0:8, cg * NB:(cg + 1) * NB], lhsT=wwx[:, k, :], rhs=sh1b[:, k, :],
                      start=(k == 0), stop=(k == 7))
        I(P, 'act', 'activation', out=bias[:, 0:24, :].rearrange("p c b -> p (c b)"), in_=pbm[:, 0:24 * NB], func=AF.Copy)
        I(P, 'act', 'activation', out=bias[0:8, 24, :], in_=pbm[0:8, 24 * NB:25 * NB], func=AF.Copy)

        ng = NT // TG
        cur_b = -1
        si = 0
        for g in range(ng):
            tok0 = g * TG
            b = tok0 // S
            seq_start = (tok0 % S == 0)
            if b != cur_b:
                cur_b = b
                P.dma('sp', G1bc[:], mod_d[b, 1, :].partition_broadcast(128), writes=[G1bc], ctr=G1bc)
            h = hT[g % 2]
            for j in range(4):
                r0 = tok0 + j * 128
                xx = xt[j % 2]
                s_ = ss[j % 2]
                xb = xn[j % 2]
                tpp = tp[j % 2]
                P.dma('sp', xx[:], x[r0:r0 + 128, :], writes=[xx], ctr=xx)
                I(P, 'act', 'activation', out=junk[:], in_=xx[:], func=AF.Square, accum_out=s_[:, 0:1])
                I(P, 'act', 'activation', out=s_[:, 1:2], in_=s_[:, 0:1], func=AF.Sqrt, scale=1.0 / D, bias=epsc[:, 0:1])
                I(P, 'dve', 'reciprocal', out=s_[:, 1:2], in_=s_[:, 1:2])
                I(P, 'dve', 'scalar_tensor_tensor', out=xb[:], in0=xx[:], scalar=s_[:, 1:2], in1=G1bc[:], op0=ALU.mult, op1=ALU.mult)
                for k in range(8):
                    I(P, 'pe', 'transpose', out=tpp[:, k * 128:(k + 1) * 128], in_=xb[:, k * 128:(k + 1) * 128], identity=ident[:])
                I(P, 'act', 'activation', out=h[:, :, j * 128:(j + 1) * 128], in_=tpp[:].rearrange("p (k t) -> p k t", k=8), func=AF.Copy,
                  wsub=j)
            for cg in range(24):
                pq = pp[cg % 3]
                for k in range(8):
                    I(P, 'pe', 'matmul', out=pq[:], lhsT=win[:, k, cg * 128:(cg + 1) * 128], rhs=h[:, k, :], start=(k == 0), stop=(k == 7))
                sg = stg[si % 3]
                si += 1
                if cg < 10:
                    I(P, 'act', 'activation', out=sg[:], in_=pq[:], func=AF.Identity, bias=bias[:, cg, b:b + 1])
                else:
                    gi = cg - 10
                    pbb = pb[gi]
                    td = tmpd[gi % 2]
                    if seq_start:
                        I(P, 'dve', 'memset', ap=pbb[:, 0:1], constant=0.0, extra_writes=[pbb])
                    else:
                        I(P, 'dve', 'tensor_copy', out=pbb[:, 0:1], in_=pbb[:, TG:TG + 1])
                    I(P, 'act', 'activation', out=pbb[:, 1:TG + 1], in_=pq[:], func=AF.Identity, bias=bias[:, cg, b:b + 1])
                    I(P, 'dve', 'tensor_tensor', out=td[:], in0=pbb[:, 0:TG], in1=pbb[:, 1:TG + 1], op=ALU.subtract)
                    I(P, 'dve', 'scalar_tensor_tensor', out=sg[:], in0=td[:], scalar=mu[:, gi:gi + 1], in1=pbb[:, 1:TG + 1],
                      op0=ALU.mult, op1=ALU.add)
                P.dma('pool', projT[cg * 128:(cg + 1) * 128, tok0:tok0 + TG], sg[:], reads=[sg], writes=[(projT, (cg, g))], ctr=sg)
            pq = pp[0]
            for k in range(8):
                I(P, 'pe', 'matmul', out=pq[0:8, :], lhsT=wwx[:, k, :], rhs=h[:, k, :], start=(k == 0), stop=(k == 7))
            sw = stw[g % 2]
            I(P, 'act', 'activation', out=sw[:], in_=pq[0:8, :], func=AF.Identity, bias=bias[0:8, 24, b:b + 1])
            P.dma('pool', widxT[:, tok0:tok0 + TG], sw[:], reads=[sw], writes=[(widxT, g)], ctr=sw)


def phase_outproj(P, nc, x, xout, mixedT, w_out, mod_d, NB, S):
    NT = NB * S
    with contextlib.ExitStack() as st:
        wo = P.sb([128, 8, D], BF16, 'wo', st)
        gt1bc = P.sb([128, D], F32, 'gt1bc', st)
        mt = [P.sb([128, 8, 128], BF16, 'mt', st) for _ in range(2)]
        xt = [P.sb([128, D], F32, 'xt', st) for _ in range(2)]
        tmp = [P.sb([128, 512], F32, 'tmp', st) for _ in range(2)]
        pp = [P.ps([128, 512], F32, 'pp', st) for _ in range(2)]
        for k in range(8):
            P.dma('pool', wo[:, k, :], w_out[k * 128:(k + 1) * 128, :], writes=[(wo, k)], ctr=wo)
        cur_b = -1
        for i in range(NT // 128):
            r0 = i * 128
            b = r0 // S
            if b != cur_b:
                cur_b = b
                P.dma('sp', gt1bc[:], mod_d[b, 2, :].partition_broadcast(128), writes=[gt1bc], ctr=gt1bc)
            m = mt[i % 2]
            xx = xt[i % 2]
            P.dma('sp', m[:], mixedT[:, r0:r0 + 128].rearrange("(k p) t -> p k t", p=128), writes=[m], ctr=m)
            P.dma('sp', xx[:], x[r0:r0 + 128, :], writes=[xx], ctr=xx)
            for hf in range(2):
                pq = pp[hf]
                tm = tmp[hf]
                for k in range(8):
                    I(P, 'pe', 'matmul', out=pq[:], lhsT=m[:, k, :], rhs=wo[:, k, hf * 512:(hf + 1) * 512], start=(k == 0), stop=(k == 7))
                I(P, 'dve', 'tensor_tensor', out=tm[:], in0=pq[:], in1=gt1bc[:, hf * 512:(hf + 1) * 512], op=ALU.mult)
                I(P, 'dve', 'tensor_tensor', out=xx[:, hf * 512:(hf + 1) * 512], in0=tm[:], in1=xx[:, hf * 512:(hf + 1) * 512], op=ALU.add)
            P.dma('pool', xout[r0:r0 + 128, :], xx[:], reads=[xx], writes=[(xout, i)], ctr=xx)

STOP = 99

NEG = -30000.0


def phase_dsa(P, nc, projT, widxT, mixedT, C, NB, S, TOPK):
    NTL = S // 128
    NBLK = S // 512
    with contextlib.ExitStack() as st:
        sb = lambda shp, dt, nm: P.sb(shp, dt, nm, st)
        wuk = sb([128, 512], BF16, 'wuk'); wuv = sb([128, 512], BF16, 'wuv')
        ident = sb([128, 128], BF16, 'ident'); identf = sb([128, 128], F32, 'identf')
        ones = sb([128, 128], BF16, 'ones'); sel65 = sb([65, 64], BF16, 'sel65')
        ccorr = sb([128, 8, 128], BF16, 'ccorr'); dmask = sb([128, 128], F32, 'dmask')
        gq = sb([64, 1], F32, 'gq'); gk = sb([64, 1], F32, 'gk'); gqk = sb([64, 1], F32, 'gqk'); gkv = sb([128, 1], F32, 'gkv')
        epsc = sb([128, 1], F32, 'epsc')
        qaug = sb([68, 8, S], BF16, 'qaug'); kaug = sb([68, 8, S], BF16, 'kaug')
        qidx = sb([128, 4, S], BF16, 'qidx'); kidx = sb([128, S], BF16, 'kidx')
        ckv = sb([128, S], BF16, 'ckv'); vaug = sb([128, NTL, 8, 65], BF16, 'vaug')
        MbT2 = [sb([128, NTL, 512], BF16, 'MbT') for _ in range(2)]; scs = [sb([128, S], F32, 'sc') for _ in range(4)]; junk = [sb([128, S], BF16, 'junkS')] * 2
        lo = sb([128, 4], F32, 'lo'); negt = sb([128, 4], F32, 'negt'); Sg = sb([128, 4], F32, 'Sg'); dd = sb([128, 4], F32, 'dd')
        Mb = sb([128, S], BF16, 'Mb'); mx = sb([128, 8], F32, 'mx'); thr = sb([128, 1], F32, 'thr')
        clat = sb([128, 512], BF16, 'clat'); sqc = sb([128, 512], BF16, 'sqc')
        qraw = scs[3][:].bitcast(BF16)[0:64, :].rearrange("p (h t) -> p h t", h=8)
        sqk = [sb([64, 512], BF16, 'sqk') for _ in range(2)]
        wT = sb([8, 128], F32, 'wT'); wv = sb([128, 8], F32, 'wv'); rl = [sb([128, 512], F32, 'rl') for _ in range(2)]
        rcb = rl[0]; rr = [rl[1][0:64, :], sb([64, 512], F32, 'rr')]
        Pt = [sb([128, 512], BF16, 'Pt') for _ in range(3)]
        Osb = sb([65, 512], BF16, 'Osb'); rden = sb([64, 512], F32, 'rden'); oa = [sb([64, 512], BF16, 'oa') for _ in range(2)]
        PB = [P.ps([128, 512], F32, 'PB', st) for _ in range(7)]
        psA = PB[0:3]
        psB = PB[3:5]
        I(P, 'dve', 'memset', ap=epsc[:], constant=RMS_EPS, extra_writes=[epsc])
        for t_, d_ in ((wuk, 'w_uk'), (wuv, 'w_uv'), (ident, 'ident'), (ones, 'ones128'), (sel65, 'sel65'), (ccorr, 'ccorr')):
            P.dma('pool', t_[:], C[d_], writes=[t_], ctr=t_)
        for t_, d_ in ((identf, 'ident'), (dmask, 'dmask'), (gq, 'g_q'), (gk, 'g_k'), (gkv, 'g_kv')):
            P.dma('sp', t_[:], C[d_], writes=[t_], ctr=t_)
        I(P, 'dve', 'tensor_tensor', out=gqk[:], in0=gq[:], in1=gk[:], op=ALU.mult)
        I(P, 'dve', 'tensor_scalar', out=gqk[:], in0=gqk[:], scalar1=0.125, scalar2=None, op0=ALU.mult)
        for h in range(8):
            P.dma('pool', kaug[64:68, h, :], C['kaug'], writes=[(kaug, ('a', h))], ctr=(kaug, 'a'))
            P.dma('pool', qaug[64:68, h, :], C['qaug'][h], writes=[(qaug, ('a', h))], ctr=(qaug, 'a'))
        I(P, 'dve', 'memset', ap=vaug[:, :, :, 64:65], constant=1.0, extra_writes=[(vaug, 'one')])
        for b in range(NB):
            T0 = b * S
            P.dma('sp', qidx[:], projT[512:1024, T0:T0 + S].rearrange("(m p) t -> p m t", p=128), writes=[qidx], ctr=qidx)
            P.dma('sp', kidx[:], projT[1152:1280, T0:T0 + S], writes=[kidx], ctr=kidx)
            for blk in range(NBLK):
                c0 = blk * 512
                P.dma('sp', clat[:], projT[1024:1152, T0 + c0:T0 + c0 + 512], writes=[clat], ctr=clat)
                P.dma('sp', qraw[:], projT[0:512, T0 + c0:T0 + c0 + 512].rearrange("(h p) t -> p h t", p=64), writes=[qraw], ctr=qraw)
                I(P, 'dve', 'tensor_tensor', out=sqc[:], in0=clat[:], in1=clat[:], op=ALU.mult)
                pa = psA[0]
                I(P, 'pe', 'matmul', out=pa[:], lhsT=ones[:], rhs=sqc[:], start=True, stop=True)
                I(P, 'act', 'activation', out=rcb[:], in_=pa[:], func=AF.Sqrt, scale=1.0 / 128, bias=epsc[:, 0:1])
                I(P, 'dve', 'reciprocal', out=rcb[:], in_=rcb[:])
                I(P, 'dve', 'scalar_tensor_tensor', out=ckv[:, c0:c0 + 512], in0=clat[:], scalar=gkv[:, 0:1], in1=rcb[:],
                  op0=ALU.mult, op1=ALU.mult, wsub=blk)
                for j in range(4):
                    ti = blk * 4 + j
                    pv = psB[j % 2]
                    I(P, 'pe', 'matmul', out=pv[:], lhsT=ckv[:, ti * 128:(ti + 1) * 128], rhs=wuv[:], start=True, stop=True)
                    I(P, 'act', 'activation', out=vaug[:, ti, :, 0:64], in_=pv[:].rearrange("p (h d) -> p h d", h=8), func=AF.Copy,
                      wsub=('v', ti))
                for h in range(8):
                    pk = psA[1 + h % 2]; p2 = psB[h % 2]; sq = sqk[h % 2]; r_ = rr[h % 2]
                    I(P, 'pe', 'matmul', out=pk[0:64, :], lhsT=wuk[:, h * 64:(h + 1) * 64], rhs=ckv[:, c0:c0 + 512], start=True, stop=True)
                    I(P, 'act', 'activation', out=sq[:], in_=pk[0:64, :], func=AF.Square)
                    I(P, 'pe', 'matmul', out=p2[0:64, :], lhsT=ones[0:64, 0:64], rhs=sq[:], start=True, stop=True)
                    I(P, 'act', 'activation', out=r_[:], in_=p2[0:64, :], func=AF.Sqrt, scale=1.0 / 64, bias=epsc[0:64, 0:1])
                    I(P, 'dve', 'reciprocal', out=r_[:], in_=r_[:])
                    I(P, 'dve', 'tensor_tensor', out=kaug[0:64, h, c0:c0 + 512], in0=pk[0:64, :], in1=r_[:], op=ALU.mult, wsub=('k', h, blk))
                    p3 = psB[h % 2]
                    I(P, 'act', 'activation', out=sq[:], in_=qraw[:, h, :], func=AF.Square)
                    I(P, 'pe', 'matmul', out=p3[0:64, :], lhsT=ones[0:64, 0:64], rhs=sq[:], start=True, stop=True)
                    I(P, 'act', 'activation', out=r_[:], in_=p3[0:64, :], func=AF.Sqrt, scale=1.0 / 64, bias=epsc[0:64, 0:1])
                    I(P, 'dve', 'reciprocal', out=r_[:], in_=r_[:])
                    I(P, 'dve', 'scalar_tensor_tensor', out=qaug[0:64, h, c0:c0 + 512], in0=qraw[:, h, :], scalar=gqk[:, 0:1], in1=r_[:],
                      op0=ALU.mult, op1=ALU.mult, wsub=('q', h, blk))
            if STOP <= 1:
                continue
            if STOP <= 1:
                continue
            def prep(q, T0=T0):
                MbT = MbT2[q % 2]
                I(P, 'dve', 'memset', ap=MbT[:], constant=NEG, extra_writes=[MbT])
                RNG = 1024.0
                NIT = 26
                for ti in range(4):
                    i = q * 4 + ti
                    L = 128 * (i + 1)
                    sc = scs[ti]
                    P.dma('sp', wT[:], widxT[:, T0 + i * 128:T0 + (i + 1) * 128], writes=[wT], ctr=wT)
                    pw = PB[4]
                    I(P, 'pe', 'transpose', out=pw[:, 0:8], in_=wT[:], identity=identf[0:8, 0:8])
                    I(P, 'act', 'activation', out=wv[:], in_=pw[:, 0:8], func=AF.Copy)
                    nkb = (L + 511) // 512
                    for kb in range(nkb):
                        cols = min(512, L - kb * 512)
                        for h in range(8):
                            base = (h % 2) * 64; m = h // 2
                            pi = PB[2 + h % 2]; r_ = rl[h % 2]
                            I(P, 'pe', 'matmul', out=pi[:, 0:cols], lhsT=qidx[base:base + 64, m, i * 128:(i + 1) * 128],
                              rhs=kidx[base:base + 64, kb * 512:kb * 512 + cols], start=True, stop=True)
                            I(P, 'act', 'activation', out=r_[:, 0:cols], in_=pi[:, 0:cols], func=AF.Relu)
                            if h == 0:
                                I(P, 'dve', 'tensor_scalar', out=sc[:, kb * 512:kb * 512 + cols], in0=r_[:, 0:cols], scalar1=wv[:, 0:1],
                                  scalar2=None, op0=ALU.mult)
                            else:
                                I(P, 'dve', 'scalar_tensor_tensor', out=sc[:, kb * 512:kb * 512 + cols], in0=r_[:, 0:cols],
                                  scalar=wv[:, h:h + 1], in1=sc[:, kb * 512:kb * 512 + cols], op0=ALU.mult, op1=ALU.add)
                    I(P, 'dve', 'tensor_tensor', out=sc[:, L - 128:L], in0=sc[:, L - 128:L], in1=dmask[:], op=ALU.add)
                    yield 1
                act_t = [ti for ti in range(4) if 128 * (q * 4 + ti + 1) > TOPK]
                if act_t:
                    I(P, 'dve', 'memset', ap=lo[:], constant=-RNG, extra_writes=[lo])
                    for k in range(NIT):
                        s_k = RNG / (2.0 ** k)
                        for ti in act_t:
                            L = 128 * (q * 4 + ti + 1)
                            I(P, 'dve', 'tensor_scalar', out=negt[:, ti:ti + 1], in0=lo[:, ti:ti + 1], scalar1=-1.0, scalar2=-s_k,
                              op0=ALU.mult, op1=ALU.add, wsub=ti, rsub=ti)
                            I(P, 'act', 'activation', out=junk[ti % 2][:, 0:L], in_=scs[ti][:, 0:L], func=AF.Sign, bias=negt[:, ti:ti + 1],
                              accum_out=Sg[:, ti:ti + 1], wsub={Sg.name: ti}, rsub=ti)
                        if k % 3 == 2:
                            yield 1
                        for ti in act_t:
                            L = 128 * (q * 4 + ti + 1)
                            I(P, 'dve', 'tensor_scalar', out=dd[:, ti:ti + 1], in0=Sg[:, ti:ti + 1], scalar1=float(2 * TOPK - L), scalar2=None,
                              op0=ALU.is_ge, wsub=ti, rsub=ti)
                            I(P, 'dve', 'scalar_tensor_tensor', out=lo[:, ti:ti + 1], in0=dd[:, ti:ti + 1], scalar=s_k, in1=lo[:, ti:ti + 1],
                              op0=ALU.mult, op1=ALU.add, wsub=ti, rsub=ti)
                for ti in range(4):
                    i = q * 4 + ti
                    L = 128 * (i + 1)
                    sc = scs[ti]
                    if L > TOPK:
                        I(P, 'dve', 'tensor_scalar', out=Mb[:, 0:L], in0=sc[:, 0:L], scalar1=lo[:, ti:ti + 1], scalar2=NEG, op0=ALU.is_le, op1=ALU.mult)
                    else:
                        I(P, 'dve', 'tensor_scalar', out=Mb[:, 0:L], in0=sc[:, 0:L], scalar1=-1e29, scalar2=NEG, op0=ALU.is_lt, op1=ALU.mult)
                    for j0 in range(0, i + 1, 8):
                        j1 = min(i + 1, j0 + 8)
                        pt = PB[6]
                        ptb = pt[:].bitcast(BF16)
                        for j in range(j0, j1):
                            I(P, 'pe', 'transpose', out=ptb[:, (j - j0) * 128:(j - j0 + 1) * 128], in_=Mb[:, j * 128:(j + 1) * 128], identity=ident[:])
                        I(P, 'act', 'activation', out=MbT[:, j0:j1, ti * 128:(ti + 1) * 128],
                          in_=ptb[:, 0:(j1 - j0) * 128].rearrange("p (j t) -> p j t", t=128), func=AF.Copy, wsub=('t', i, j0))
                    yield 1

            def attn(q, T0=T0, b=b):
                MbT = MbT2[q % 2]
                nsb = 4 * q + 4
                for h in range(8):
                    po = PB[5]
                    for j in range(nsb):
                        pl = PB[j % 2]; pt_ = Pt[j % 3]
                        I(P, 'pe', 'matmul', out=pl[:], lhsT=kaug[0:68, h, j * 128:(j + 1) * 128], rhs=qaug[0:68, h, q * 512:(q + 1) * 512],
                          start=True, stop=False)
                        diag = j >= 4 * q
                        I(P, 'pe', 'matmul', out=pl[:], lhsT=ident[:], rhs=MbT[:, j, :], start=False, stop=not diag)
                        if diag:
                            dj = j - 4 * q
                            I(P, 'pe', 'matmul', out=pl[:, dj * 128:(dj + 1) * 128], lhsT=ident[:], rhs=ccorr[:, h, :], start=False, stop=True)
                        I(P, 'act', 'activation', out=pt_[:], in_=pl[:], func=AF.Exp)
                        I(P, 'pe', 'matmul', out=po[0:65, :], lhsT=vaug[:, j, h, :], rhs=pt_[:], start=(j == 0), stop=(j == nsb - 1))
                        if j % 4 == 3 and j != nsb - 1:
                            yield 1
                    I(P, 'act', 'activation', out=Osb[:], in_=po[0:65, :], func=AF.Copy)
                    pd = PB[4]
                    I(P, 'pe', 'matmul', out=pd[0:64, :], lhsT=sel65[:], rhs=Osb[:], start=True, stop=True)
                    I(P, 'dve', 'reciprocal', out=rden[:], in_=pd[0:64, :])
                    o_ = oa[h % 2]
                    I(P, 'dve', 'tensor_tensor', out=o_[:], in0=Osb[0:64, :], in1=rden[:], op=ALU.mult)
                    P.dma('pool', mixedT[h * 64:(h + 1) * 64, T0 + q * 512:T0 + (q + 1) * 512], o_[:], reads=[o_], writes=[(mixedT, (h, b, q))], ctr=o_)
                    yield 1

            for _ in prep(0):
                pass
            for q in range(NBLK):
                ga = attn(q)
                gp = prep(q + 1) if q + 1 < NBLK else iter(())
                da = dp = False
                while not (da and dp):
                    if not dp and next(gp, None) is None:
                        dp = True
                    if not da and next(ga, None) is None:
                        da = True

RSTOP = 99

GN_EPS = 64e-5


def phase_rwkv(P, nc, projT, mixedT, C, NB, S):
    TGR = 256
    NCH = 4
    NU = NCH * 8
    c0 = -float(np.exp(-0.5))
    with contextlib.ExitStack() as st:
        sb = lambda shp, dt, nm: P.sb(shp, dt, nm, st)
        w2b = sb([64, 512], BF16, 'w2b'); a2b = sb([64, 512], BF16, 'a2b'); g2b = sb([128, 512], BF16, 'g2b')
        vec = sb([64, 5, 8], F32, 'vec'); omk = sb([64, 8], F32, 'omk')
        lnw = sb([64, 512], F32, 'lnw'); lnb = sb([64, 512], F32, 'lnb'); rkr = sb([64, 512], F32, 'rkr')
        ones = sb([128, 128], BF16, 'onesr'); ident = sb([128, 128], BF16, 'identr')
        mask1 = sb([64, 128], F32, 'mask1'); mask3 = sb([64, 64], F32, 'mask3'); rst = sb([64, TGR], F32, 'rst')
        gneps = sb([64, 1], F32, 'gneps')
        F3 = [64, 8, TGR]
        rT = sb(F3, BF16, 'rT'); kT = sb(F3, BF16, 'kT'); vT = sb(F3, BF16, 'vT')
        xw = sb([64, TGR], BF16, 'xw'); xa = sb([64, TGR], BF16, 'xa'); xg = sb([128, TGR], BF16, 'xg')
        thb = sb([64, TGR], BF16, 'thb'); sgx = sb([128, TGR], BF16, 'sgx')
        sg = sb(F3, F32, 'sg'); cs = sb(F3, F32, 'cs'); Ea = sb(F3, F32, 'Ea'); Eb = sb(F3, F32, 'Eb')
        a_ = sb(F3, BF16, 'a_'); kkr = sb(F3, F32, 'kkr'); sq = sb(F3, BF16, 'sq')
        kk = sb(F3, F32, 'kk'); t1 = sb(F3, F32, 't1'); nrm = t1
        kmod = sb(F3, F32, 'kmod'); beta = kkr; gT = sb(F3, BF16, 'gT')
        bp = sb(F3, BF16, 'bp'); kp = sb(F3, BF16, 'kp'); kmb = sq
        AR = sb([64, 8, NCH, 2, 64], BF16, 'AR'); BK = sb([64, 8, NCH, 2, 64], BF16, 'BK'); GCt = sb([64, 8, NCH], F32, 'GCt')
        T3 = [64, NCH, 512]
        bpTM = sb(T3, BF16, 'bpTM'); kpTM = sb(T3, BF16, 'kpTM'); vTM = sb(T3, BF16, 'vTM')
        rTM = sb(T3, BF16, 'rTM'); kmTM = sb(T3, BF16, 'kmTM'); gTM = sb(T3, BF16, 'gTM')
        NM1 = [sb([64, 8, 128], BF16, 'NM1') for _ in range(NCH)]; NM2 = [sb([64, 8, 128], BF16, 'NM2') for _ in range(NCH)]
        NTt = [sb([64, 8, 64], BF16, 'NTt') for _ in range(NCH)]; X = [sb([64, 8, 64], BF16, 'X') for _ in range(NCH)]
        Pm = [[sb([64, 8, 64], BF16, 'Pm')] * 2 for _ in range(NCH)]; PTm = [[sb([64, 8, 64], BF16, 'PTm')] * 2 for _ in range(NCH)]
        Ysb = sb([64, 512], BF16, 'Ysb'); Usb = sb([64, 512], BF16, 'Usb')
        hst = sb([64, 8, 64], F32, 'hst'); hs = sb([64, 8, 64], F32, 'hs'); hbf = sb([64, 8, 64], BF16, 'hbf')
        OTM = sb(T3, F32, 'OTM')
        s1 = sb([64, NU], F32, 's1'); s2 = sb([64, NU], F32, 's2'); bs = sb([64, NU], F32, 'bs')
        ob = sb(T3, BF16, 'ob'); obT = [sb(F3, BF16, 'obT')] * 2
        PS = [P.ps([128, 512], F32, 'PSr', st) for _ in range(7)]
        cnt = [0]

        def nxt():
            cnt[0] += 1
            return PS[cnt[0] % 7]

        for t_, d_ in ((w2b, 'w2'), (a2b, 'a2'), (g2b, 'g2'), (ones, 'ones128'), (ident, 'ident')):
            P.dma('pool', t_[:], C[d_], writes=[t_], ctr=t_)
        for t_, d_ in ((vec, 'vec8'), (mask1, 'mask1'), (mask3, 'mask3')):
            P.dma('sp', t_[:], C[d_], writes=[t_], ctr=t_)
        P.dma('sp', rst[:], C['rst'][0:64, :], writes=[rst], ctr=rst)
        for t_, d_ in ((lnw, 'ln_w'), (lnb, 'ln_b'), (rkr, 'r_k1')):
            P.dma('sp', t_[:], C[d_].partition_broadcast(64), writes=[t_], ctr=t_)
        I(P, 'dve', 'memset', ap=gneps[:], constant=GN_EPS, extra_writes=[gneps])
        I(P, 'dve', 'tensor_scalar', out=omk[:], in0=vec[:, 3, :], scalar1=-1.0, scalar2=1.0, op0=ALU.mult, op1=ALU.add)

        def v3(t, h):
            return t[:, h, :].rearrange("p (n c) -> p n c", c=64)

        def fm_load(t_, r0, tok):
            P.dma('sp', t_[:], projT[r0:r0 + 512, tok].rearrange("(h p) t -> p h t", p=64), writes=[t_], ctr=t_)

        gi = 0
        for b in range(NB):
            I(P, 'dve', 'memset', ap=hst[:], constant=0.0, extra_writes=[hst])
            I(P, 'dve', 'memset', ap=hbf[:], constant=0.0, extra_writes=[hbf])
            for g in range(S // TGR):
                tok = b * S + g * TGR
                sl = slice(tok, tok + TGR)
                fm_load(rT, 1280, sl); fm_load(kT, 1792, sl); fm_load(vT, 2304, sl)
                P.dma('sp', xw[:], projT[2816:2880, sl], writes=[xw], ctr=xw)
                P.dma('sp', xa[:], projT[2880:2944, sl], writes=[xa], ctr=xa)
                P.dma('sp', xg[:], projT[2944:3072, sl], writes=[xg], ctr=xg)
                I(P, 'act', 'activation', out=thb[:], in_=xw[:], func=AF.Tanh)
                I(P, 'act', 'activation', out=sgx[:], in_=xg[:], func=AF.Sigmoid)
                for hp in range(4):
                    p1, p2, p3 = nxt(), nxt(), nxt()
                    for e2 in range(2):
                        h = hp * 2 + e2
                        hc = slice(h * 64, (h + 1) * 64)
                        oc = slice(e2 * TGR, (e2 + 1) * TGR)
                        I(P, 'pe', 'matmul', out=p1[0:64, oc], lhsT=w2b[:, hc], rhs=thb[:], start=True, stop=True)
                        I(P, 'pe', 'matmul', out=p2[0:64, oc], lhsT=a2b[:, hc], rhs=xa[:], start=True, stop=True)
                        I(P, 'pe', 'matmul', out=p3[0:64, oc], lhsT=g2b[:, hc], rhs=sgx[:], start=True, stop=True)
                    for e2 in range(2):
                        h = hp * 2 + e2
                        oc = slice(e2 * TGR, (e2 + 1) * TGR)
                        I(P, 'act', 'activation', out=sg[:, h, :], in_=p1[0:64, oc], func=AF.Sigmoid, bias=vec[:, 0, h:h + 1], wsub=h)
                        I(P, 'act', 'activation', out=a_[:, h, :], in_=p2[0:64, oc], func=AF.Sigmoid, bias=vec[:, 1, h:h + 1], wsub=h)
                    I(P, 'act', 'activation', out=gT[:, hp * 2:hp * 2 + 2, :], in_=p3[0:64, :].rearrange("p (e t) -> p e t", e=2), func=AF.Copy, wsub=hp)
                for h in range(8):
                    I(P, 'dve', 'tensor_scalar', out=kkr[:, h, :], in0=kT[:, h, :], scalar1=vec[:, 2, h:h + 1], scalar2=None, op0=ALU.mult, wsub=h)
                I(P, 'dve', 'tensor_tensor', out=sq[:], in0=kkr[:], in1=kkr[:], op=ALU.mult)
                for hp in range(4):
                    p1 = nxt()
                    for e2 in range(2):
                        h = hp * 2 + e2
                        I(P, 'pe', 'matmul', out=p1[0:64, e2 * TGR:(e2 + 1) * TGR], lhsT=ones[0:64, 0:64], rhs=sq[:, h, :], start=True, stop=True)
                    I(P, 'act', 'activation', out=nrm[:, hp * 2:hp * 2 + 2, :], in_=p1[0:64, :].rearrange("p (e t) -> p e t", e=2), func=AF.Sqrt, wsub=hp)
                I(P, 'dve', 'tensor_scalar', out=nrm[:], in0=nrm[:], scalar1=1e-12, scalar2=None, op0=ALU.max)
                I(P, 'dve', 'reciprocal', out=nrm[:], in_=nrm[:])
                I(P, 'dve', 'tensor_tensor', out=kk[:], in0=kkr[:], in1=nrm[:], op=ALU.mult)
                for h in range(8):
                    I(P, 'dve', 'tensor_scalar', out=t1[:, h, :], in0=a_[:, h, :], scalar1=vec[:, 3, h:h + 1], scalar2=omk[:, h:h + 1],
                      op0=ALU.mult, op1=ALU.add, wsub=h)
                I(P, 'dve', 'tensor_tensor', out=kmod[:], in0=kT[:], in1=t1[:], op=ALU.mult)
                I(P, 'dve', 'tensor_copy', out=kmb[:], in_=kmod[:])
                I(P, 'dve', 'tensor_tensor', out=beta[:], in0=kk[:], in1=a_[:], op=ALU.mult)
                for h in range(8):
                    I(P, 'dve', 'tensor_tensor_scan', out=cs[:, h, :], data0=rst[:], data1=sg[:, h, :], initial=0.0, op0=ALU.mult, op1=ALU.add,
                      wsub=h)
                I(P, 'act', 'activation', out=Ea[:], in_=cs[:], func=AF.Exp, scale=c0)
                for h in range(8):
                    I(P, 'dve', 'tensor_tensor', out=AR[:, h, :, 1, :], in0=v3(rT, h), in1=v3(Ea, h), op=ALU.mult, wsub=('r', h))
                I(P, 'dve', 'tensor_copy', out=GCt[:], in_=Ea[:].rearrange("p h (n c) -> p h n c", c=64)[:, :, :, 63])
                I(P, 'act', 'activation', out=Eb[:], in_=cs[:], func=AF.Exp, scale=-c0)
                for h in range(8):
                    I(P, 'dve', 'tensor_tensor', out=BK[:, h, :, 0, :], in0=v3(beta, h), in1=v3(Eb, h), op=ALU.mult, wsub=('b', h))
                    I(P, 'dve', 'tensor_tensor', out=BK[:, h, :, 1, :], in0=v3(kmod, h), in1=v3(Eb, h), op=ALU.mult, wsub=('k', h))
                I(P, 'dve', 'tensor_tensor', out=Ea[:], in0=cs[:], in1=sg[:], op=ALU.subtract)
                I(P, 'act', 'activation', out=Ea[:], in_=Ea[:], func=AF.Exp, scale=c0)
                for h in range(8):
                    I(P, 'dve', 'scalar_tensor_tensor', out=AR[:, h, :, 0, :], in0=v3(kk, h), scalar=-1.0, in1=v3(Ea, h), op0=ALU.mult, op1=ALU.mult,
                      wsub=('a', h))
                csv = cs[:].rearrange("p h (n c) -> p (h n) c", c=64)
                I(P, 'dve', 'tensor_tensor', out=Eb[:].rearrange("p h (n c) -> p (h n) c", c=64), in0=csv[:, :, 63:64].to_broadcast([64, 8 * NCH, 64]),
                  in1=csv, op=ALU.subtract)
                I(P, 'act', 'activation', out=Eb[:], in_=Eb[:], func=AF.Exp, scale=c0)
                I(P, 'dve', 'tensor_tensor', out=bp[:], in0=beta[:], in1=Eb[:], op=ALU.mult)
                I(P, 'dve', 'tensor_tensor', out=kp[:], in0=kmod[:], in1=Eb[:], op=ALU.mult)
                if RSTOP <= 1:
                    continue
                tcnt = 0
                for Xf, Xt in ((bp, bpTM), (kp, kpTM), (vT, vTM), (rT, rTM), (kmb, kmTM), (gT, gTM)):
                    for n in range(NCH):
                        ps = nxt()
                        pb_ = ps[:].bitcast(BF16)
                        for h in range(8):
                            I(P, 'pe', 'transpose', out=pb_[0:64, h * 64:(h + 1) * 64], in_=Xf[:, h, n * 64:(n + 1) * 64], identity=ident[0:64, 0:64])
                        tcnt += 1
                        if tcnt % 2:
                            I(P, 'act', 'activation', out=Xt[:, n, :], in_=pb_[0:64, 0:512], func=AF.Copy, wsub=n)
                        else:
                            I(P, 'dve', 'tensor_copy', out=Xt[:, n, :], in_=pb_[0:64, 0:512], wsub=n)
                if RSTOP <= 2:
                    continue
                Pcs = {}
                PTcs = {}
                for n in range(NCH):
                    nm1 = NM1[n]; nm2 = NM2[n]; ntt = NTt[n]; x_ = X[n]
                    for hq in range(2):
                        pa, pb2, pc = nxt(), nxt(), nxt()
                        for hh in range(4):
                            h = hq * 4 + hh
                            arv = AR[:, h, n, :, :].rearrange("p a c -> p (a c)")
                            I(P, 'pe', 'matmul', out=pa[0:64, hh * 128:(hh + 1) * 128], lhsT=BK[:, h, n, 0, :], rhs=arv, start=True, stop=True)
                            I(P, 'pe', 'matmul', out=pb2[0:64, hh * 128:(hh + 1) * 128], lhsT=BK[:, h, n, 1, :], rhs=arv, start=True, stop=True)
                            I(P, 'pe', 'matmul', out=pc[0:64, hh * 64:(hh + 1) * 64], lhsT=AR[:, h, n, 0, :], rhs=BK[:, h, n, 0, :], start=True, stop=True)
                        m1b = mask1[:].unsqueeze(1).to_broadcast([64, 4, 128])
                        I(P, 'dve', 'tensor_tensor', out=nm1[:, hq * 4:(hq + 1) * 4, :], in0=pa[0:64, :].rearrange("p (h c) -> p h c", c=128), in1=m1b,
                          op=ALU.mult, wsub=hq)
                        I(P, 'dve', 'tensor_tensor', out=nm2[:, hq * 4:(hq + 1) * 4, :], in0=pb2[0:64, :].rearrange("p (h c) -> p h c", c=128), in1=m1b,
                          op=ALU.mult, wsub=hq)
                        I(P, 'dve', 'tensor_tensor', out=ntt[:, hq * 4:(hq + 1) * 4, :], in0=pc[0:64, 0:256].rearrange("p (h c) -> p h c", c=64),
                          in1=mask3[:].unsqueeze(1).to_broadcast([64, 4, 64]), op=ALU.mult, wsub=hq)
                    I(P, 'dve', 'tensor_tensor', out=x_[:], in0=nm1[:, :, 0:64], in1=ident[0:64, 0:64].unsqueeze(1).to_broadcast([64, 8, 64]), op=ALU.add)
                    Pcs[n] = (lambda u, nm1=nm1: nm1[:, u, 0:64])
                    PTcs[n] = (lambda u, ntt=ntt: ntt[:, u, :])
                for k in range(6):
                    for n in range(NCH):
                        x_ = X[n]; Pc = Pcs[n]; PTc = PTcs[n]
                        if k >= 1:
                            pX = nxt()
                            for u in range(8):
                                I(P, 'pe', 'matmul', out=pX[0:64, u * 64:(u + 1) * 64], lhsT=PTc(u), rhs=x_[:, u, :], start=True, stop=True)
                        if k < 5:
                            pP, pPT = nxt(), nxt()
                            for u in range(8):
                                I(P, 'pe', 'matmul', out=pP[0:64, u * 64:(u + 1) * 64], lhsT=PTc(u), rhs=Pc(u), start=True, stop=True)
                                I(P, 'pe', 'matmul', out=pPT[0:64, u * 64:(u + 1) * 64], lhsT=Pc(u), rhs=PTc(u), start=True, stop=True)
                        if k >= 1:
                            I(P, 'dve', 'tensor_tensor', out=x_[:], in0=x_[:], in1=pX[0:64, :].rearrange("p (u c) -> p u c", c=64), op=ALU.add)
                        if k < 5:
                            pn = Pm[n][k % 2]; ptn = PTm[n][k % 2]
                            I(P, 'act', 'activation', out=pn[:], in_=pP[0:64, :].rearrange("p (u c) -> p u c", c=64), func=AF.Copy)
                            I(P, 'dve', 'tensor_copy', out=ptn[:], in_=pPT[0:64, :].rearrange("p (u c) -> p u c", c=64))
                            Pcs[n] = (lambda u, pn=pn: pn[:, u, :])
                            PTcs[n] = (lambda u, ptn=ptn: ptn[:, u, :])
                for n in range(NCH):
                    nm1 = NM1[n]; nm2 = NM2[n]; x_ = X[n]
                    if RSTOP <= 3:
                        continue
                    pY = nxt()
                    for h in range(8):
                        hc = slice(h * 64, (h + 1) * 64)
                        I(P, 'pe', 'matmul', out=pY[0:64, hc], lhsT=AR[:, h, n, 0, :], rhs=hbf[:, h, :], start=True, stop=False)
                        I(P, 'pe', 'matmul', out=pY[0:64, hc], lhsT=nm2[:, h, 0:64], rhs=vTM[:, n, hc], start=False, stop=True)
                    I(P, 'act', 'activation', out=Ysb[:], in_=pY[0:64, :], func=AF.Copy)
                    pU = nxt()
                    for h in range(8):
                        hc = slice(h * 64, (h + 1) * 64)
                        I(P, 'pe', 'matmul', out=pU[0:64, hc], lhsT=x_[:, h, :], rhs=Ysb[:, hc], start=True, stop=True)
                    I(P, 'dve', 'tensor_copy', out=Usb[:], in_=pU[0:64, :])
                    pO = nxt()
                    for h in range(8):
                        hc = slice(h * 64, (h + 1) * 64)
                        I(P, 'pe', 'matmul', out=pO[0:64, hc], lhsT=AR[:, h, n, 1, :], rhs=hbf[:, h, :], start=True, stop=False)
                        I(P, 'pe', 'matmul', out=pO[0:64, hc], lhsT=nm1[:, h, 64:128], rhs=Usb[:, hc], start=False, stop=False)
                        I(P, 'pe', 'matmul', out=pO[0:64, hc], lhsT=nm2[:, h, 64:128], rhs=vTM[:, n, hc], start=False, stop=True)
                    I(P, 'act', 'activation', out=OTM[:, n, :], in_=pO[0:64, :], func=AF.Copy, wsub=n)
                    pH = nxt()
                    for h in range(8):
                        hc = slice(h * 64, (h + 1) * 64)
                        I(P, 'pe', 'matmul', out=pH[0:64, hc], lhsT=bpTM[:, n, hc], rhs=Usb[:, hc], start=True, stop=False)
                        I(P, 'pe', 'matmul', out=pH[0:64, hc], lhsT=kpTM[:, n, hc], rhs=vTM[:, n, hc], start=False, stop=True)
                    I(P, 'dve', 'tensor_tensor', out=hs[:], in0=hst[:], in1=GCt[:, :, n:n + 1].to_broadcast([64, 8, 64]), op=ALU.mult)
                    I(P, 'dve', 'tensor_tensor', out=hst[:], in0=hs[:], in1=pH[0:64, :].rearrange("p (h c) -> p h c", c=64), op=ALU.add)
                    I(P, 'dve', 'tensor_copy', out=hbf[:], in_=hst[:])
                if RSTOP <= 4:
                    continue
                O3 = OTM[:].rearrange("p n (h d) -> p (n h) d", d=64)
                OcA = Ea[:].rearrange("p h t -> p (h t)").rearrange("p (n c) -> p n c", c=512)
                sqA = Eb[:].rearrange("p h t -> p (h t)").rearrange("p (n c) -> p n c", c=512)
                Oc3 = OcA.rearrange("p n (h d) -> p (n h) d", d=64)
                sq3 = sqA.rearrange("p n (h d) -> p (n h) d", d=64)
                bc = lambda t: t[:, :].unsqueeze(2).to_broadcast([64, NU, 64])
                I(P, 'dve', 'tensor_reduce', out=s1[:], in_=O3, axis=AX.X, op=ALU.add)
                I(P, 'dve', 'tensor_scalar', out=s1[:], in0=s1[:], scalar1=1.0 / 64, scalar2=None, op0=ALU.mult)
                I(P, 'dve', 'tensor_tensor', out=Oc3, in0=O3, in1=bc(s1), op=ALU.subtract)
                I(P, 'dve', 'tensor_tensor', out=sqA, in0=OcA, in1=OcA, op=ALU.mult)
                I(P, 'dve', 'tensor_reduce', out=s2[:], in_=sq3, axis=AX.X, op=ALU.add)
                I(P, 'act', 'activation', out=s2[:], in_=s2[:], func=AF.Sqrt, scale=1.0 / 64, bias=gneps[:, 0:1])
                I(P, 'dve', 'reciprocal', out=s2[:], in_=s2[:])
                I(P, 'dve', 'tensor_tensor', out=Oc3, in0=Oc3, in1=bc(s2), op=ALU.mult)
                I(P, 'dve', 'tensor_tensor', out=OcA, in0=OcA, in1=lnw[:].unsqueeze(1).to_broadcast([64, NCH, 512]), op=ALU.mult)
                I(P, 'dve', 'tensor_tensor', out=OcA, in0=OcA, in1=lnb[:].unsqueeze(1).to_broadcast([64, NCH, 512]), op=ALU.add)
                I(P, 'dve', 'tensor_tensor', out=sqA, in0=rTM[:], in1=kmTM[:], op=ALU.mult)
                I(P, 'dve', 'tensor_tensor', out=sqA, in0=sqA, in1=rkr[:].unsqueeze(1).to_broadcast([64, NCH, 512]), op=ALU.mult)
                I(P, 'dve', 'tensor_reduce', out=bs[:], in_=sq3, axis=AX.X, op=ALU.add)
                I(P, 'dve', 'tensor_tensor', out=sq3, in0=vTM[:].rearrange("p n (h d) -> p (n h) d", d=64), in1=bc(bs), op=ALU.mult)
                I(P, 'dve', 'tensor_tensor', out=OcA, in0=OcA, in1=sqA, op=ALU.add)
                I(P, 'dve', 'tensor_tensor', out=ob[:], in0=OcA, in1=gTM[:], op=ALU.mult)
                oT = obT[g % 2]
                for n in range(NCH):
                    ps = nxt()
                    pb_ = ps[:].bitcast(BF16)
                    for h in range(8):
                        I(P, 'pe', 'transpose', out=pb_[0:64, h * 64:(h + 1) * 64], in_=ob[:, n, h * 64:(h + 1) * 64], identity=ident[0:64, 0:64])
                    I(P, 'act', 'activation', out=oT[:, :, n * 64:(n + 1) * 64], in_=pb_[0:64, 0:512].rearrange("p (h c) -> p h c", c=64), func=AF.Copy, wsub=n)
                P.dma('pool', mixedT[512:1024, sl].rearrange("(h p) t -> p h t", p=64), oT[:], reads=[oT], writes=[(mixedT, ('ob', b, g))], ctr=oT)


A_W, KVL, IH, ID_ = 512, 128, 8, 64
N_IN_A = 1224


def perm_cols():
    q = list(range(0, 512))
    clat = list(range(512, 640))
    qidx = list(range(640, 1152))
    kidx = list(range(1152, 1216))
    B0 = N_IN_A
    r = list(range(B0, B0 + 512))
    k = list(range(B0 + 512, B0 + 1024))
    v = list(range(B0 + 1024, B0 + 1536))
    xwxa = list(range(B0 + 1536, B0 + 1664))
    xg = list(range(B0 + 1664, B0 + 1792))
    return q + qidx + clat + kidx + kidx + r + k + v + xwxa + xg


def host_consts(S):
    f = np.float32
    t = np.arange(S)
    slopes = 2.0 ** (-np.arange(1, 9, dtype=np.float64))
    kaug = np.stack([np.ones(S), np.ones(S), 64.0 * (t // 64), (t % 64) * 1.0]).astype(f)
    qaug = np.stack([np.stack([-s * 64.0 * (t // 64), -s * (t % 64), s * np.ones(S), s * np.ones(S)]) for s in slopes]).astype(f)
    ss, tt = np.meshgrid(np.arange(128), np.arange(128), indexing='ij')
    same = (ss // 64) == (tt // 64)
    ccorr = np.stack([np.where(same, -2.0 * s * np.maximum(ss - tt, 0), 0.0) for s in slopes], axis=1).astype(f)
    tq, sk = np.meshgrid(np.arange(128), np.arange(128), indexing='ij')
    dmask = np.where(sk < (tq // 64 + 1) * 64, 0.0, -1e30).astype(f)
    sel65 = np.zeros((65, 64), f)
    sel65[64, :] = 1.0
    return dict(kaug=kaug, qaug=qaug, ccorr=ccorr, dmask=dmask, sel65=sel65,
                ident=np.eye(128, dtype=f), ones128=np.ones((128, 128), f))


def rwkv_consts():
    f = np.float32
    i = np.arange(128)
    bones = ((i[:, None] // 64) == (i[None, :] // 64)).astype(f)
    s_, t_ = np.meshgrid(np.arange(64), np.arange(64), indexing='ij')
    mask1 = np.concatenate([(s_ < t_), (s_ <= t_)], axis=1).astype(f)
    mask3 = (t_ < s_).astype(f)
    rst = np.ones((128, 256), f)
    rst[:, 0::64] = 0.0
    return dict(bones=bones, mask1=mask1, mask3=mask3, rst=rst)


def rwkv_params(w0, w2, a0, a2, g2, k_k, k_a, r_k, ln_w, ln_b):
    f = np.float32
    col = lambda v: np.asarray(v, f).reshape(8, 64).T
    vec8 = np.ascontiguousarray(np.stack([col(w0), col(a0), col(k_k), col(k_a), col(np.asarray(r_k, f).reshape(512))], axis=1))
    return dict(w2=np.ascontiguousarray(np.asarray(w2, f)), a2=np.ascontiguousarray(np.asarray(a2, f)), g2=np.ascontiguousarray(np.asarray(g2, f)),
                vec8=vec8, ln_w=np.ascontiguousarray(np.asarray(ln_w, f).reshape(512)), ln_b=np.ascontiguousarray(np.asarray(ln_b, f).reshape(512)),
                r_k1=np.ascontiguousarray(np.asarray(r_k, f).reshape(512)))


NCORES = 8
NB_ = 4
S_ = 2048


def build_nc(shapes):
    NB, S = NB_, S_
    NT = NB * S
    nc = bass.Bass("TRN2", target_bir_lowering=False)
    A = {k: nc.dram_tensor(k, list(shp), F32, kind="ExternalInput").ap() for k, shp in shapes.items()}
    out = nc.dram_tensor("out", [NT, D], F32, kind="ExternalOutput").ap()
    mod_d = nc.dram_tensor("mod_d", [NB, 6, D], F32, kind="Internal").ap()
    projT = nc.dram_tensor("projT", [NPROJ, NT], BF16, kind="Internal").ap()
    widxT = nc.dram_tensor("widxT", [8, NT], F32, kind="Internal").ap()
    mixedT = nc.dram_tensor("mixedT", [1024, NT], BF16, kind="Internal").ap()
    with contextlib.ExitStack() as stack:
        P = Prog(nc, stack)
        phase_mod(P, nc, A['cT'], A['w_ada'], A['b_ada'], A['g_mix'], A['g_ffn'], mod_d, NB)
        P.barrier()
        phase_inproj(P, nc, A['x'], projT, widxT, A['w_inp'], A['w_widx'], A['mu'], mod_d, A['ident'], NB, S)
        P.barrier()
        phase_dsa(P, nc, projT, widxT, mixedT, A, NB, S, min(256, S // 4))
        P.barrier()
        phase_rwkv(P, nc, projT, mixedT, A, NB, S)
        P.barrier()
        phase_outproj(P, nc, A['x'], out, mixedT, A['w_out'], mod_d, NB, S)
        P.barrier()
        outs = phase_ffn(P, nc, out, out, A['w_ff1'], A['w_ff2'], mod_d, A['ident'], NB, S)
        P.emit(final_wait_ops=outs)
    return nc


def kernel(x, c, w_ada, b_ada, g_mix, g_ffn, w_in, g_q, g_k, g_kv, w_uk, w_uv, mu_shift, w0, w2, a0, a2, g2,
           k_k, k_a, r_k, ln_w, ln_b, w_out, w_ff1, w_ff2):
    f = np.float32
    x = np.asarray(x, f); c = np.asarray(c, f)
    B, S, Dm = x.shape
    NB = B // NCORES
    pc = perm_cols()
    hc = host_consts(S)
    w_in0 = np.asarray(w_in, f)[0]
    mu_full = np.zeros(w_in0.shape[1], f)
    mu_full[N_IN_A:] = np.asarray(mu_shift, f)[0]
    mu_perm = mu_full[pc]
    l0 = lambda a: np.asarray(a, f)[0]
    shared = dict(
        w_ada=np.ascontiguousarray(l0(w_ada)), b_ada=np.ascontiguousarray(l0(b_ada)),
        g_mix=np.ascontiguousarray(l0(g_mix)), g_ffn=np.ascontiguousarray(l0(g_ffn)),
        w_inp=np.ascontiguousarray(w_in0[:, pc]), w_widx=np.ascontiguousarray(w_in0[:, 1216:1224]),
        mu=np.ascontiguousarray(mu_perm[1280:].reshape(14, 128).T),
        ident=hc['ident'],
        w_uk=np.ascontiguousarray(l0(w_uk).reshape(128, 512)), w_uv=np.ascontiguousarray(l0(w_uv).reshape(128, 512)),
        g_q=np.ascontiguousarray(l0(g_q).reshape(64, 1)), g_k=np.ascontiguousarray(l0(g_k).reshape(64, 1)),
        g_kv=np.ascontiguousarray(l0(g_kv).reshape(128, 1)),
        kaug=hc['kaug'], qaug=hc['qaug'], ccorr=hc['ccorr'], dmask=hc['dmask'], sel65=hc['sel65'], ones128=hc['ones128'],
        w_out=np.ascontiguousarray(l0(w_out)), w_ff1=np.ascontiguousarray(l0(w_ff1)), w_ff2=np.ascontiguousarray(l0(w_ff2)),
        **rwkv_consts(),
        **rwkv_params(l0(w0), l0(w2), l0(a0), l0(a2), l0(g2), l0(k_k), l0(k_a), l0(r_k), l0(ln_w), l0(ln_b)),
    )
    in_maps = []
    for i in range(NCORES):
        m = dict(shared)
        m['x'] = np.ascontiguousarray(x[i * NB:(i + 1) * NB].reshape(NB * S, Dm))
        ci = c[i * NB:(i + 1) * NB]
        m['cT'] = np.ascontiguousarray(ci.T.reshape(8, 128, NB).transpose(1, 0, 2))
        in_maps.append(m)
    nc = build_nc({k: v.shape for k, v in in_maps[0].items()})
    res = run_bass_kernel_spmd(nc, in_maps, core_ids=list(range(NCORES)))
    outs = [np.asarray(r["out"], f).reshape(NB, S, Dm) for r in res.results]
    return np.concatenate(outs, axis=0)
```

```python
import contextlib
import os
import numpy as np
import concourse.bass as bass
import concourse.mybir as mybir
from concourse.bass_utils import run_bass_kernel_spmd


F32 = mybir.dt.float32
BF16 = mybir.dt.bfloat16
AF = mybir.ActivationFunctionType
ALU = mybir.AluOpType
AX = mybir.AxisListType

EPOCH = 16000
ENGS = ['pe', 'dve', 'act', 'pool', 'sp']


class Op:
    __slots__ = ('eng', 'fn', 'deps', 'pos', 'signal', 'seq', 'is_dma', 'ctr', 'val', 'waits', 'tag')


class Counter:
    def __init__(self, name):
        self.name = name
        self.sems = []
        self.count = 0

    def need(self, prog, v):
        ep = (v - 1) // EPOCH
        while len(self.sems) <= ep:
            self.sems.append(prog.new_sem())

    def sem_for(self, v):
        ep = (v - 1) // EPOCH
        return self.sems[ep], v - ep * EPOCH


class Prog:
    def __init__(self, nc, stack):
        self.nc = nc
        self.stack = stack
        self.eng_ops = {e: [] for e in ENGS}
        self.res = {}
        self.engctr = {e: Counter(e) for e in ENGS}
        self.dmactr = {}
        self.nsem = 0
        self.ntile = 0
        self.all_dma = []
        self.keep = []
        self._bar_t = {e: self.sb([1, 8], F32, name=f"bar{e}") for e in ENGS}
        self._bar_ps = self.ps([1, 8], F32, name="barps")
        self._bar_tok = {e: f"__bar_{e}" for e in ENGS}
        self._bar_init = False
        self._bar_last = {}

    def new_sem(self):
        self.nsem += 1
        return self.stack.enter_context(self.nc.semaphore(f"sm{self.nsem}"))

    def sb(self, shape, dtype, name=None, stack=None):
        self.ntile += 1
        nm = f"{name or 't'}_{self.ntile}"
        t = (stack or self.stack).enter_context(self.nc.sbuf_tensor(nm, list(shape), dtype))
        self.keep.append(t)
        return t

    def ps(self, shape, dtype=F32, name=None, stack=None):
        self.ntile += 1
        nm = f"{name or 'p'}_{self.ntile}"
        t = (stack or self.stack).enter_context(self.nc.psum_tensor(nm, list(shape), dtype))
        self.keep.append(t)
        return t

    @staticmethod
    def _key(r):
        if isinstance(r, tuple):
            return id(r[0]), r[1]
        return id(r), None

    def _deps(self, op, reads, writes):
        deps = []
        for r in reads:
            tk, sk = self._key(r)
            ent = self.res.setdefault(tk, {})
            for k2, e2 in ent.items():
                if sk is None or k2 is None or k2 == sk:
                    if e2[0] is not None:
                        deps.append((e2[0], 'raw'))
            e = ent.setdefault(sk, [None, []])
            if not op.is_dma:
                e[1] = [o for o in e[1] if o.is_dma or o.eng != op.eng]
            e[1].append(op)
        for w in writes:
            tk, sk = self._key(w)
            ent = self.res.setdefault(tk, {})
            for k2, e2 in ent.items():
                if sk is None or k2 is None or k2 == sk:
                    if e2[0] is not None:
                        deps.append((e2[0], 'waw'))
                    for o in e2[1]:
                        if o is not op:
                            deps.append((o, 'war'))
            if sk is None:
                ent.clear()
            ent[sk] = [op, []]
        return deps

    def op(self, eng, fn, reads=(), writes=(), tag=None):
        o = Op()
        o.eng = eng
        o.fn = fn
        o.is_dma = False
        o.signal = False
        o.seq = 0
        o.ctr = None
        o.val = 0
        o.tag = tag
        o.deps = self._deps(o, reads, writes)
        self.eng_ops[eng].append(o)
        o.pos = len(self.eng_ops[eng])
        return o

    def dma(self, q, out, in_, reads=(), writes=(), ctr=None, tag=None, **kw):
        o = Op()
        o.eng = q
        o.is_dma = True
        o.signal = True
        o.seq = 0
        o.tag = tag
        o.fn = lambda e: e.dma_start(out=out, in_=in_, **kw)
        ck = self._key(ctr)
        c = self.dmactr.get(ck)
        if c is None:
            c = self.dmactr[ck] = Counter(f"d{len(self.dmactr)}")
        c.count += 16
        o.ctr = c
        o.val = c.count
        o.deps = self._deps(o, reads, writes)
        self.eng_ops[q].append(o)
        o.pos = len(self.eng_ops[q])
        self.all_dma.append(o)
        return o

    def barrier(self):
        if not hasattr(self, '_bar_t'):
            self._bar_t = {e: self.sb([1, 8], F32, name=f"bar{e}") for e in ENGS}
            self._bar_ps = self.ps([1, 8], F32, name="barps")
            self._bar_tok = {e: f"__bar_{e}" for e in ENGS}
            self.keep.append(self._bar_tok)
        bt = self._bar_t
        if not self._bar_init:
            self._bar_init = True
            for e_ in ('pe', 'act'):
                self.op('dve', (lambda eng, e_=e_: eng.memset(bt[e_][:], 0.0)), writes=[bt[e_]])
        dmas = [d for d in self.all_dma]
        self.all_dma = []
        first = []
        for e in ENGS:
            if e == 'pe':
                fn = lambda eng: eng.matmul(self._bar_ps[0:1, 0:1], bt['pe'][0:1, 0:1], bt['pe'][0:1, 1:2], start=True, stop=True)
            elif e == 'sp':
                fn = lambda eng: eng.nop()
            elif e == 'act':
                fn = lambda eng: eng.activation(out=bt['act'][0:1, 2:3], in_=bt['act'][0:1, 3:4], func=AF.Copy)
            elif e == 'dve':
                fn = lambda eng: eng.memset(bt['dve'][0:1, 2:3], 0.0)
            else:
                fn = lambda eng: eng.memset(bt['pool'][0:1, 2:3], 0.0)
            o = self.op(e, fn, reads=([bt[e]] if e in ('pe', 'act') else []), writes=[(self._bar_tok[e], 0)])
            if e in self._bar_last:
                o.deps.append((self._bar_last[e], 'waw'))
            self._bar_last[e] = o
            if e == 'sp':
                o.deps += [(d, 'raw') for d in dmas]
            first.append(o)
        for e in ENGS:
            if e == 'pe':
                fn = lambda eng: eng.matmul(self._bar_ps[0:1, 4:5], bt['pe'][0:1, 0:1], bt['pe'][0:1, 1:2], start=True, stop=True)
            elif e == 'sp':
                fn = lambda eng: eng.nop()
            elif e == 'act':
                fn = lambda eng: eng.activation(out=bt['act'][0:1, 4:5], in_=bt['act'][0:1, 3:4], func=AF.Copy)
            elif e == 'dve':
                fn = lambda eng: eng.memset(bt['dve'][0:1, 4:5], 0.0)
            else:
                fn = lambda eng: eng.memset(bt['pool'][0:1, 4:5], 0.0)
            o = self.op(e, fn, reads=([bt[e]] if e in ('pe', 'act') else []))
            o.deps += [(f, 'raw') for f in first if f.eng != e]
            o.deps.append((self._bar_last[e], 'waw'))
            self._bar_last[e] = o
        self.res.clear()

    def init_bar(self):
        self.barrier_ready = True

    def emit(self, final_wait_ops=()):
        fin = self.op('sp', lambda eng: eng.nop())
        fin.deps += [(d, 'raw') for d in final_wait_ops]
        for e in ENGS:
            waited = {}
            for op in self.eng_ops[e]:
                op.waits = []
                best = {}
                for d, kind in op.deps:
                    if d.is_dma:
                        key = ('c', id(d.ctr))
                        v = d.val
                    else:
                        if d.eng == e and kind != 'raw' and e == 'pe':
                            continue
                        key = ('e', d.eng)
                        v = d.pos
                    if waited.get(key, 0) >= v:
                        continue
                    if key not in best or best[key][0] < v:
                        best[key] = (v, d)
                for key, (v, d) in best.items():
                    waited[key] = v
                    d.signal = True
                    op.waits.append(d)
        for e in ENGS:
            seq = 0
            for op in self.eng_ops[e]:
                if op.is_dma:
                    op.ctr.need(self, op.val)
                elif op.signal:
                    seq += 1
                    op.seq = seq
                    self.engctr[e].need(self, seq)
        nc = self.nc

        def run(e, eng):
            for op in self.eng_ops[e]:
                for d in op.waits:
                    if d.is_dma:
                        sem, v = d.ctr.sem_for(d.val)
                    else:
                        sem, v = self.engctr[d.eng].sem_for(d.seq)
                    eng.wait_ge(sem, v)
                ins = op.fn(eng)
                if op.is_dma:
                    sem, _ = op.ctr.sem_for(op.val)
                    ins.then_inc(sem, 16)
                elif op.signal:
                    sem, _ = self.engctr[e].sem_for(op.seq)
                    ins.then_inc(sem, 1)

        with nc.Block() as block:
            @block.tensor
            def _(eng):
                run('pe', eng)

            @block.vector
            def _(eng):
                run('dve', eng)

            @block.scalar
            def _(eng):
                run('act', eng)

            @block.gpsimd
            def _(eng):
                run('pool', eng)

            @block.sync
            def _(eng):
                run('sp', eng)

    def stats(self):
        return {e: len(v) for e, v in self.eng_ops.items()}, self.nsem


def _apname(a):
    try:
        return a.name
    except Exception:
        return a.tensor.name


def I(P, eng, meth, wsub=None, rsub=None, extra_reads=(), extra_writes=(), **kw):
    reads, writes = list(extra_reads), list(extra_writes)
    for k, v in kw.items():
        if isinstance(v, bass.AP):
            nm = _apname(v)
            if k in ('out', 'accum_out'):
                writes.append((nm, wsub.get(nm) if isinstance(wsub, dict) else wsub))
            else:
                reads.append((nm, rsub.get(nm) if isinstance(rsub, dict) else rsub))
    return P.op(eng, (lambda e: getattr(e, meth)(**kw)), reads=reads, writes=writes)


def _key2(r):
    if isinstance(r, tuple):
        a, s = r
    else:
        a, s = r, None
    if isinstance(a, str):
        return a, s
    try:
        return a.name, s
    except Exception:
        return a.tensor.name, s


Prog._key = staticmethod(_key2)


D = 1024
DFF = 4096
RMS_EPS = 1e-6


def bcast_rows(ap1d, n):
    return ap1d.partition_broadcast(n)


def phase_mod(P, nc, cT, w_ada, b_ada, g_mix, g_ffn, mod_d, NB):
    with contextlib.ExitStack() as st:
        ct = P.sb([128, 8, NB], F32, 'ct', st)
        sil = P.sb([128, 8, NB], F32, 'sil', st)
        ones = P.sb([1, NB], F32, 'ones', st)
        mod = P.sb([NB, 6 * D], F32, 'mod', st)
        gb = P.sb([NB, 2, D], F32, 'gb', st)
        wa = [P.sb([128, 8, 512], F32, 'wa', st) for _ in range(2)]
        ba = [P.sb([1, 512], F32, 'ba', st) for _ in range(2)]
        pm = [P.ps([128, 512], F32, 'pm', st) for _ in range(2)]
        P.dma('sp', ct[:], cT, writes=[ct], ctr=ct)
        P.op('act', lambda e: e.activation(out=sil[:], in_=ct[:], func=AF.Silu), reads=[ct], writes=[sil])
        P.op('dve', lambda e: e.memset(ones[:], 1.0), writes=[ones])
        P.dma('sp', gb[:, 0, :], g_mix.partition_broadcast(NB), writes=[(gb, 0)], ctr=gb)
        P.dma('sp', gb[:, 1, :], g_ffn.partition_broadcast(NB), writes=[(gb, 1)], ctr=gb)
        for cg in range(12):
            w = wa[cg % 2]
            bb = ba[cg % 2]
            pp = pm[cg % 2]
            P.dma('sp', w[:], w_ada[:, cg * 512:(cg + 1) * 512].rearrange("(k p) e -> p k e", p=128),
                  writes=[w], ctr=w)
            P.dma('sp', bb[:], b_ada[cg * 512:(cg + 1) * 512].partition_broadcast(1), writes=[bb], ctr=bb)
            for k in range(8):
                P.op('pe', (lambda e, k=k, w=w, pp=pp: e.matmul(pp[0:NB, :], sil[:, k, :], w[:, k, :], start=(k == 0), stop=False)),
                     reads=[sil, w], writes=[pp])
            P.op('pe', (lambda e, bb=bb, pp=pp: e.matmul(pp[0:NB, :], ones[:, :], bb[:, :], start=False, stop=True)),
                 reads=[ones, bb], writes=[pp])
            P.op('act', (lambda e, pp=pp, cg=cg: e.activation(out=mod[:, cg * 512:(cg + 1) * 512], in_=pp[0:NB, :], func=AF.Copy)),
                 reads=[pp], writes=[(mod, cg)])
        for r, gi in ((1, 0), (4, 1)):
            P.op('dve', (lambda e, r=r, gi=gi: e.scalar_tensor_tensor(out=mod[:, r * D:(r + 1) * D], in0=mod[:, r * D:(r + 1) * D],
                                                                     scalar=1.0, in1=gb[:, gi, :], op0=ALU.add, op1=ALU.mult)),
                 reads=[mod, gb], writes=[mod])
        o = P.dma('sp', mod_d.rearrange("b r d -> b (r d)"), mod[:], reads=[mod], writes=[mod_d], ctr=mod)
    return o


def phase_ffn(P, nc, xin, xout, w_ff1, w_ff2, mod_d, ident_d, NB, S):
    NT = NB * S
    TG = 256
    outs = []
    with contextlib.ExitStack() as st:
        w1 = P.sb([128, 8, DFF], BF16, 'w1', st)
        w2 = P.sb([128, 32, D], BF16, 'w2', st)
        ident = P.sb([128, 128], BF16, 'ident', st)
        sh2f = P.sb([128, 8, NB], F32, 'sh2f', st)
        sh2b = P.sb([128, 8, NB], BF16, 'sh2b', st)
        bias1 = P.sb([128, 32, NB], F32, 'bias1', st)
        G2bc = P.sb([128, D], F32, 'G2bc', st)
        gt2bc = P.sb([128, D], F32, 'gt2bc', st)
        xt = [P.sb([128, D], F32, 'xt', st) for _ in range(4)]
        xn = [P.sb([128, D], BF16, 'xn', st) for _ in range(2)]
        junk = P.sb([128, D], BF16, 'junk', st)
        epsc = P.sb([128, 1], F32, 'epsc', st)
        P.op('dve', lambda e: e.memset(epsc[:], RMS_EPS), writes=[epsc])
        ss = [P.sb([128, 2], F32, 'ss', st) for _ in range(2)]
        h2T = [P.sb([128, 8, TG], BF16, 'h2T', st) for _ in range(2)]
        u2T = P.sb([128, 32, TG], BF16, 'u2T', st)
        rr = [P.sb([128, TG], BF16, 'rr', st) for _ in range(2)]
        tmp = [P.sb([128, 512], F32, 'tmp', st) for _ in range(2)]
        tp = [P.ps([128, 1024], BF16, 'tp', st) for _ in range(2)]
        ps1 = [P.ps([128, 512], F32, 'ps1', st) for _ in range(2)]
        ps2 = [P.ps([128, 512], F32, 'ps2', st) for _ in range(2)]

        P.dma('pool', ident[:], ident_d, writes=[ident], ctr=ident)
        for k in range(8):
            P.dma('pool', w1[:, k, :], w_ff1[k * 128:(k + 1) * 128, :], writes=[(w1, k)], ctr=w1)
        for c in range(32):
            P.dma('pool', w2[:, c, :], w_ff2[c * 128:(c + 1) * 128, :], writes=[(w2, c)], ctr=w2)
        for b_ in range(NB):
            P.dma('sp', sh2f[:, :, b_], mod_d[b_, 3, :].rearrange("(k p) -> p k", p=128), writes=[(sh2f, b_)], ctr=sh2f,
                  allow_slow_non_contiguous=True)
        P.op('dve', lambda e: e.tensor_copy(out=sh2b[:], in_=sh2f[:]), reads=[sh2f], writes=[sh2b])
        pb = ps1[0]
        for c in range(32):
            for k in range(8):
                P.op('pe', (lambda e, c=c, k=k: e.matmul(pb[:, c * NB:(c + 1) * NB], w1[:, k, c * 128:(c + 1) * 128], sh2b[:, k, :],
                                                        start=(k == 0), stop=(k == 7))),
                     reads=[w1, sh2b], writes=[pb])
        P.op('act', lambda e: e.activation(out=bias1[:].rearrange("p c b -> p (c b)"), in_=pb[:, 0:32 * NB], func=AF.Copy),
             reads=[pb], writes=[bias1])

        ng = NT // TG
        cur_b = -1
        for g in range(ng):
            b = (g * TG) // S
            if b != cur_b:
                cur_b = b
                P.dma('sp', G2bc[:], mod_d[b, 4, :].partition_broadcast(128), writes=[G2bc], ctr=G2bc)
                P.dma('sp', gt2bc[:], mod_d[b, 5, :].partition_broadcast(128), writes=[gt2bc], ctr=gt2bc)
            hT = h2T[g % 2]
            xs = [xt[(g % 2) * 2 + j] for j in range(2)]
            for j in range(2):
                r0 = g * TG + j * 128
                x = xs[j]
                P.dma('sp', x[:], xin[r0:r0 + 128, :], writes=[x], ctr=x)
                s_ = ss[j]
                xb = xn[j]
                tpp = tp[j]
                P.op('act', (lambda e, x=x, s_=s_: e.activation(out=junk[:], in_=x[:], func=AF.Square, accum_out=s_[:, 0:1])),
                     reads=[x], writes=[junk, s_])
                P.op('act', (lambda e, s_=s_: e.activation(out=s_[:, 1:2], in_=s_[:, 0:1], func=AF.Sqrt, scale=1.0 / D, bias=epsc[:, 0:1])),
                     reads=[s_, epsc], writes=[s_])
                P.op('dve', (lambda e, s_=s_: e.reciprocal(out=s_[:, 1:2], in_=s_[:, 1:2])), reads=[s_], writes=[s_])
                P.op('dve', (lambda e, x=x, s_=s_, xb=xb: e.scalar_tensor_tensor(out=xb[:], in0=x[:], scalar=s_[:, 1:2], in1=G2bc[:],
                                                                               op0=ALU.mult, op1=ALU.mult)),
                     reads=[x, s_, G2bc], writes=[xb])
                for k in range(8):
                    P.op('pe', (lambda e, k=k, xb=xb, tpp=tpp: e.transpose(out=tpp[:, k * 128:(k + 1) * 128], in_=xb[:, k * 128:(k + 1) * 128],
                                                                        identity=ident[:])),
                         reads=[xb, ident], writes=[tpp])
                P.op('act', (lambda e, tpp=tpp, hT=hT, j=j: e.activation(out=hT[:, :, j * 128:(j + 1) * 128],
                                                                       in_=tpp[:].rearrange("p (k t) -> p k t", k=8), func=AF.Copy)),
                     reads=[tpp], writes=[(hT, j)])
            for c in range(32):
                pp = ps1[c % 2]
                r_ = rr[c % 2]
                for k in range(8):
                    P.op('pe', (lambda e, c=c, k=k, pp=pp, hT=hT: e.matmul(pp[:, 0:TG], w1[:, k, c * 128:(c + 1) * 128], hT[:, k, :],
                                                                        start=(k == 0), stop=(k == 7))),
                         reads=[w1, hT], writes=[pp])
                P.op('act', (lambda e, c=c, pp=pp, r_=r_, b=b: e.activation(out=r_[:], in_=pp[:, 0:TG], func=AF.Relu, bias=bias1[:, c, b:b + 1])),
                     reads=[pp, bias1], writes=[r_])
                P.op('dve', (lambda e, c=c, r_=r_: e.tensor_tensor(out=u2T[:, c, :], in0=r_[:], in1=r_[:], op=ALU.mult)),
                     reads=[r_], writes=[(u2T, c)])
            for j in range(2):
                x = xs[j]
                for hf in range(2):
                    pp = ps2[hf]
                    tm = tmp[hf]
                    for c in range(32):
                        P.op('pe', (lambda e, c=c, j=j, hf=hf, pp=pp: e.matmul(pp[:], u2T[:, c, j * 128:(j + 1) * 128], w2[:, c, hf * 512:(hf + 1) * 512],
                                                                            start=(c == 0), stop=(c == 31))),
                             reads=[(u2T, c), w2], writes=[pp])
                    P.op('dve', (lambda e, pp=pp, tm=tm, hf=hf: e.tensor_tensor(out=tm[:], in0=pp[:], in1=gt2bc[:, hf * 512:(hf + 1) * 512], op=ALU.mult)),
                         reads=[pp, gt2bc], writes=[tm])
                    P.op('dve', (lambda e, x=x, tm=tm, hf=hf: e.tensor_tensor(out=x[:, hf * 512:(hf + 1) * 512], in0=tm[:], in1=x[:, hf * 512:(hf + 1) * 512], op=ALU.add)),
                         reads=[tm, x], writes=[x])
                r0 = g * TG + j * 128
                outs.append(P.dma('pool', xout[r0:r0 + 128, :], x[:], reads=[x], writes=[(xout, r0)], ctr=x))
    return outs


NPROJ = 3072


def phase_inproj(P, nc, x, projT, widxT, w_inp, w_widx, mu_d, mod_d, ident_d, NB, S):
    NT = NB * S
    TG = 512
    with contextlib.ExitStack() as st:
        win = P.sb([128, 8, NPROJ], BF16, 'win', st)
        wwx = P.sb([128, 8, 8], BF16, 'wwx', st)
        ident = P.sb([128, 128], BF16, 'ident', st)
        sh1f = P.sb([128, 8, NB], F32, 'sh1f', st)
        sh1b = P.sb([128, 8, NB], BF16, 'sh1b', st)
        bias = P.sb([128, 25, NB], F32, 'bias', st)
        mu = P.sb([128, 14], F32, 'mu', st)
        G1bc = P.sb([128, D], F32, 'G1bc', st)
        epsc = P.sb([128, 1], F32, 'epsc', st)
        xt = [P.sb([128, D], F32, 'xt', st) for _ in range(2)]
        xn = [P.sb([128, D], BF16, 'xn', st) for _ in range(2)]
        junk = P.sb([128, D], BF16, 'junk', st)
        ss = [P.sb([128, 2], F32, 'ss', st) for _ in range(2)]
        hT = [P.sb([128, 8, TG], BF16, 'hT', st) for _ in range(2)]
        pb = [P.sb([128, TG + 1], BF16, 'pb', st) for _ in range(14)]
        tmpd = [P.sb([128, TG], BF16, 'tmpd', st) for _ in range(2)]
        stg = [P.sb([128, TG], BF16, 'stg', st) for _ in range(3)]
        stw = [P.sb([8, TG], F32, 'stw', st) for _ in range(2)]
        tp = [P.ps([128, 1024], BF16, 'tp', st) for _ in range(2)]
        pp = [P.ps([128, 512], F32, 'pp', st) for _ in range(3)]

        I(P, 'dve', 'memset', ap=epsc[:], constant=RMS_EPS, extra_writes=[epsc])
        P.dma('pool', ident[:], ident_d, writes=[ident], ctr=ident)
        for k in range(8):
            P.dma('pool', win[:, k, :], w_inp[k * 128:(k + 1) * 128, :], writes=[(win, k)], ctr=win)
        P.dma('pool', wwx[:], w_widx.rearrange("(k p) e -> p k e", p=128), writes=[wwx], ctr=wwx)
        P.dma('sp', mu[:], mu_d, writes=[mu], ctr=mu)
        for b_ in range(NB):
            P.dma('sp', sh1f[:, :, b_], mod_d[b_, 0, :].rearrange("(k p) -> p k", p=128), writes=[(sh1f, b_)], ctr=sh1f,
                  allow_slow_non_contiguous=True)
        I(P, 'dve', 'tensor_copy', out=sh1b[:], in_=sh1f[:])
        pbm = pp[0]
        for cg in range(25):
            for k in range(8):
                if cg < 24:
                    I(P, 'pe', 'matmul', out=pbm[:, cg * NB:(cg + 1) * NB], lhsT=win[:, k, cg * 128:(cg + 1) * 128], rhs=sh1b[:, k, :],
                      start=(k == 0), stop=(k == 7))
                else:
                    I(P, 'pe', 'matmul', out=pbm[0:8, cg * NB:(cg + 1) * NB], lhsT=wwx[:, k, :], rhs=sh1b[:, k, :],
                      start=(k == 0), stop=(k == 7))
        I(P, 'act', 'activation', out=bias[:, 0:24, :].rearrange("p c b -> p (c b)"), in_=pbm[:, 0:24 * NB], func=AF.Copy)
        I(P, 'act', 'activation', out=bias[0:8, 24, :], in_=pbm[0:8, 24 * NB:25 * NB], func=AF.Copy)

        ng = NT // TG
        cur_b = -1
        si = 0
        for g in range(ng):
            tok0 = g * TG
            b = tok0 // S
            seq_start = (tok0 % S == 0)
            if b != cur_b:
                cur_b = b
                P.dma('sp', G1bc[:], mod_d[b, 1, :].partition_broadcast(128), writes=[G1bc], ctr=G1bc)
            h = hT[g % 2]
            for j in range(4):
                r0 = tok0 + j * 128
                xx = xt[j % 2]
                s_ = ss[j % 2]
                xb = xn[j % 2]
                tpp = tp[j % 2]
                P.dma('sp', xx[:], x[r0:r0 + 128, :], writes=[xx], ctr=xx)
                I(P, 'act', 'activation', out=junk[:], in_=xx[:], func=AF.Square, accum_out=s_[:, 0:1])
                I(P, 'act', 'activation', out=s_[:, 1:2], in_=s_[:, 0:1], func=AF.Sqrt, scale=1.0 / D, bias=epsc[:, 0:1])
                I(P, 'dve', 'reciprocal', out=s_[:, 1:2], in_=s_[:, 1:2])
                I(P, 'dve', 'scalar_tensor_tensor', out=xb[:], in0=xx[:], scalar=s_[:, 1:2], in1=G1bc[:], op0=ALU.mult, op1=ALU.mult)
                for k in range(8):
                    I(P, 'pe', 'transpose', out=tpp[:, k * 128:(k + 1) * 128], in_=xb[:, k * 128:(k + 1) * 128], identity=ident[:])
                I(P, 'act', 'activation', out=h[:, :, j * 128:(j + 1) * 128], in_=tpp[:].rearrange("p (k t) -> p k t", k=8), func=AF.Copy,
                  wsub=j)
            for cg in range(24):
                pq = pp[cg % 3]
                for k in range(8):
                    I(P, 'pe', 'matmul', out=pq[:], lhsT=win[:, k, cg * 128:(cg + 1) * 128], rhs=h[:, k, :], start=(k == 0), stop=(k == 7))
                sg = stg[si % 3]
                si += 1
                if cg < 10:
                    I(P, 'act', 'activation', out=sg[:], in_=pq[:], func=AF.Identity, bias=bias[:, cg, b:b + 1])
                else:
                    gi = cg - 10
                    pbb = pb[gi]
                    td = tmpd[gi % 2]
                    if seq_start:
                        I(P, 'dve', 'memset', ap=pbb[:, 0:1], constant=0.0, extra_writes=[pbb])
                    else:
                        I(P, 'dve', 'tensor_copy', out=pbb[:, 0:1], in_=pbb[:, TG:TG + 1])
                    I(P, 'act', 'activation', out=pbb[:, 1:TG + 1], in_=pq[:], func=AF.Identity, bias=bias[:, cg, b:b + 1])
                    I(P, 'dve', 'tensor_tensor', out=td[:], in0=pbb[:, 0:TG], in1=pbb[:, 1:TG + 1], op=ALU.subtract)
                    I(P, 'dve', 'scalar_tensor_tensor', out=sg[:], in0=td[:], scalar=mu[:, gi:gi + 1], in1=pbb[:, 1:TG + 1],
                      op0=ALU.mult, op1=ALU.add)
                P.dma('pool', projT[cg * 128:(cg + 1) * 128, tok0:tok0 + TG], sg[:], reads=[sg], writes=[(projT, (cg, g))], ctr=sg)
            pq = pp[0]
            for k in range(8):
                I(P, 'pe', 'matmul', out=pq[0:8, :], lhsT=wwx[:, k, :], rhs=h[:, k, :], start=(k == 0), stop=(k == 7))
            sw = stw[g % 2]
            I(P, 'act', 'activation', out=sw[:], in_=pq[0:8, :], func=AF.Identity, bias=bias[0:8, 24, b:b + 1])
            P.dma('pool', widxT[:, tok0:tok0 + TG], sw[:], reads=[sw], writes=[(widxT, g)], ctr=sw)


def phase_outproj(P, nc, x, xout, mixedT, w_out, mod_d, NB, S):
    NT = NB * S
    with contextlib.ExitStack() as st:
        wo = P.sb([128, 8, D], BF16, 'wo', st)
        gt1bc = P.sb([128, D], F32, 'gt1bc', st)
        mt = [P.sb([128, 8, 128], BF16, 'mt', st) for _ in range(2)]
        xt = [P.sb([128, D], F32, 'xt', st) for _ in range(2)]
        tmp = [P.sb([128, 512], F32, 'tmp', st) for _ in range(2)]
        pp = [P.ps([128, 512], F32, 'pp', st) for _ in range(2)]
        for k in range(8):
            P.dma('pool', wo[:, k, :], w_out[k * 128:(k + 1) * 128, :], writes=[(wo, k)], ctr=wo)
        cur_b = -1
        for i in range(NT // 128):
            r0 = i * 128
            b = r0 // S
            if b != cur_b:
                cur_b = b
                P.dma('sp', gt1bc[:], mod_d[b, 2, :].partition_broadcast(128), writes=[gt1bc], ctr=gt1bc)
            m = mt[i % 2]
            xx = xt[i % 2]
            P.dma('sp', m[:], mixedT[:, r0:r0 + 128].rearrange("(k p) t -> p k t", p=128), writes=[m], ctr=m)
            P.dma('sp', xx[:], x[r0:r0 + 128, :], writes=[xx], ctr=xx)
            for hf in range(2):
                pq = pp[hf]
                tm = tmp[hf]
                for k in range(8):
                    I(P, 'pe', 'matmul', out=pq[:], lhsT=m[:, k, :], rhs=wo[:, k, hf * 512:(hf + 1) * 512], start=(k == 0), stop=(k == 7))
                I(P, 'dve', 'tensor_tensor', out=tm[:], in0=pq[:], in1=gt1bc[:, hf * 512:(hf + 1) * 512], op=ALU.mult)
                I(P, 'dve', 'tensor_tensor', out=xx[:, hf * 512:(hf + 1) * 512], in0=tm[:], in1=xx[:, hf * 512:(hf + 1) * 512], op=ALU.add)
            P.dma('pool', xout[r0:r0 + 128, :], xx[:], reads=[xx], writes=[(xout, i)], ctr=xx)

STOP = 99

NEG = -30000.0


def phase_dsa(P, nc, projT, widxT, mixedT, C, NB, S, TOPK):
    NTL = S // 128
    NBLK = S // 512
    with contextlib.ExitStack() as st:
        sb = lambda shp, dt, nm: P.sb(shp, dt, nm, st)
        wuk = sb([128, 512], BF16, 'wuk'); wuv = sb([128, 512], BF16, 'wuv')
        ident = sb([128, 128], BF16, 'ident'); identf = sb([128, 128], F32, 'identf')
        ones = sb([128, 128], BF16, 'ones'); sel65 = sb([65, 64], BF16, 'sel65')
        ccorr = sb([128, 8, 128], BF16, 'ccorr'); dmask = sb([128, 128], F32, 'dmask')
        gq = sb([64, 1], F32, 'gq'); gk = sb([64, 1], F32, 'gk'); gqk = sb([64, 1], F32, 'gqk'); gkv = sb([128, 1], F32, 'gkv')
        epsc = sb([128, 1], F32, 'epsc')
        qaug = sb([68, 8, S], BF16, 'qaug'); kaug = sb([68, 8, S], BF16, 'kaug')
        qidx = sb([128, 4, S], BF16, 'qidx'); kidx = sb([128, S], BF16, 'kidx')
        ckv = sb([128, S], BF16, 'ckv'); vaug = sb([128, NTL, 8, 65], BF16, 'vaug')
        MbT2 = [sb([128, NTL, 512], BF16, 'MbT') for _ in range(2)]; scs = [sb([128, S], F32, 'sc') for _ in range(4)]; junk = [sb([128, S], BF16, 'junkS') for _ in range(2)]
        lo = sb([128, 4], F32, 'lo'); negt = sb([128, 4], F32, 'negt'); Sg = sb([128, 4], F32, 'Sg'); dd = sb([128, 4], F32, 'dd')
        Mb = sb([128, S], BF16, 'Mb'); mx = sb([128, 8], F32, 'mx'); thr = sb([128, 1], F32, 'thr')
        clat = sb([128, 512], BF16, 'clat'); sqc = sb([128, 512], BF16, 'sqc')
        qraw = scs[3][:].bitcast(BF16)[0:64, :].rearrange("p (h t) -> p h t", h=8)
        sqk = [sb([64, 512], BF16, 'sqk') for _ in range(2)]
        wT = sb([8, 128], F32, 'wT'); wv = sb([128, 8], F32, 'wv'); rl = [sb([128, 512], F32, 'rl') for _ in range(2)]
        rcb = rl[0]; rr = [rl[1][0:64, :], sb([64, 512], F32, 'rr')]
        Pt = [sb([128, 512], BF16, 'Pt') for _ in range(3)]
        Osb = sb([65, 512], BF16, 'Osb'); rden = sb([64, 512], F32, 'rden'); oa = [sb([64, 512], BF16, 'oa') for _ in range(2)]
        PB = [P.ps([128, 512], F32, 'PB', st) for _ in range(7)]
        psA = PB[0:3]
        psB = PB[3:5]
        I(P, 'dve', 'memset', ap=epsc[:], constant=RMS_EPS, extra_writes=[epsc])
        for t_, d_ in ((wuk, 'w_uk'), (wuv, 'w_uv'), (ident, 'ident'), (ones, 'ones128'), (sel65, 'sel65'), (ccorr, 'ccorr')):
            P.dma('pool', t_[:], C[d_], writes=[t_], ctr=t_)
        for t_, d_ in ((identf, 'ident'), (dmask, 'dmask'), (gq, 'g_q'), (gk, 'g_k'), (gkv, 'g_kv')):
            P.dma('sp', t_[:], C[d_], writes=[t_], ctr=t_)
        I(P, 'dve', 'tensor_tensor', out=gqk[:], in0=gq[:], in1=gk[:], op=ALU.mult)
        I(P, 'dve', 'tensor_scalar', out=gqk[:], in0=gqk[:], scalar1=0.125, scalar2=None, op0=ALU.mult)
        for h in range(8):
            P.dma('pool', kaug[64:68, h, :], C['kaug'], writes=[(kaug, ('a', h))], ctr=(kaug, 'a'))
            P.dma('pool', qaug[64:68, h, :], C['qaug'][h], writes=[(qaug, ('a', h))], ctr=(qaug, 'a'))
        I(P, 'dve', 'memset', ap=vaug[:, :, :, 64:65], constant=1.0, extra_writes=[(vaug, 'one')])
        for b in range(NB):
            T0 = b * S
            P.dma('sp', qidx[:], projT[512:1024, T0:T0 + S].rearrange("(m p) t -> p m t", p=128), writes=[qidx], ctr=qidx)
            P.dma('sp', kidx[:], projT[1152:1280, T0:T0 + S], writes=[kidx], ctr=kidx)
            for blk in range(NBLK):
                c0 = blk * 512
                P.dma('sp', clat[:], projT[1024:1152, T0 + c0:T0 + c0 + 512], writes=[clat], ctr=clat)
                P.dma('sp', qraw[:], projT[0:512, T0 + c0:T0 + c0 + 512].rearrange("(h p) t -> p h t", p=64), writes=[qraw], ctr=qraw)
                I(P, 'dve', 'tensor_tensor', out=sqc[:], in0=clat[:], in1=clat[:], op=ALU.mult)
                pa = psA[0]
                I(P, 'pe', 'matmul', out=pa[:], lhsT=ones[:], rhs=sqc[:], start=True, stop=True)
                I(P, 'act', 'activation', out=rcb[:], in_=pa[:], func=AF.Sqrt, scale=1.0 / 128, bias=epsc[:, 0:1])
                I(P, 'dve', 'reciprocal', out=rcb[:], in_=rcb[:])
                I(P, 'dve', 'scalar_tensor_tensor', out=ckv[:, c0:c0 + 512], in0=clat[:], scalar=gkv[:, 0:1], in1=rcb[:],
                  op0=ALU.mult, op1=ALU.mult, wsub=blk)
                for j in range(4):
                    ti = blk * 4 + j
                    pv = psB[j % 2]
                    I(P, 'pe', 'matmul', out=pv[:], lhsT=ckv[:, ti * 128:(ti + 1) * 128], rhs=wuv[:], start=True, stop=True)
                    I(P, 'act', 'activation', out=vaug[:, ti, :, 0:64], in_=pv[:].rearrange("p (h d) -> p h d", h=8), func=AF.Copy,
                      wsub=('v', ti))
                for h in range(8):
                    pk = psA[1 + h % 2]; p2 = psB[h % 2]; sq = sqk[h % 2]; r_ = rr[h % 2]
                    I(P, 'pe', 'matmul', out=pk[0:64, :], lhsT=wuk[:, h * 64:(h + 1) * 64], rhs=ckv[:, c0:c0 + 512], start=True, stop=True)
                    I(P, 'act', 'activation', out=sq[:], in_=pk[0:64, :], func=AF.Square)
                    I(P, 'pe', 'matmul', out=p2[0:64, :], lhsT=ones[0:64, 0:64], rhs=sq[:], start=True, stop=True)
                    I(P, 'act', 'activation', out=r_[:], in_=p2[0:64, :], func=AF.Sqrt, scale=1.0 / 64, bias=epsc[0:64, 0:1])
                    I(P, 'dve', 'reciprocal', out=r_[:], in_=r_[:])
                    I(P, 'dve', 'tensor_tensor', out=kaug[0:64, h, c0:c0 + 512], in0=pk[0:64, :], in1=r_[:], op=ALU.mult, wsub=('k', h, blk))
                    p3 = psB[h % 2]
                    I(P, 'act', 'activation', out=sq[:], in_=qraw[:, h, :], func=AF.Square)
                    I(P, 'pe', 'matmul', out=p3[0:64, :], lhsT=ones[0:64, 0:64], rhs=sq[:], start=True, stop=True)
                    I(P, 'act', 'activation', out=r_[:], in_=p3[0:64, :], func=AF.Sqrt, scale=1.0 / 64, bias=epsc[0:64, 0:1])
                    I(P, 'dve', 'reciprocal', out=r_[:], in_=r_[:])
                    I(P, 'dve', 'scalar_tensor_tensor', out=qaug[0:64, h, c0:c0 + 512], in0=qraw[:, h, :], scalar=gqk[:, 0:1], in1=r_[:],
                      op0=ALU.mult, op1=ALU.mult, wsub=('q', h, blk))
            if STOP <= 1:
                continue
            if STOP <= 1:
                continue
            def prep(q, T0=T0):
                MbT = MbT2[q % 2]
                I(P, 'dve', 'memset', ap=MbT[:], constant=NEG, extra_writes=[MbT])
                RNG = 1024.0
                NIT = 26
                for ti in range(4):
                    i = q * 4 + ti
                    L = 128 * (i + 1)
                    sc = scs[ti]
                    P.dma('sp', wT[:], widxT[:, T0 + i * 128:T0 + (i + 1) * 128], writes=[wT], ctr=wT)
                    pw = PB[4]
                    I(P, 'pe', 'transpose', out=pw[:, 0:8], in_=wT[:], identity=identf[0:8, 0:8])
                    I(P, 'act', 'activation', out=wv[:], in_=pw[:, 0:8], func=AF.Copy)
                    nkb = (L + 511) // 512
                    for kb in range(nkb):
                        cols = min(512, L - kb * 512)
                        for h in range(8):
                            base = (h % 2) * 64; m = h // 2
                            pi = PB[2 + h % 2]; r_ = rl[h % 2]
                            I(P, 'pe', 'matmul', out=pi[:, 0:cols], lhsT=qidx[base:base + 64, m, i * 128:(i + 1) * 128],
                              rhs=kidx[base:base + 64, kb * 512:kb * 512 + cols], start=True, stop=True)
                            I(P, 'act', 'activation', out=r_[:, 0:cols], in_=pi[:, 0:cols], func=AF.Relu)
                            if h == 0:
                                I(P, 'dve', 'tensor_scalar', out=sc[:, kb * 512:kb * 512 + cols], in0=r_[:, 0:cols], scalar1=wv[:, 0:1],
                                  scalar2=None, op0=ALU.mult)
                            else:
                                I(P, 'dve', 'scalar_tensor_tensor', out=sc[:, kb * 512:kb * 512 + cols], in0=r_[:, 0:cols],
                                  scalar=wv[:, h:h + 1], in1=sc[:, kb * 512:kb * 512 + cols], op0=ALU.mult, op1=ALU.add)
                    I(P, 'dve', 'tensor_tensor', out=sc[:, L - 128:L], in0=sc[:, L - 128:L], in1=dmask[:], op=ALU.add)
                    yield 1
                act_t = [ti for ti in range(4) if 128 * (q * 4 + ti + 1) > TOPK]
                if act_t:
                    I(P, 'dve', 'memset', ap=lo[:], constant=-RNG, extra_writes=[lo])
                    for k in range(NIT):
                        s_k = RNG / (2.0 ** k)
                        for ti in act_t:
                            L = 128 * (q * 4 + ti + 1)
                            I(P, 'dve', 'tensor_scalar', out=negt[:, ti:ti + 1], in0=lo[:, ti:ti + 1], scalar1=-1.0, scalar2=-s_k,
                              op0=ALU.mult, op1=ALU.add, wsub=ti, rsub=ti)
                            I(P, 'act', 'activation', out=junk[ti % 2][:, 0:L], in_=scs[ti][:, 0:L], func=AF.Sign, bias=negt[:, ti:ti + 1],
                              accum_out=Sg[:, ti:ti + 1], wsub={Sg.name: ti}, rsub=ti)
                        if k % 3 == 2:
                            yield 1
                        for ti in act_t:
                            L = 128 * (q * 4 + ti + 1)
                            I(P, 'dve', 'tensor_scalar', out=dd[:, ti:ti + 1], in0=Sg[:, ti:ti + 1], scalar1=float(2 * TOPK - L), scalar2=None,
                              op0=ALU.is_ge, wsub=ti, rsub=ti)
                            I(P, 'dve', 'scalar_tensor_tensor', out=lo[:, ti:ti + 1], in0=dd[:, ti:ti + 1], scalar=s_k, in1=lo[:, ti:ti + 1],
                              op0=ALU.mult, op1=ALU.add, wsub=ti, rsub=ti)
                for ti in range(4):
                    i = q * 4 + ti
                    L = 128 * (i + 1)
                    sc = scs[ti]
                    if L > TOPK:
                        I(P, 'dve', 'tensor_scalar', out=Mb[:, 0:L], in0=sc[:, 0:L], scalar1=lo[:, ti:ti + 1], scalar2=NEG, op0=ALU.is_le, op1=ALU.mult)
                    else:
                        I(P, 'dve', 'tensor_scalar', out=Mb[:, 0:L], in0=sc[:, 0:L], scalar1=-1e29, scalar2=NEG, op0=ALU.is_lt, op1=ALU.mult)
                    for j0 in range(0, i + 1, 8):
                        j1 = min(i + 1, j0 + 8)
                        pt = PB[6]
                        ptb = pt[:].bitcast(BF16)
                        for j in range(j0, j1):
                            I(P, 'pe', 'transpose', out=ptb[:, (j - j0) * 128:(j - j0 + 1) * 128], in_=Mb[:, j * 128:(j + 1) * 128], identity=ident[:])
                        I(P, 'act', 'activation', out=MbT[:, j0:j1, ti * 128:(ti + 1) * 128],
                          in_=ptb[:, 0:(j1 - j0) * 128].rearrange("p (j t) -> p j t", t=128), func=AF.Copy, wsub=('t', i, j0))
                    yield 1

            def attn(q, T0=T0, b=b):
                MbT = MbT2[q % 2]
                nsb = 4 * q + 4
                for h in range(8):
                    po = PB[5]
                    for j in range(nsb):
                        pl = PB[j % 2]; pt_ = Pt[j % 3]
                        I(P, 'pe', 'matmul', out=pl[:], lhsT=kaug[0:68, h, j * 128:(j + 1) * 128], rhs=qaug[0:68, h, q * 512:(q + 1) * 512],
                          start=True, stop=False)
                        diag = j >= 4 * q
                        I(P, 'pe', 'matmul', out=pl[:], lhsT=ident[:], rhs=MbT[:, j, :], start=False, stop=not diag)
                        if diag:
                            dj = j - 4 * q
                            I(P, 'pe', 'matmul', out=pl[:, dj * 128:(dj + 1) * 128], lhsT=ident[:], rhs=ccorr[:, h, :], start=False, stop=True)
                        I(P, 'act', 'activation', out=pt_[:], in_=pl[:], func=AF.Exp)
                        I(P, 'pe', 'matmul', out=po[0:65, :], lhsT=vaug[:, j, h, :], rhs=pt_[:], start=(j == 0), stop=(j == nsb - 1))
                        if j % 4 == 3 and j != nsb - 1:
                            yield 1
                    I(P, 'act', 'activation', out=Osb[:], in_=po[0:65, :], func=AF.Copy)
                    pd = PB[4]
                    I(P, 'pe', 'matmul', out=pd[0:64, :], lhsT=sel65[:], rhs=Osb[:], start=True, stop=True)
                    I(P, 'dve', 'reciprocal', out=rden[:], in_=pd[0:64, :])
                    o_ = oa[h % 2]
                    I(P, 'dve', 'tensor_tensor', out=o_[:], in0=Osb[0:64, :], in1=rden[:], op=ALU.mult)
                    P.dma('pool', mixedT[h * 64:(h + 1) * 64, T0 + q * 512:T0 + (q + 1) * 512], o_[:], reads=[o_], writes=[(mixedT, (h, b, q))], ctr=o_)
                    yield 1

            for _ in prep(0):
                pass
            for q in range(NBLK):
                ga = attn(q)
                gp = prep(q + 1) if q + 1 < NBLK else iter(())
                da = dp = False
                while not (da and dp):
                    if not dp and next(gp, None) is None:
                        dp = True
                    if not da and next(ga, None) is None:
                        da = True

RSTOP = 99

GN_EPS = 64e-5


def phase_rwkv(P, nc, projT, mixedT, C, NB, S):
    TGR = 256
    NCH = 4
    NU = NCH * 8
    c0 = -float(np.exp(-0.5))
    with contextlib.ExitStack() as st:
        sb = lambda shp, dt, nm: P.sb(shp, dt, nm, st)
        w2b = sb([64, 512], BF16, 'w2b'); a2b = sb([64, 512], BF16, 'a2b'); g2b = sb([128, 512], BF16, 'g2b')
        vec = sb([64, 5, 8], F32, 'vec'); omk = sb([64, 8], F32, 'omk')
        lnw = sb([64, 512], F32, 'lnw'); lnb = sb([64, 512], F32, 'lnb'); rkr = sb([64, 512], F32, 'rkr')
        ones = sb([128, 128], BF16, 'onesr'); ident = sb([128, 128], BF16, 'identr')
        mask1 = sb([64, 128], F32, 'mask1'); mask3 = sb([64, 64], F32, 'mask3'); rst = sb([64, TGR], F32, 'rst')
        gneps = sb([64, 1], F32, 'gneps')
        F3 = [64, 8, TGR]
        rT = sb(F3, BF16, 'rT'); kT = sb(F3, BF16, 'kT'); vT = sb(F3, BF16, 'vT')
        xw = sb([64, TGR], BF16, 'xw'); xa = sb([64, TGR], BF16, 'xa'); xg = sb([128, TGR], BF16, 'xg')
        thb = sb([64, TGR], BF16, 'thb'); sgx = sb([128, TGR], BF16, 'sgx')
        sg = sb(F3, F32, 'sg'); cs = sb(F3, F32, 'cs'); Ea = sb(F3, F32, 'Ea'); Eb = sb(F3, F32, 'Eb')
        a_ = sb(F3, BF16, 'a_'); kkr = sb(F3, F32, 'kkr'); sq = sb(F3, BF16, 'sq')
        kk = sb(F3, F32, 'kk'); t1 = sb(F3, F32, 't1'); nrm = t1
        kmod = sb(F3, F32, 'kmod'); beta = kkr; gT = sb(F3, BF16, 'gT')
        bp = sb(F3, BF16, 'bp'); kp = sb(F3, BF16, 'kp'); kmb = sq
        AR = sb([64, 8, NCH, 2, 64], BF16, 'AR'); BK = sb([64, 8, NCH, 2, 64], BF16, 'BK'); GCt = sb([64, 8, NCH], F32, 'GCt')
        T3 = [64, NCH, 512]
        bpTM = sb(T3, BF16, 'bpTM'); kpTM = sb(T3, BF16, 'kpTM'); vTM = sb(T3, BF16, 'vTM')
        rTM = sb(T3, BF16, 'rTM'); kmTM = sb(T3, BF16, 'kmTM'); gTM = sb(T3, BF16, 'gTM')
        NM1 = [sb([64, 8, 128], BF16, 'NM1') for _ in range(NCH)]; NM2 = [sb([64, 8, 128], BF16, 'NM2') for _ in range(NCH)]
        NTt = [sb([64, 8, 64], BF16, 'NTt') for _ in range(NCH)]; X = [sb([64, 8, 64], BF16, 'X') for _ in range(NCH)]
        Pm = [[sb([64, 8, 64], BF16, 'Pm')] * 2 for _ in range(NCH)]; PTm = [[sb([64, 8, 64], BF16, 'PTm')] * 2 for _ in range(NCH)]
        Ysb = sb([64, 512], BF16, 'Ysb'); Usb = sb([64, 512], BF16, 'Usb')
        hst = sb([64, 8, 64], F32, 'hst'); hs = sb([64, 8, 64], F32, 'hs'); hbf = sb([64, 8, 64], BF16, 'hbf')
        OTM = sb(T3, F32, 'OTM')
        s1 = sb([64, NU], F32, 's1'); s2 = sb([64, NU], F32, 's2'); bs = sb([64, NU], F32, 'bs')
        ob = sb(T3, BF16, 'ob'); obT = [sb(F3, BF16, 'obT')] * 2
        PS = [P.ps([128, 512], F32, 'PSr', st) for _ in range(7)]
        cnt = [0]

        def nxt():
            cnt[0] += 1
            return PS[cnt[0] % 7]

        for t_, d_ in ((w2b, 'w2'), (a2b, 'a2'), (g2b, 'g2'), (ones, 'ones128'), (ident, 'ident')):
            P.dma('pool', t_[:], C[d_], writes=[t_], ctr=t_)
        for t_, d_ in ((vec, 'vec8'), (mask1, 'mask1'), (mask3, 'mask3')):
            P.dma('sp', t_[:], C[d_], writes=[t_], ctr=t_)
        P.dma('sp', rst[:], C['rst'][0:64, :], writes=[rst], ctr=rst)
        for t_, d_ in ((lnw, 'ln_w'), (lnb, 'ln_b'), (rkr, 'r_k1')):
            P.dma('sp', t_[:], C[d_].partition_broadcast(64), writes=[t_], ctr=t_)
        I(P, 'dve', 'memset', ap=gneps[:], constant=GN_EPS, extra_writes=[gneps])
        I(P, 'dve', 'tensor_scalar', out=omk[:], in0=vec[:, 3, :], scalar1=-1.0, scalar2=1.0, op0=ALU.mult, op1=ALU.add)

        def v3(t, h):
            return t[:, h, :].rearrange("p (n c) -> p n c", c=64)

        def fm_load(t_, r0, tok):
            P.dma('sp', t_[:], projT[r0:r0 + 512, tok].rearrange("(h p) t -> p h t", p=64), writes=[t_], ctr=t_)

        gi = 0
        for b in range(NB):
            I(P, 'dve', 'memset', ap=hst[:], constant=0.0, extra_writes=[hst])
            I(P, 'dve', 'memset', ap=hbf[:], constant=0.0, extra_writes=[hbf])
            for g in range(S // TGR):
                tok = b * S + g * TGR
                sl = slice(tok, tok + TGR)
                fm_load(rT, 1280, sl); fm_load(kT, 1792, sl); fm_load(vT, 2304, sl)
                P.dma('sp', xw[:], projT[2816:2880, sl], writes=[xw], ctr=xw)
                P.dma('sp', xa[:], projT[2880:2944, sl], writes=[xa], ctr=xa)
                P.dma('sp', xg[:], projT[2944:3072, sl], writes=[xg], ctr=xg)
                I(P, 'act', 'activation', out=thb[:], in_=xw[:], func=AF.Tanh)
                I(P, 'act', 'activation', out=sgx[:], in_=xg[:], func=AF.Sigmoid)
                for hp in range(4):
                    p1, p2, p3 = nxt(), nxt(), nxt()
                    for e2 in range(2):
                        h = hp * 2 + e2
                        hc = slice(h * 64, (h + 1) * 64)
                        oc = slice(e2 * TGR, (e2 + 1) * TGR)
                        I(P, 'pe', 'matmul', out=p1[0:64, oc], lhsT=w2b[:, hc], rhs=thb[:], start=True, stop=True)
                        I(P, 'pe', 'matmul', out=p2[0:64, oc], lhsT=a2b[:, hc], rhs=xa[:], start=True, stop=True)
                        I(P, 'pe', 'matmul', out=p3[0:64, oc], lhsT=g2b[:, hc], rhs=sgx[:], start=True, stop=True)
                    for e2 in range(2):
                        h = hp * 2 + e2
                        oc = slice(e2 * TGR, (e2 + 1) * TGR)
                        I(P, 'act', 'activation', out=sg[:, h, :], in_=p1[0:64, oc], func=AF.Sigmoid, bias=vec[:, 0, h:h + 1], wsub=h)
                        I(P, 'act', 'activation', out=a_[:, h, :], in_=p2[0:64, oc], func=AF.Sigmoid, bias=vec[:, 1, h:h + 1], wsub=h)
                    I(P, 'act', 'activation', out=gT[:, hp * 2:hp * 2 + 2, :], in_=p3[0:64, :].rearrange("p (e t) -> p e t", e=2), func=AF.Copy, wsub=hp)
                for h in range(8):
                    I(P, 'dve', 'tensor_scalar', out=kkr[:, h, :], in0=kT[:, h, :], scalar1=vec[:, 2, h:h + 1], scalar2=None, op0=ALU.mult, wsub=h)
                I(P, 'dve', 'tensor_tensor', out=sq[:], in0=kkr[:], in1=kkr[:], op=ALU.mult)
                for hp in range(4):
                    p1 = nxt()
                    for e2 in range(2):
                        h = hp * 2 + e2
                        I(P, 'pe', 'matmul', out=p1[0:64, e2 * TGR:(e2 + 1) * TGR], lhsT=ones[0:64, 0:64], rhs=sq[:, h, :], start=True, stop=True)
                    I(P, 'act', 'activation', out=nrm[:, hp * 2:hp * 2 + 2, :], in_=p1[0:64, :].rearrange("p (e t) -> p e t", e=2), func=AF.Sqrt, wsub=hp)
                I(P, 'dve', 'tensor_scalar', out=nrm[:], in0=nrm[:], scalar1=1e-12, scalar2=None, op0=ALU.max)
                I(P, 'dve', 'reciprocal', out=nrm[:], in_=nrm[:])
                I(P, 'dve', 'tensor_tensor', out=kk[:], in0=kkr[:], in1=nrm[:], op=ALU.mult)
                for h in range(8):
                    I(P, 'dve', 'tensor_scalar', out=t1[:, h, :], in0=a_[:, h, :], scalar1=vec[:, 3, h:h + 1], scalar2=omk[:, h:h + 1],
                      op0=ALU.mult, op1=ALU.add, wsub=h)
                I(P, 'dve', 'tensor_tensor', out=kmod[:], in0=kT[:], in1=t1[:], op=ALU.mult)
                I(P, 'dve', 'tensor_copy', out=kmb[:], in_=kmod[:])
                I(P, 'dve', 'tensor_tensor', out=beta[:], in0=kk[:], in1=a_[:], op=ALU.mult)
                for h in range(8):
                    I(P, 'dve', 'tensor_tensor_scan', out=cs[:, h, :], data0=rst[:], data1=sg[:, h, :], initial=0.0, op0=ALU.mult, op1=ALU.add,
                      wsub=h)
                I(P, 'act', 'activation', out=Ea[:], in_=cs[:], func=AF.Exp, scale=c0)
                for h in range(8):
                    I(P, 'dve', 'tensor_tensor', out=AR[:, h, :, 1, :], in0=v3(rT, h), in1=v3(Ea, h), op=ALU.mult, wsub=('r', h))
                I(P, 'dve', 'tensor_copy', out=GCt[:], in_=Ea[:].rearrange("p h (n c) -> p h n c", c=64)[:, :, :, 63])
                I(P, 'act', 'activation', out=Eb[:], in_=cs[:], func=AF.Exp, scale=-c0)
                for h in range(8):
                    I(P, 'dve', 'tensor_tensor', out=BK[:, h, :, 0, :], in0=v3(beta, h), in1=v3(Eb, h), op=ALU.mult, wsub=('b', h))
                    I(P, 'dve', 'tensor_tensor', out=BK[:, h, :, 1, :], in0=v3(kmod, h), in1=v3(Eb, h), op=ALU.mult, wsub=('k', h))
                I(P, 'dve', 'tensor_tensor', out=Ea[:], in0=cs[:], in1=sg[:], op=ALU.subtract)
                I(P, 'act', 'activation', out=Ea[:], in_=Ea[:], func=AF.Exp, scale=c0)
                for h in range(8):
                    I(P, 'dve', 'scalar_tensor_tensor', out=AR[:, h, :, 0, :], in0=v3(kk, h), scalar=-1.0, in1=v3(Ea, h), op0=ALU.mult, op1=ALU.mult,
                      wsub=('a', h))
                csv = cs[:].rearrange("p h (n c) -> p (h n) c", c=64)
                I(P, 'dve', 'tensor_tensor', out=Eb[:].rearrange("p h (n c) -> p (h n) c", c=64), in0=csv[:, :, 63:64].to_broadcast([64, 8 * NCH, 64]),
                  in1=csv, op=ALU.subtract)
                I(P, 'act', 'activation', out=Eb[:], in_=Eb[:], func=AF.Exp, scale=c0)
                I(P, 'dve', 'tensor_tensor', out=bp[:], in0=beta[:], in1=Eb[:], op=ALU.mult)
                I(P, 'dve', 'tensor_tensor', out=kp[:], in0=kmod[:], in1=Eb[:], op=ALU.mult)
                if RSTOP <= 1:
                    continue
                tcnt = 0
                for Xf, Xt in ((bp, bpTM), (kp, kpTM), (vT, vTM), (rT, rTM), (kmb, kmTM), (gT, gTM)):
                    for n in range(NCH):
                        ps = nxt()
                        pb_ = ps[:].bitcast(BF16)
                        for h in range(8):
                            I(P, 'pe', 'transpose', out=pb_[0:64, h * 64:(h + 1) * 64], in_=Xf[:, h, n * 64:(n + 1) * 64], identity=ident[0:64, 0:64])
                        tcnt += 1
                        if tcnt % 2:
                            I(P, 'act', 'activation', out=Xt[:, n, :], in_=pb_[0:64, 0:512], func=AF.Copy, wsub=n)
                        else:
                            I(P, 'dve', 'tensor_copy', out=Xt[:, n, :], in_=pb_[0:64, 0:512], wsub=n)
                if RSTOP <= 2:
                    continue
                Pcs = {}
                PTcs = {}
                for n in range(NCH):
                    nm1 = NM1[n]; nm2 = NM2[n]; ntt = NTt[n]; x_ = X[n]
                    for hq in range(2):
                        pa, pb2, pc = nxt(), nxt(), nxt()
                        for hh in range(4):
                            h = hq * 4 + hh
                            arv = AR[:, h, n, :, :].rearrange("p a c -> p (a c)")
                            I(P, 'pe', 'matmul', out=pa[0:64, hh * 128:(hh + 1) * 128], lhsT=BK[:, h, n, 0, :], rhs=arv, start=True, stop=True)
                            I(P, 'pe', 'matmul', out=pb2[0:64, hh * 128:(hh + 1) * 128], lhsT=BK[:, h, n, 1, :], rhs=arv, start=True, stop=True)
                            I(P, 'pe', 'matmul', out=pc[0:64, hh * 64:(hh + 1) * 64], lhsT=AR[:, h, n, 0, :], rhs=BK[:, h, n, 0, :], start=True, stop=True)
                        m1b = mask1[:].unsqueeze(1).to_broadcast([64, 4, 128])
                        I(P, 'dve', 'tensor_tensor', out=nm1[:, hq * 4:(hq + 1) * 4, :], in0=pa[0:64, :].rearrange("p (h c) -> p h c", c=128), in1=m1b,
                          op=ALU.mult, wsub=hq)
                        I(P, 'dve', 'tensor_tensor', out=nm2[:, hq * 4:(hq + 1) * 4, :], in0=pb2[0:64, :].rearrange("p (h c) -> p h c", c=128), in1=m1b,
                          op=ALU.mult, wsub=hq)
                        I(P, 'dve', 'tensor_tensor', out=ntt[:, hq * 4:(hq + 1) * 4, :], in0=pc[0:64, 0:256].rearrange("p (h c) -> p h c", c=64),
                          in1=mask3[:].unsqueeze(1).to_broadcast([64, 4, 64]), op=ALU.mult, wsub=hq)
                    I(P, 'dve', 'tensor_tensor', out=x_[:], in0=nm1[:, :, 0:64], in1=ident[0:64, 0:64].unsqueeze(1).to_broadcast([64, 8, 64]), op=ALU.add)
                    Pcs[n] = (lambda u, nm1=nm1: nm1[:, u, 0:64])
                    PTcs[n] = (lambda u, ntt=ntt: ntt[:, u, :])
                for k in range(6):
                    for n in range(NCH):
                        x_ = X[n]; Pc = Pcs[n]; PTc = PTcs[n]
                        if k >= 1:
                            pX = nxt()
                            for u in range(8):
                                I(P, 'pe', 'matmul', out=pX[0:64, u * 64:(u + 1) * 64], lhsT=PTc(u), rhs=x_[:, u, :], start=True, stop=True)
                        if k < 5:
                            pP, pPT = nxt(), nxt()
                            for u in range(8):
                                I(P, 'pe', 'matmul', out=pP[0:64, u * 64:(u + 1) * 64], lhsT=PTc(u), rhs=Pc(u), start=True, stop=True)
                                I(P, 'pe', 'matmul', out=pPT[0:64, u * 64:(u + 1) * 64], lhsT=Pc(u), rhs=PTc(u), start=True, stop=True)
                        if k >= 1:
                            I(P, 'dve', 'tensor_tensor', out=x_[:], in0=x_[:], in1=pX[0:64, :].rearrange("p (u c) -> p u c", c=64), op=ALU.add)
                        if k < 5:
                            pn = Pm[n][k % 2]; ptn = PTm[n][k % 2]
                            I(P, 'act', 'activation', out=pn[:], in_=pP[0:64, :].rearrange("p (u c) -> p u c", c=64), func=AF.Copy)
                            I(P, 'dve', 'tensor_copy', out=ptn[:], in_=pPT[0:64, :].rearrange("p (u c) -> p u c", c=64))
                            Pcs[n] = (lambda u, pn=pn: pn[:, u, :])
                            PTcs[n] = (lambda u, ptn=ptn: ptn[:, u, :])
                for n in range(NCH):
                    nm1 = NM1[n]; nm2 = NM2[n]; x_ = X[n]
                    if RSTOP <= 3:
                        continue
                    pY = nxt()
                    for h in range(8):
                        hc = slice(h * 64, (h + 1) * 64)
                        I(P, 'pe', 'matmul', out=pY[0:64, hc], lhsT=AR[:, h, n, 0, :], rhs=hbf[:, h, :], start=True, stop=False)
                        I(P, 'pe', 'matmul', out=pY[0:64, hc], lhsT=nm2[:, h, 0:64], rhs=vTM[:, n, hc], start=False, stop=True)
                    I(P, 'act', 'activation', out=Ysb[:], in_=pY[0:64, :], func=AF.Copy)
                    pU = nxt()
                    for h in range(8):
                        hc = slice(h * 64, (h + 1) * 64)
                        I(P, 'pe', 'matmul', out=pU[0:64, hc], lhsT=x_[:, h, :], rhs=Ysb[:, hc], start=True, stop=True)
                    I(P, 'dve', 'tensor_copy', out=Usb[:], in_=pU[0:64, :])
                    pO = nxt()
                    for h in range(8):
                        hc = slice(h * 64, (h + 1) * 64)
                        I(P, 'pe', 'matmul', out=pO[0:64, hc], lhsT=AR[:, h, n, 1, :], rhs=hbf[:, h, :], start=True, stop=False)
                        I(P, 'pe', 'matmul', out=pO[0:64, hc], lhsT=nm1[:, h, 64:128], rhs=Usb[:, hc], start=False, stop=False)
                        I(P, 'pe', 'matmul', out=pO[0:64, hc], lhsT=nm2[:, h, 64:128], rhs=vTM[:, n, hc], start=False, stop=True)
                    I(P, 'act', 'activation', out=OTM[:, n, :], in_=pO[0:64, :], func=AF.Copy, wsub=n)
                    pH = nxt()
                    for h in range(8):
                        hc = slice(h * 64, (h + 1) * 64)
                        I(P, 'pe', 'matmul', out=pH[0:64, hc], lhsT=bpTM[:, n, hc], rhs=Usb[:, hc], start=True, stop=False)
                        I(P, 'pe', 'matmul', out=pH[0:64, hc], lhsT=kpTM[:, n, hc], rhs=vTM[:, n, hc], start=False, stop=True)
                    I(P, 'dve', 'tensor_tensor', out=hs[:], in0=hst[:], in1=GCt[:, :, n:n + 1].to_broadcast([64, 8, 64]), op=ALU.mult)
                    I(P, 'dve', 'tensor_tensor', out=hst[:], in0=hs[:], in1=pH[0:64, :].rearrange("p (h c) -> p h c", c=64), op=ALU.add)
                    I(P, 'dve', 'tensor_copy', out=hbf[:], in_=hst[:])
                if RSTOP <= 4:
                    continue
                O3 = OTM[:].rearrange("p n (h d) -> p (n h) d", d=64)
                OcA = Ea[:].rearrange("p h t -> p (h t)").rearrange("p (n c) -> p n c", c=512)
                sqA = Eb[:].rearrange("p h t -> p (h t)").rearrange("p (n c) -> p n c", c=512)
                Oc3 = OcA.rearrange("p n (h d) -> p (n h) d", d=64)
                sq3 = sqA.rearrange("p n (h d) -> p (n h) d", d=64)
                bc = lambda t: t[:, :].unsqueeze(2).to_broadcast([64, NU, 64])
                I(P, 'dve', 'tensor_reduce', out=s1[:], in_=O3, axis=AX.X, op=ALU.add)
                I(P, 'dve', 'tensor_scalar', out=s1[:], in0=s1[:], scalar1=1.0 / 64, scalar2=None, op0=ALU.mult)
                I(P, 'dve', 'tensor_tensor', out=Oc3, in0=O3, in1=bc(s1), op=ALU.subtract)
                I(P, 'dve', 'tensor_tensor', out=sqA, in0=OcA, in1=OcA, op=ALU.mult)
                I(P, 'dve', 'tensor_reduce', out=s2[:], in_=sq3, axis=AX.X, op=ALU.add)
                I(P, 'act', 'activation', out=s2[:], in_=s2[:], func=AF.Sqrt, scale=1.0 / 64, bias=gneps[:, 0:1])
                I(P, 'dve', 'reciprocal', out=s2[:], in_=s2[:])
                I(P, 'dve', 'tensor_tensor', out=Oc3, in0=Oc3, in1=bc(s2), op=ALU.mult)
                I(P, 'dve', 'tensor_tensor', out=OcA, in0=OcA, in1=lnw[:].unsqueeze(1).to_broadcast([64, NCH, 512]), op=ALU.mult)
                I(P, 'dve', 'tensor_tensor', out=OcA, in0=OcA, in1=lnb[:].unsqueeze(1).to_broadcast([64, NCH, 512]), op=ALU.add)
                I(P, 'dve', 'tensor_tensor', out=sqA, in0=rTM[:], in1=kmTM[:], op=ALU.mult)
                I(P, 'dve', 'tensor_tensor', out=sqA, in0=sqA, in1=rkr[:].unsqueeze(1).to_broadcast([64, NCH, 512]), op=ALU.mult)
                I(P, 'dve', 'tensor_reduce', out=bs[:], in_=sq3, axis=AX.X, op=ALU.add)
                I(P, 'dve', 'tensor_tensor', out=sq3, in0=vTM[:].rearrange("p n (h d) -> p (n h) d", d=64), in1=bc(bs), op=ALU.mult)
                I(P, 'dve', 'tensor_tensor', out=OcA, in0=OcA, in1=sqA, op=ALU.add)
                I(P, 'dve', 'tensor_tensor', out=ob[:], in0=OcA, in1=gTM[:], op=ALU.mult)
                oT = obT[g % 2]
                for n in range(NCH):
                    ps = nxt()
                    pb_ = ps[:].bitcast(BF16)
                    for h in range(8):
                        I(P, 'pe', 'transpose', out=pb_[0:64, h * 64:(h + 1) * 64], in_=ob[:, n, h * 64:(h + 1) * 64], identity=ident[0:64, 0:64])
                    I(P, 'act', 'activation', out=oT[:, :, n * 64:(n + 1) * 64], in_=pb_[0:64, 0:512].rearrange("p (h c) -> p h c", c=64), func=AF.Copy, wsub=n)
                P.dma('pool', mixedT[512:1024, sl].rearrange("(h p) t -> p h t", p=64), oT[:], reads=[oT], writes=[(mixedT, ('ob', b, g))], ctr=oT)


A_W, KVL, IH, ID_ = 512, 128, 8, 64
N_IN_A = 1224


def perm_cols():
    q = list(range(0, 512))
    clat = list(range(512, 640))
    qidx = list(range(640, 1152))
    kidx = list(range(1152, 1216))
    B0 = N_IN_A
    r = list(range(B0, B0 + 512))
    k = list(range(B0 + 512, B0 + 1024))
    v = list(range(B0 + 1024, B0 + 1536))
    xwxa = list(range(B0 + 1536, B0 + 1664))
    xg = list(range(B0 + 1664, B0 + 1792))
    return q + qidx + clat + kidx + kidx + r + k + v + xwxa + xg


def host_consts(S):
    f = np.float32
    t = np.arange(S)
    slopes = 2.0 ** (-np.arange(1, 9, dtype=np.float64))
    kaug = np.stack([np.ones(S), np.ones(S), 64.0 * (t // 64), (t % 64) * 1.0]).astype(f)
    qaug = np.stack([np.stack([-s * 64.0 * (t // 64), -s * (t % 64), s * np.ones(S), s * np.ones(S)]) for s in slopes]).astype(f)
    ss, tt = np.meshgrid(np.arange(128), np.arange(128), indexing='ij')
    same = (ss // 64) == (tt // 64)
    ccorr = np.stack([np.where(same, -2.0 * s * np.maximum(ss - tt, 0), 0.0) for s in slopes], axis=1).astype(f)
    tq, sk = np.meshgrid(np.arange(128), np.arange(128), indexing='ij')
    dmask = np.where(sk < (tq // 64 + 1) * 64, 0.0, -1e30).astype(f)
    sel65 = np.zeros((65, 64), f)
    sel65[64, :] = 1.0
    return dict(kaug=kaug, qaug=qaug, ccorr=ccorr, dmask=dmask, sel65=sel65,
                ident=np.eye(128, dtype=f), ones128=np.ones((128, 128), f))


def rwkv_consts():
    f = np.float32
    i = np.arange(128)
    bones = ((i[:, None] // 64) == (i[None, :] // 64)).astype(f)
    s_, t_ = np.meshgrid(np.arange(64), np.arange(64), indexing='ij')
    mask1 = np.concatenate([(s_ < t_), (s_ <= t_)], axis=1).astype(f)
    mask3 = (t_ < s_).astype(f)
    rst = np.ones((128, 256), f)
    rst[:, 0::64] = 0.0
    return dict(bones=bones, mask1=mask1, mask3=mask3, rst=rst)


def rwkv_params(w0, w2, a0, a2, g2, k_k, k_a, r_k, ln_w, ln_b):
    f = np.float32
    col = lambda v: np.asarray(v, f).reshape(8, 64).T
    vec8 = np.ascontiguousarray(np.stack([col(w0), col(a0), col(k_k), col(k_a), col(np.asarray(r_k, f).reshape(512))], axis=1))
    return dict(w2=np.ascontiguousarray(np.asarray(w2, f)), a2=np.ascontiguousarray(np.asarray(a2, f)), g2=np.ascontiguousarray(np.asarray(g2, f)),
                vec8=vec8, ln_w=np.ascontiguousarray(np.asarray(ln_w, f).reshape(512)), ln_b=np.ascontiguousarray(np.asarray(ln_b, f).reshape(512)),
                r_k1=np.ascontiguousarray(np.asarray(r_k, f).reshape(512)))


NCORES = 8
NB_ = 4
S_ = 2048


def build_nc(shapes):
    NB, S = NB_, S_
    NT = NB * S
    nc = bass.Bass("TRN2", target_bir_lowering=False)
    A = {k: nc.dram_tensor(k, list(shp), F32, kind="ExternalInput").ap() for k, shp in shapes.items()}
    out = nc.dram_tensor("out", [NT, D], F32, kind="ExternalOutput").ap()
    mod_d = nc.dram_tensor("mod_d", [NB, 6, D], F32, kind="Internal").ap()
    projT = nc.dram_tensor("projT", [NPROJ, NT], BF16, kind="Internal").ap()
    widxT = nc.dram_tensor("widxT", [8, NT], F32, kind="Internal").ap()
    mixedT = nc.dram_tensor("mixedT", [1024, NT], BF16, kind="Internal").ap()
    with contextlib.ExitStack() as stack:
        P = Prog(nc, stack)
        phase_mod(P, nc, A['cT'], A['w_ada'], A['b_ada'], A['g_mix'], A['g_ffn'], mod_d, NB)
        P.barrier()
        phase_inproj(P, nc, A['x'], projT, widxT, A['w_inp'], A['w_widx'], A['mu'], mod_d, A['ident'], NB, S)
        P.barrier()
        phase_dsa(P, nc, projT, widxT, mixedT, A, NB, S, min(256, S // 4))
        P.barrier()
        phase_rwkv(P, nc, projT, mixedT, A, NB, S)
        P.barrier()
        phase_outproj(P, nc, A['x'], out, mixedT, A['w_out'], mod_d, NB, S)
        P.barrier()
        outs = phase_ffn(P, nc, out, out, A['w_ff1'], A['w_ff2'], mod_d, A['ident'], NB, S)
        P.emit(final_wait_ops=outs)
    return nc


def kernel(x, c, w_ada, b_ada, g_mix, g_ffn, w_in, g_q, g_k, g_kv, w_uk, w_uv, mu_shift, w0, w2, a0, a2, g2,
           k_k, k_a, r_k, ln_w, ln_b, w_out, w_ff1, w_ff2):
    f = np.float32
    x = np.asarray(x, f); c = np.asarray(c, f)
    B, S, Dm = x.shape
    NB = B // NCORES
    pc = perm_cols()
    hc = host_consts(S)
    w_in0 = np.asarray(w_in, f)[0]
    mu_full = np.zeros(w_in0.shape[1], f)
    mu_full[N_IN_A:] = np.asarray(mu_shift, f)[0]
    mu_perm = mu_full[pc]
    l0 = lambda a: np.asarray(a, f)[0]
    shared = dict(
        w_ada=np.ascontiguousarray(l0(w_ada)), b_ada=np.ascontiguousarray(l0(b_ada)),
        g_mix=np.ascontiguousarray(l0(g_mix)), g_ffn=np.ascontiguousarray(l0(g_ffn)),
        w_inp=np.ascontiguousarray(w_in0[:, pc]), w_widx=np.ascontiguousarray(w_in0[:, 1216:1224]),
        mu=np.ascontiguousarray(mu_perm[1280:].reshape(14, 128).T),
        ident=hc['ident'],
        w_uk=np.ascontiguousarray(l0(w_uk).reshape(128, 512)), w_uv=np.ascontiguousarray(l0(w_uv).reshape(128, 512)),
        g_q=np.ascontiguousarray(l0(g_q).reshape(64, 1)), g_k=np.ascontiguousarray(l0(g_k).reshape(64, 1)),
        g_kv=np.ascontiguousarray(l0(g_kv).reshape(128, 1)),
        kaug=hc['kaug'], qaug=hc['qaug'], ccorr=hc['ccorr'], dmask=hc['dmask'], sel65=hc['sel65'], ones128=hc['ones128'],
        w_out=np.ascontiguousarray(l0(w_out)), w_ff1=np.ascontiguousarray(l0(w_ff1)), w_ff2=np.ascontiguousarray(l0(w_ff2)),
        **rwkv_consts(),
        **rwkv_params(l0(w0), l0(w2), l0(a0), l0(a2), l0(g2), l0(k_k), l0(k_a), l0(r_k), l0(ln_w), l0(ln_b)),
    )
    in_maps = []
    for i in range(NCORES):
        m = dict(shared)
        m['x'] = np.ascontiguousarray(x[i * NB:(i + 1) * NB].reshape(NB * S, Dm))
        ci = c[i * NB:(i + 1) * NB]
        m['cT'] = np.ascontiguousarray(ci.T.reshape(8, 128, NB).transpose(1, 0, 2))
        in_maps.append(m)
    nc = build_nc({k: v.shape for k, v in in_maps[0].items()})
    res = run_bass_kernel_spmd(nc, in_maps, core_ids=list(range(NCORES)))
    outs = [np.asarray(r["out"], f).reshape(NB, S, Dm) for r in res.results]
    return np.concatenate(outs, axis=0)
```
